# Optimizing a Trainium2 kernel written in Bass

```python
import math
import jax, jax.numpy as jnp
from jax import lax
import numpy as np

D_MODEL = 2048
BATCH = 4
SEQ = 4096
DEPTH = 1

HEAD_DIM = 128
NSA_HEADS = 8
NSA_KV_GROUPS = 2
NSA_REP = NSA_HEADS // NSA_KV_GROUPS
NSA_CMP_LEN = 32
NSA_CMP_STRIDE = 16
NSA_SEL_LEN = 64
NSA_TOP_N = 16
NSA_WINDOW = 512
NSA_SEL_QBLOCK = 64
DIL_CONFIGS = ((128, 1), (512, 4), (2048, 16))
DIL_HEADS_PER_GROUP = 4
DIL_HEADS = DIL_HEADS_PER_GROUP * len(DIL_CONFIGS)
DIL_OUT = DIL_HEADS_PER_GROUP * HEAD_DIM
ATTN_QBLOCK = 128
REL_BUCKETS = 32
REL_MAX_DIST = 2048
N_BIAS_HEADS = NSA_HEADS + DIL_HEADS
PEER_HEADS = 8
PEER_N_KEYS = 128
PEER_N_EXPERTS = PEER_N_KEYS * PEER_N_KEYS
PEER_TOPK = 16
PEER_HALF = 128
PEER_TBLOCK = 128

EPS = 1e-6
NEG = -1e30
SEL_FORCE = 1e4

NSA_Q_COLS = NSA_HEADS * HEAD_DIM
NSA_KV_COLS = 3 * 2 * NSA_KV_GROUPS * HEAD_DIM
NSA_GATE_COLS = 3 * NSA_HEADS
DIL_COLS = 3 * DIL_HEADS * HEAD_DIM
MERGE_COLS = 2 * D_MODEL
OFF_KV = NSA_Q_COLS
OFF_GATE = OFF_KV + NSA_KV_COLS
OFF_DIL = OFF_GATE + NSA_GATE_COLS
OFF_MERGE = OFF_DIL + DIL_COLS
IN_COLS = OFF_MERGE + MERGE_COLS

kernel_name = "hybrid_nsa_dilated_peer_block"


def rms_norm(x, g):
    xf = x.astype(jnp.float32)
    y = xf * lax.rsqrt(jnp.mean(xf * xf, axis=-1, keepdims=True) + EPS)
    return (y * g.astype(jnp.float32)).astype(x.dtype)


def t5_bucket(dist):
    n = jnp.maximum(dist, 0)
    max_exact = REL_BUCKETS // 2
    nf = jnp.maximum(n, max_exact).astype(jnp.float32)
    large = max_exact + (jnp.log(nf / max_exact) / math.log(REL_MAX_DIST / max_exact)
                         * (REL_BUCKETS - max_exact)).astype(jnp.int32)
    large = jnp.minimum(large, REL_BUCKETS - 1)
    return jnp.where(n < max_exact, n, large)


def to_heads(t, n_heads):
    b, s, _ = t.shape
    return t.reshape(b, s, n_heads, HEAD_DIM).transpose(0, 2, 1, 3)


def banded_attention(q, k, v, span, dist_scale, tbl):
    b, hq, L, dh = q.shape
    hk = k.shape[1]
    rep = hq // hk
    qb = ATTN_QBLOCK
    nb = -(-L // qb)
    lp = nb * qb
    pad_end = lp - L
    n_prev = -(-span // qb)
    w = (n_prev + 1) * qb
    q = jnp.pad(q, ((0, 0), (0, 0), (0, pad_end), (0, 0)))
    kp = jnp.pad(k, ((0, 0), (0, 0), (n_prev * qb, pad_end), (0, 0))).reshape(b, hk, nb + n_prev, qb, dh)
    vp = jnp.pad(v, ((0, 0), (0, 0), (n_prev * qb, pad_end), (0, 0))).reshape(b, hk, nb + n_prev, qb, dh)
    kw = jnp.concatenate([kp[:, :, j:j + nb] for j in range(n_prev + 1)], axis=3)
    vw = jnp.concatenate([vp[:, :, j:j + nb] for j in range(n_prev + 1)], axis=3)
    qr = q.reshape(b, hk, rep, nb, qb, dh)
    logits = jnp.einsum('bgrnqd,bgnkd->bgrnqk', qr, kw).astype(jnp.float32) * (dh ** -0.5)
    dist = jnp.arange(qb)[:, None] + n_prev * qb - jnp.arange(w)[None, :]
    kpos = (jnp.arange(nb)[:, None] - n_prev) * qb + jnp.arange(w)[None, :]
    valid = ((dist >= 0) & (dist <= span))[None, :, :] & (kpos >= 0)[:, None, :]
    bias = jnp.take(tbl.astype(jnp.float32), t5_bucket(dist * dist_scale), axis=1)
    bias = bias.reshape(hk, rep, 1, qb, w)
    logits = jnp.where(valid, logits + bias, NEG)
    lse = jax.nn.logsumexp(logits, axis=-1)
    p = jnp.exp(logits - lse[..., None])
    out = jnp.einsum('bgrnqk,bgnkd->bgrnqd', p.astype(v.dtype), vw)
    out = out.reshape(b, hq, lp, dh)[:, :, :L]
    lse = lse.reshape(b, hq, lp)[:, :, :L]
    return out, lse


def nsa_mixer(q, kv, gates, k_gain, cmp_pos, cmp_w1, cmp_w2, tbl):
    b, nh, s, dh = q.shape
    G, rep = NSA_KV_GROUPS, NSA_REP
    scale = dh ** -0.5
    k_cmp, v_cmp, k_sel, v_sel, k_win, v_win = [kv[:, i] for i in range(6)]
    k_sel = rms_norm(k_sel, k_gain[1])
    k_win = rms_norm(k_win, k_gain[2])
    tbl = tbl.astype(jnp.float32)

    nc = (s - NSA_CMP_LEN) // NSA_CMP_STRIDE + 1
    cidx = jnp.arange(nc)[:, None] * NSA_CMP_STRIDE + jnp.arange(NSA_CMP_LEN)[None, :]

    def compress(t, j):
        blk = t[:, :, cidx] + cmp_pos[j]
        hid = jax.nn.gelu(blk.reshape(b, G, nc, NSA_CMP_LEN * dh) @ cmp_w1[j])
        return hid @ cmp_w2[j]

    kc = rms_norm(compress(k_cmp, 0), k_gain[0])
    vc = compress(v_cmp, 1)
    qg = q.reshape(b, G, rep, s, dh)
    logits = jnp.einsum('bgrsd,bgcd->bgrsc', qg, kc).astype(jnp.float32) * scale
    cend = jnp.arange(nc) * NSA_CMP_STRIDE + NSA_CMP_LEN - 1
    cdist = jnp.arange(s)[:, None] - cend[None, :]
    cvalid = cdist >= 0
    cbias = jnp.take(tbl, t5_bucket(cdist), axis=1).reshape(G, rep, s, nc)
    p_cmp = jax.nn.softmax(jnp.where(cvalid, logits + cbias, NEG), axis=-1) * cvalid
    o_cmp = jnp.einsum('bgrsc,bgcd->bgrsd', p_cmp.astype(vc.dtype), vc).reshape(b, nh, s, dh)

    ns = s // NSA_SEL_LEN
    cstart = jnp.arange(nc) * NSA_CMP_STRIDE
    sstart = jnp.arange(ns) * NSA_SEL_LEN
    overlap = ((cstart[:, None] < sstart[None, :] + NSA_SEL_LEN)
               & (cstart[:, None] + NSA_CMP_LEN > sstart[None, :])).astype(jnp.float32)
    imp = jnp.einsum('bgrsc,cj->bgsj', p_cmp, overlap)
    cur = (jnp.arange(s) // NSA_SEL_LEN)[:, None]
    jb = jnp.arange(ns)[None, :]
    forced = (jb == 0) | (jb == cur) | (jb == cur - 1)
    score = jnp.where(forced, SEL_FORCE, jnp.where(jb > cur, -SEL_FORCE, imp))
    n_sel = min(NSA_TOP_N, ns)
    _, sel_idx = lax.top_k(score, n_sel)

    kb = k_sel.reshape(b, G, ns, NSA_SEL_LEN, dh)
    vb = v_sel.reshape(b, G, ns, NSA_SEL_LEN, dh)
    gather = jax.vmap(jax.vmap(lambda blocks, idx: blocks[idx]))
    tbl_g = tbl.reshape(G, rep, REL_BUCKETS)
    g_ix = jnp.arange(G)[:, None, None, None]
    r_ix = jnp.arange(rep)[:, None, None]
    qs = NSA_SEL_QBLOCK
    nqb = s // qs
    m = n_sel * NSA_SEL_LEN

    def sel_block(args):
        qblk, iblk, q0 = args
        ks = gather(kb, iblk).reshape(b, G, qs, m, dh)
        vs = gather(vb, iblk).reshape(b, G, qs, m, dh)
        kpos = (iblk[..., None] * NSA_SEL_LEN + jnp.arange(NSA_SEL_LEN)).reshape(b, G, qs, m)
        dist = (q0 + jnp.arange(qs))[None, None, :, None] - kpos
        bias = tbl_g[g_ix, r_ix, t5_bucket(dist)[:, :, None]]
        lg = jnp.einsum('bgrqd,bgqmd->bgrqm', qblk, ks).astype(jnp.float32) * scale
        lg = jnp.where((dist >= 0)[:, :, None], lg + bias, NEG)
        p = jax.nn.softmax(lg, axis=-1)
        return jnp.einsum('bgrqm,bgqmd->bgrqd', p.astype(vs.dtype), vs)

    q_blocks = qg.reshape(b, G, rep, nqb, qs, dh).transpose(3, 0, 1, 2, 4, 5)
    i_blocks = sel_idx.reshape(b, G, nqb, qs, n_sel).transpose(2, 0, 1, 3, 4)
    starts = jnp.arange(nqb, dtype=jnp.int32) * qs
    o_sel = lax.map(sel_block, (q_blocks, i_blocks, starts))
    o_sel = o_sel.transpose(1, 2, 3, 0, 4, 5).reshape(b, nh, s, dh)

    o_win, _ = banded_attention(q, k_win, v_win, NSA_WINDOW - 1, 1, tbl)

    g = gates.transpose(0, 2, 1, 3)
    o = g[..., 0:1] * o_cmp + g[..., 1:2] * o_sel + g[..., 2:3] * o_win
    return o.transpose(0, 2, 1, 3).reshape(b, s, nh * dh)


def dilated_mixer(q, k, v, tbl):
    b, _, s, dh = q.shape
    P = DIL_HEADS_PER_GROUP
    outs, lses = [], []
    for gi, (window, dil) in enumerate(DIL_CONFIGS):
        lo, hi = gi * P, (gi + 1) * P
        L = s // dil

        def split(t):
            return t[:, lo:hi].reshape(b, P, L, dil, dh).transpose(0, 1, 3, 2, 4).reshape(b, P * dil, L, dh)

        o, lse = banded_attention(split(q), split(k), split(v), window // dil, dil,
                                  jnp.repeat(tbl[lo:hi], dil, axis=0))
        outs.append(o.reshape(b, P, dil, L, dh).transpose(0, 1, 3, 2, 4).reshape(b, P, s, dh))
        lses.append(lse.reshape(b, P, dil, L).transpose(0, 1, 3, 2).reshape(b, P, s))
    w = jax.nn.softmax(jnp.stack(lses, axis=0), axis=0)
    o = jnp.sum(w[..., None].astype(q.dtype) * jnp.stack(outs, axis=0), axis=0)
    return o.transpose(0, 2, 1, 3).reshape(b, s, P * dh)


def token_mixer(h, w_in, nsa_q_gain, nsa_k_gain, cmp_pos, cmp_w1, cmp_w2, dil_q_gain, dil_k_gain,
                w_br_nsa, w_br_dil, w_out, tbl_nsa, tbl_dil):
    b, s, _ = h.shape
    proj = h @ w_in
    q_n = rms_norm(to_heads(proj[..., :OFF_KV], NSA_HEADS), nsa_q_gain)
    kv_n = to_heads(proj[..., OFF_KV:OFF_GATE], 6 * NSA_KV_GROUPS).reshape(b, 6, NSA_KV_GROUPS, s, HEAD_DIM)
    gates_n = jax.nn.sigmoid(proj[..., OFF_GATE:OFF_DIL].reshape(b, s, NSA_HEADS, 3))
    qkv_d = to_heads(proj[..., OFF_DIL:OFF_MERGE], 3 * DIL_HEADS).reshape(b, 3, DIL_HEADS, s, HEAD_DIM)
    merge_g = jax.nn.sigmoid(proj[..., OFF_MERGE:])
    y_nsa = nsa_mixer(q_n, kv_n, gates_n, nsa_k_gain, cmp_pos, cmp_w1, cmp_w2, tbl_nsa)
    y_dil = dilated_mixer(rms_norm(qkv_d[:, 0], dil_q_gain), rms_norm(qkv_d[:, 1], dil_k_gain),
                          qkv_d[:, 2], tbl_dil)
    merged = merge_g[..., :D_MODEL] * (y_nsa @ w_br_nsa) + merge_g[..., D_MODEL:] * (y_dil @ w_br_dil)
    return merged @ w_out


def peer_ffn(h, w_q, q_gain, sub_keys, u, v):
    b, s, d = h.shape
    t = b * s
    xt = h.reshape(t, d)
    q = rms_norm((xt @ w_q).reshape(t, PEER_HEADS, 2, PEER_HALF), q_gain)
    sc = jnp.einsum('thpk,hpnk->thpn', q, sub_keys).astype(jnp.float32)
    s1, i1 = lax.top_k(sc[:, :, 0], PEER_TOPK)
    s2, i2 = lax.top_k(sc[:, :, 1], PEER_TOPK)
    cand = (s1[..., :, None] + s2[..., None, :]).reshape(t, PEER_HEADS, PEER_TOPK * PEER_TOPK)
    cand_idx = (i1[..., :, None] * PEER_N_KEYS + i2[..., None, :]).reshape(t, PEER_HEADS, PEER_TOPK * PEER_TOPK)
    top_s, pos = lax.top_k(cand, PEER_TOPK)
    expert = jnp.take_along_axis(cand_idx, pos, axis=-1)
    gate = jax.nn.softmax(top_s, axis=-1).astype(h.dtype)
    nb = t // PEER_TBLOCK
    hk = PEER_HEADS * PEER_TOPK

    def block(args):
        xb, eb, gb = args
        act = jax.nn.gelu(jnp.einsum('td,ted->te', xb, u[eb]))
        return jnp.einsum('te,ted->td', gb * act, v[eb])

    out = lax.map(block, (xt.reshape(nb, PEER_TBLOCK, d), expert.reshape(nb, PEER_TBLOCK, hk),
                          gate.reshape(nb, PEER_TBLOCK, hk)))
    return out.reshape(b, s, d)


def _normal(key, shape, scale):
    return jax.random.normal(key, shape, jnp.float32) * scale


def setup_inputs(seed: int = 0) -> dict:
    key = jax.random.key(seed)
    ks = jax.random.split(key, 24)
    D, L, dh = D_MODEL, DEPTH, HEAD_DIM
    return {
        "x": _normal(ks[0], (BATCH, SEQ, D), 1.0),
        "c": _normal(ks[1], (BATCH, D), 1.0),
        "w_ada": _normal(ks[2], (L, D, 6 * D), 0.5 * D ** -0.5),
        "b_ada": _normal(ks[3], (L, 6 * D), 0.01),
        "norm1_g": 1.0 + _normal(ks[4], (L, D), 0.01),
        "w_in": _normal(ks[5], (L, D, IN_COLS), D ** -0.5),
        "nsa_q_gain": 1.0 + _normal(ks[6], (L, dh), 0.01),
        "nsa_k_gain": 1.0 + _normal(ks[7], (L, 3, dh), 0.01),
        "cmp_pos": _normal(ks[8], (L, 2, NSA_CMP_LEN, dh), 0.02),
        "cmp_w1": _normal(ks[9], (L, 2, NSA_CMP_LEN * dh, dh), (NSA_CMP_LEN * dh) ** -0.5),
        "cmp_w2": _normal(ks[10], (L, 2, dh, dh), dh ** -0.5),
        "dil_q_gain": 1.0 + _normal(ks[11], (L, dh), 0.01),
        "dil_k_gain": 1.0 + _normal(ks[12], (L, dh), 0.01),
        "w_br_nsa": _normal(ks[13], (L, NSA_HEADS * dh, D), (NSA_HEADS * dh) ** -0.5),
        "w_br_dil": _normal(ks[14], (L, DIL_OUT, D), DIL_OUT ** -0.5),
        "w_out": _normal(ks[15], (L, D, D), D ** -0.5),
        "norm2_g": 1.0 + _normal(ks[16], (L, D), 0.01),
        "peer_w_q": _normal(ks[17], (L, D, PEER_HEADS * 2 * PEER_HALF), D ** -0.5),
        "peer_q_gain": 1.0 + _normal(ks[18], (L, PEER_HEADS, 2, PEER_HALF), 0.01),
        "peer_sub_keys": _normal(ks[19], (L, PEER_HEADS, 2, PEER_N_KEYS, PEER_HALF), PEER_HALF ** -0.5),
        "peer_u": _normal(ks[20], (L, PEER_N_EXPERTS, D), D ** -0.5),
        "peer_v": _normal(ks[21], (L, PEER_N_EXPERTS, D), 0.5),
        "rel_bias": _normal(ks[22], (N_BIAS_HEADS, REL_BUCKETS), 0.1),
    }


def reference(x, c, w_ada, b_ada, norm1_g, w_in, nsa_q_gain, nsa_k_gain, cmp_pos, cmp_w1, cmp_w2,
              dil_q_gain, dil_k_gain, w_br_nsa, w_br_dil, w_out, norm2_g, peer_w_q, peer_q_gain,
              peer_sub_keys, peer_u, peer_v, rel_bias):
    tbl_nsa = rel_bias[:NSA_HEADS]
    tbl_dil = rel_bias[NSA_HEADS:]
    cond = jax.nn.silu(c)
    for layer in range(DEPTH):
        mod = cond @ w_ada[layer] + b_ada[layer]
        sh1, sc1, g1, sh2, sc2, g2 = [m[:, None, :] for m in jnp.split(mod, 6, axis=-1)]
        h = rms_norm(x, norm1_g[layer]) * (1.0 + sc1) + sh1
        x = x + g1 * token_mixer(h, w_in[layer], nsa_q_gain[layer], nsa_k_gain[layer], cmp_pos[layer],
                                 cmp_w1[layer], cmp_w2[layer], dil_q_gain[layer], dil_k_gain[layer],
                                 w_br_nsa[layer], w_br_dil[layer], w_out[layer], tbl_nsa, tbl_dil)
        h = rms_norm(x, norm2_g[layer]) * (1.0 + sc2) + sh2
        x = x + g2 * peer_ffn(h, peer_w_q[layer], peer_q_gain[layer], peer_sub_keys[layer],
                              peer_u[layer], peer_v[layer])
    return x
```

```python
import math
from contextlib import ExitStack
import numpy as np
import concourse.bass as bass
import concourse.mybir as mybir
from concourse.bass_utils import run_bass_kernel_spmd

F32 = mybir.dt.float32
BF16 = mybir.dt.bfloat16
I32 = mybir.dt.int32
U32 = mybir.dt.uint32
AF = mybir.ActivationFunctionType
ALU = mybir.AluOpType
AX = mybir.AxisListType

D = 2048
KC = 16
NLOC = 4096
NOWN = 2048
DH = 128
OFF_KV = 1024
OFF_GATE = OFF_KV + 1536
OFF_DIL = OFF_GATE + 24
OFF_MERGE = OFF_DIL + 4608
IN_COLS = OFF_MERGE + 4096
DIL_CFG = ((128, 1), (512, 4), (2048, 16))
EPS = 1e-6
NEGB = -30000.0
SCALE = DH ** -0.5
SB_BASE = 16640
SB_END = 229344


class Op:
    __slots__ = ("eng", "fn", "deps", "sig", "need", "isdma", "idx")


class Sched:
    COMPUTE = ("pe", "act", "dve", "pool")
    RING = 8

    def __init__(self, nc, same_engine_sync=True):
        self.nc = nc
        self.ops = []
        self.last_w = {}
        self.readers = {}
        self.ses = same_engine_sync
        self.last_on = {}
        self.dma_pend = []

    def add(self, eng, fn, r=(), w=(), dma=False, extra_deps=()):
        op = Op()
        op.eng = eng; op.fn = fn; op.isdma = dma; op.need = dma; op.sig = None
        op.idx = len(self.ops)
        deps = set(extra_deps)
        for k in r:
            lw = self.last_w.get(k)
            if lw is not None:
                deps.add(lw)
        for k in w:
            lw = self.last_w.get(k)
            if lw is not None:
                deps.add(lw)
            for rd in self.readers.get(k, ()):
                deps.add(rd)
        deps.discard(op.idx)
        fd = []
        for d in deps:
            o = self.ops[d]
            if (not o.isdma) and o.eng == eng and (eng == "pe" or not self.ses):
                continue
            o.need = True
            fd.append(d)
        op.deps = sorted(fd)
        self.ops.append(op)
        for k in w:
            self.last_w[k] = op.idx
            self.readers[k] = []
        for k in r:
            lst = self.readers.setdefault(k, [])
            if not dma:
                lst[:] = [x for x in lst if self.ops[x].isdma or self.ops[x].eng != eng]
            lst.append(op.idx)
        if not dma:
            self.last_on[eng] = op.idx
        else:
            self.dma_pend.append(op.idx)
        return op.idx

    def dma(self, eng, out, in_, r=(), w=(), **kw):
        return self.add(eng, lambda e: e.dma_start(out=out, in_=in_, **kw), r=r, w=w, dma=True)

    def barrier(self):
        lasts = dict(self.last_on)
        engs = ["pe", "act", "dve", "pool", "sp"]
        deps = list(lasts.values()) + list(self.dma_pend)
        for e in engs:
            self.add(e, lambda en: en.nop(), extra_deps=[d for d in deps])
        self.dma_pend = []
        self.last_w = {}
        self.readers = {}

    def emit(self, stack):
        nc = self.nc
        engs = sorted({o.eng for o in self.ops})
        csem = {}
        for e in engs:
            csem[e] = stack.enter_context(nc.semaphore("c_" + e))
        dring = {}
        for e in engs:
            if any(o.isdma and o.eng == e for o in self.ops):
                dring[e] = [stack.enter_context(nc.semaphore("d_%s%d" % (e, i))) for i in range(self.RING)]
        ccount = {e: 0 for e in engs}
        dcount = {e: 0 for e in engs}
        ringtot = {e: [0] * self.RING for e in engs}
        pre = {}
        for o in self.ops:
            if o.isdma:
                n = dcount[o.eng]; dcount[o.eng] += 1
                slot = n % self.RING
                prev = ringtot[o.eng][slot]
                ringtot[o.eng][slot] += 16
                pre[o.idx] = (dring[o.eng][slot], prev)
                o.sig = (dring[o.eng][slot], prev + 16, 16)
            elif o.need:
                ccount[o.eng] += 1
                o.sig = (csem[o.eng], ccount[o.eng], 1)
        per = {e: [o for o in self.ops if o.eng == e] for e in engs}
        self.stats = {e: (len(per[e]), ccount[e], dcount[e]) for e in engs}
        ops = self.ops
        blk = stack.enter_context(nc.Block())

        def runner(e):
            def body(engine):
                seen = {}
                for o in per[e]:
                    waits = []
                    if o.isdma:
                        s, v = pre[o.idx]
                        if v > 0:
                            waits.append((s, v))
                    for d in o.deps:
                        s, v, _ = ops[d].sig
                        waits.append((s, v))
                    for s, v in waits:
                        key = id(s)
                        if seen.get(key, 0) < v:
                            engine.wait_ge(s, v)
                            seen[key] = v
                    ins = o.fn(engine)
                    if o.sig is not None:
                        ins.then_inc(o.sig[0], o.sig[2])
            return body

        for e in engs:
            reg = {"pe": blk.tensor, "act": blk.scalar, "dve": blk.vector,
                   "pool": blk.gpsimd, "sp": blk.sync}[e]
            reg(runner(e))


def _dtsize(dt):
    return 2 if dt == BF16 else 4


class Builder:
    def __init__(self, stop_after=None, dbg=()):
        self.nc = bass.Bass("TRN2", target_bir_lowering=False)
        self.S = Sched(self.nc)
        self.off = SB_BASE
        self.cnt = 0
        self.stop_after = stop_after
        self.dbg = dbg
        self.inputs = {}
        self.outs = {}
        nc = self.nc
        self.psf = [nc.alloc_psum_tensor("psb%d" % i, [128, 512], F32) for i in range(8)]

    def sb(self, name, shape, dt):
        nbytes = int(np.prod(shape[1:])) * _dtsize(dt)
        self.cnt += 1
        t = self.nc.alloc_sbuf_tensor_at("%s_%d" % (name, self.cnt), list(shape), dt, offset=self.off)
        self.off += (nbytes + 63) // 64 * 64
        self.maxoff = max(getattr(self, "maxoff", 0), self.off)
        assert self.off <= SB_END, ("SBUF overflow", name, self.off)
        return t

    def mark(self):
        return self.off

    def release(self, m):
        self.S.barrier()
        self.off = m

    def din(self, name, shape, dt=F32):
        t = self.nc.dram_tensor(name, list(shape), dt, kind="ExternalInput")
        self.inputs[name] = t
        return t

    def dout(self, name, shape, dt=F32):
        t = self.nc.dram_tensor(name, list(shape), dt, kind="ExternalOutput")
        self.outs[name] = t
        return t

    def dscratch(self, name, shape, dt):
        return self.nc.dram_tensor(name, list(shape), dt)

    def psb(self, i, dt=F32):
        a = self.psf[i][:]
        if dt == BF16:
            a = a.bitcast(BF16)
        return a

    def PE(self, fn, r, w):
        return self.S.add("pe", fn, r=r, w=w)

    def mm(self, out, lhsT, rhs, start, stop, r, w):
        return self.S.add("pe", lambda e: e.matmul(out, lhsT=lhsT, rhs=rhs, start=start, stop=stop), r=r, w=w)

    def tr(self, out, in_, ident, r, w):
        return self.S.add("pe", lambda e: e.transpose(out, in_, ident), r=r, w=w)

    def ACT(self, out, in_, func, r, w, **kw):
        return self.S.add("act", lambda e: e.activation(out=out, in_=in_, func=func, **kw), r=r, w=w)

    def V(self, eng, name, r, w, *a, **kw):
        return self.S.add(eng, lambda e: getattr(e, name)(*a, **kw), r=r, w=w)

    def DVE(self, name, r, w, *a, **kw):
        return self.V("dve", name, r, w, *a, **kw)

    def POOL(self, name, r, w, *a, **kw):
        return self.V("pool", name, r, w, *a, **kw)

    def dump(self, name, src_tensor, shape, dt, rkeys):
        o = self.dout("dbg_" + name, shape, dt)
        self.S.dma("sp", o.ap(), src_tensor[:], r=rkeys)

    def phase0(self):
        S = self.S
        self.ident_f = self.sb("ident_f", [128, 128], F32)
        self.ident_b = self.sb("ident_b", [128, 128], BF16)
        self.ones_b = self.sb("ones_b", [128, 128], BF16)
        self.ones_f = self.sb("ones_f", [128, 128], F32)
        self.zero1 = self.sb("zero1", [128, 1], F32)
        self.pfxb = self.sb("pfxb", [128, 1], F32)
        idf = self.ident_f
        self.POOL("memset", [], ["ident_f"], idf[:], 0.0)
        S.add("pool", lambda e: e.affine_select(out=idf[:], in_=idf[:], pattern=[[-1, 128]],
                                                compare_op=ALU.not_equal, fill=1.0, base=0,
                                                channel_multiplier=1), r=["ident_f"], w=["ident_f"])
        self.DVE("tensor_copy", ["ident_f"], ["ident_b"], out=self.ident_b[:], in_=idf[:])
        self.POOL("memset", [], ["ones_b"], self.ones_b[:], 1.0)
        self.POOL("memset", [], ["ones_f"], self.ones_f[:], 1.0)
        self.POOL("memset", [], ["zero1"], self.zero1[:], 0.0)
        pf = self.din("pfx_bias", [128, 1])
        S.dma("sp", self.pfxb[:], pf.ap(), w=["pfxb"])

        c_in = self.din("c_fm", [128, 16])
        w_ada = self.din("w_ada", [D, 6 * D])
        b_ada = self.din("b_ada_fm", [128, 96])
        n1g = self.din("norm1_g_fm", [128, 16])
        n2g = self.din("norm2_g_fm", [128, 16])
        self.modT = self.sb("modT", [128, 96], F32)
        self.A1 = self.sb("A1", [128, 16], F32)
        self.A2 = self.sb("A2", [128, 16], F32)
        m = self.mark()
        cT = self.sb("cT", [128, 16], F32)
        sg = self.sb("sg", [128, 16], F32)
        condT = self.sb("condT", [128, 16], F32)
        bT = self.sb("bT", [128, 96], F32)
        g1T = self.sb("g1T", [128, 16], F32)
        g2T = self.sb("g2T", [128, 16], F32)
        wa = [self.sb("wa%d" % i, [128, 6 * D], BF16) for i in range(3)]
        condB = self.sb("condB", [128, 16], BF16)
        S.dma("sp", cT[:], c_in.ap(), w=["cT"])
        S.dma("sp", bT[:], b_ada.ap(), w=["bT"])
        S.dma("sp", g1T[:], n1g.ap(), w=["g1T"])
        S.dma("sp", g2T[:], n2g.ap(), w=["g2T"])
        self.ACT(condT[:], cT[:], AF.Silu, ["cT"], ["condT"])
        self.DVE("tensor_copy", ["condT"], ["condB"], out=condB[:], in_=condT[:])
        ps = self.psb(0)
        for k in range(KC):
            wk = wa[k % 3]
            key = "wa%d" % (k % 3)
            for hh in range(4):
                cs = slice(hh * 3072, (hh + 1) * 3072)
                S.dma("pool", wk[:, cs], w_ada.ap()[k * 128:(k + 1) * 128, cs], w=[key + "_%d" % hh])
            for j in range(96):
                self.mm(ps[:, j:j + 1], wk[:, j * 128:(j + 1) * 128], condB[:, k:k + 1], True, True,
                        [key + "_%d" % (j // 24), "condB"], [("ps", 0)])
            if k == 0:
                self.DVE("tensor_copy", [("ps", 0)], ["modT"], out=self.modT[:], in_=ps[:, 0:96])
            else:
                self.DVE("tensor_tensor", [("ps", 0), "modT"], ["modT"], out=self.modT[:], in0=self.modT[:],
                         in1=ps[:, 0:96], op=ALU.add)
        self.DVE("tensor_tensor", ["modT", "bT"], ["modT"], out=self.modT[:], in0=self.modT[:], in1=bT[:], op=ALU.add)
        self.DVE("scalar_tensor_tensor", ["modT", "g1T"], ["A1"], out=self.A1[:], in0=self.modT[:, 16:32],
                 scalar=1.0, in1=g1T[:], op0=ALU.add, op1=ALU.mult)
        self.DVE("scalar_tensor_tensor", ["modT", "g2T"], ["A2"], out=self.A2[:], in0=self.modT[:, 64:80],
                 scalar=1.0, in1=g2T[:], op0=ALU.add, op1=ALU.mult)
        if "mod" in self.dbg:
            self.dump("mod", self.modT, [128, 96], F32, ["modT"])
        self.release(m)

    def phase1(self):
        S = self.S
        x_in = self.din("x_loc", [NLOC, D])
        self.x_in = x_in
        self.hT_d = self.dscratch("hT_pfx", [KC, 128, NOWN], BF16)
        self.m_hT = self.mark()
        self.hT = self.sb("hT", [128, KC, NOWN], BF16)
        m = self.mark()
        xt = [self.sb("xt%d" % i, [128, D], F32) for i in range(2)]
        xn = [self.sb("xn%d" % i, [128, D], BF16) for i in range(2)]
        junk = self.sb("junk", [128, D], BF16)
        st = self.sb("st", [128, 3 * 32], F32)
        hg = [self.sb("hg%d" % i, [128, KC, 512], BF16) for i in range(2)]
        def stage1(tt):
            b = tt % 2
            S.dma("sp", xt[b][:], x_in.ap()[tt * 128:(tt + 1) * 128, :], w=["xt%d" % b])
            self.ACT(junk[:], xt[b][:], AF.Square, ["xt%d" % b], ["junk", ("st", tt)], accum_out=st[:, tt:tt + 1])
            self.ACT(st[:, 32 + tt:33 + tt], st[:, tt:tt + 1], AF.Sqrt, [("st", tt)], [("st", tt)],
                     scale=1.0 / D, bias=EPS)
            self.DVE("reciprocal", [("st", tt)], [("st", tt)], out=st[:, 64 + tt:65 + tt], in_=st[:, 32 + tt:33 + tt])
            self.DVE("tensor_scalar", ["xt%d" % b, ("st", tt)], ["xn%d" % b], out=xn[b][:], in0=xt[b][:],
                     scalar1=st[:, 64 + tt:65 + tt], scalar2=None, op0=ALU.mult)

        def stage2(tt):
            g, i = tt // 4, tt % 4
            b = tt % 2
            for half in range(2):
                pst = self.psb(half, BF16)
                for kk in range(8):
                    k = half * 8 + kk
                    self.tr(pst[:, kk * 128:(kk + 1) * 128], xn[b][:, k * 128:(k + 1) * 128], self.ident_b[:],
                            ["xn%d" % b, "ident_b"], [("ps", half)])
                for kk in range(8):
                    k = half * 8 + kk
                    if g < 4:
                        dst = hg[g % 2][:, k, i * 128:(i + 1) * 128]
                        wkey = [("hg", g % 2, i)]
                    else:
                        dst = self.hT[:, k, (g - 4) * 512 + i * 128:(g - 4) * 512 + (i + 1) * 128]
                        wkey = [("hT", g - 4)]
                    src = pst[:, kk * 128:(kk + 1) * 128]
                    if half == 0:
                        self.DVE("tensor_scalar", [("ps", half), "A1", "modT"], wkey, out=dst, in0=src,
                                 scalar1=self.A1[:, k:k + 1], scalar2=self.modT[:, k:k + 1], op0=ALU.mult, op1=ALU.add)
                    else:
                        self.ACT(dst, src, AF.Identity, [("ps", half), "A1", "modT"], wkey,
                                 scale=self.A1[:, k:k + 1], bias=self.modT[:, k:k + 1])
            if g < 4 and i == 3:
                S.dma("sp", self.hT_d.ap().rearrange("k p t -> p k t")[:, :, g * 512:(g + 1) * 512], hg[g % 2][:],
                      r=[("hg", g % 2, ii) for ii in range(4)], w=[("hTd", g)])

        stage1(0)
        for tt in range(32):
            if tt + 1 < 32:
                stage1(tt + 1)
            stage2(tt)
        if "hT" in self.dbg:
            self.dump("hT", self.hT, [128, KC, NOWN], BF16, [("hT", q) for q in range(4)])
        self.release(m)

    def load_w(self, eng, dst, col0, ncols, wkey):
        src = self.w_in.ap().rearrange("(k p) c -> p k c", p=128)[:, :, col0:col0 + ncols]
        self.S.dma("pool", dst, src, w=[wkey])

    def hchunk(self, tc):
        if tc >= 4:
            base = (tc - 4) * 512
            return (lambda k, lo=0, hi=512, st=1: self.hT[:, k, base + lo:base + hi:st]), [("hT", tc - 4)]
        b = self.pf_i % 2
        self.pf_i += 1
        buf = self.pfbuf[b]
        key = ("pfbuf", b)
        self.S.dma("sp", buf[:], self.hT_d.ap().rearrange("k p t -> p k t")[:, :, tc * 512:(tc + 1) * 512],
                   r=[("hTd", tc)], w=[key])
        return (lambda k, lo=0, hi=512, st=1: buf[:, k, lo:hi:st]), [key]

    def norm_fm(self, ps, pskey, dst, dkeys, gain_ap, gkey, n):
        kf, sq, rs = self.nb_kf, self.nb_sq, self.nb_rs
        self.ACT(kf[:, :n], ps, AF.Copy, [pskey], ["nb_kf"])
        self.ACT(sq[:, :n], ps, AF.Square, [pskey], ["nb_sq"])
        p7 = self.psb(7)
        self.mm(p7[:, :n], self.ones_b[:], sq[:, :n], True, True, ["ones_b", "nb_sq"], [("ps", 7)])
        self.ACT(rs[:, :n], p7[:, :n], AF.Sqrt, [("ps", 7)], ["nb_rs"], scale=1.0 / DH, bias=EPS)
        self.DVE("reciprocal", ["nb_rs"], ["nb_rs"], out=rs[:, :n], in_=rs[:, :n])
        self.DVE("scalar_tensor_tensor", ["nb_kf", "nb_rs", gkey], dkeys, out=dst, in0=kf[:, :n], scalar=gain_ap,
                 in1=rs[:, :n], op0=ALU.mult, op1=ALU.mult)

    def attn(self, tiles, q_rhs, qkeys, nq, ob, zb, imp=None):
        O = self.psb(ob)[:, :nq]
        Z = self.psb(zb)[:, :nq]
        n = len(tiles)

        def emit_S(i):
            t = tiles[i]
            sb_ = i % 2
            Sps = self.psb(sb_)[:, :nq]
            adds = t.get("adds", [])
            self.mm(Sps, t["kT"], q_rhs, True, len(adds) == 0, t["kkeys"] + qkeys, [("ps", sb_)])
            for ai, (l, r_, ks) in enumerate(adds):
                self.mm(Sps, l, r_, False, ai == len(adds) - 1, ks, [("ps", sb_)])

        emit_S(0)
        for i in range(n):
            if i + 1 < n:
                emit_S(i + 1)
            t = tiles[i]
            pb = self.P_i % 3
            self.P_i += 1
            P = self.Pbuf[pb][:, :nq]
            self.ACT(P, self.psb(i % 2)[:, :nq], AF.Exp, [("ps", i % 2), t["bkey"]], [("P", pb)], bias=t["bias"])
            self.mm(O, t["v"], P, i == 0, i == n - 1, t["vkeys"] + [("P", pb)], [("ps", ob)])
            self.mm(Z, self.ones_b[:], P, i == 0, i == n - 1, ["ones_b", ("P", pb)], [("ps", zb)])
            if imp is not None:
                lfn, ib = imp
                self.mm(self.psb(ib)[0:64, :nq], lfn(i), P, i == 0, i == n - 1, ["ovl", ("P", pb)], [("ps", ib)])

    def phase2(self):
        S = self.S
        self.w_in = self.din("w_in", [D, IN_COLS])
        gq = self.din("nsa_q_gain_fm", [128, 1])
        gk = self.din("nsa_k_gain_fm", [128, 3])
        posT_in = self.din("cmp_posT", [128, 2, 32])
        w1_in = self.din("cmp_w1", [2, 4096, 128])
        w2_in = self.din("cmp_w2", [2, 128, 128])
        strips_in = self.din("nsa_strips", [8, 128, 8064])
        ovl_in = self.din("ovl", [128, 2, 64])
        eall_in = self.din("e_all", [64, 32, 128])
        selg_in = self.din("selg", [24, 24, 128])
        mmask_in = self.din("sel_mult", [128, 16, 64])
        amask_in = self.din("sel_add", [128, 16, 64])
        self.ynsa_d = self.dscratch("ynsa_d", [8, 128, NOWN], BF16)

        self.Pbuf = [self.sb("P%d" % i, [128, 512], BF16) for i in range(3)]
        self.P_i = 0
        self.nb_kf = self.sb("nb_kf", [128, 512], F32)
        self.nb_sq = self.sb("nb_sq", [128, 512], BF16)
        self.nb_rs = self.sb("nb_rs", [128, 512], F32)
        m = self.mark()
        gqs = self.sb("gqs", [128, 1], F32)
        gkt = self.sb("gkt", [128, 3], F32)
        S.dma("sp", gqs[:], gq.ap(), w=["gqs"])
        S.dma("sp", gkt[:], gk.ap(), w=["gkt"])
        self.DVE("tensor_scalar", ["gqs"], ["gqs"], out=gqs[:], in0=gqs[:], scalar1=SCALE, scalar2=None, op0=ALU.mult)
        posterm = self.sb("posterm", [128, 2], F32)
        gateT = self.sb("gateT", [24, NOWN], BF16)
        Wg = self.sb("Wg", [128, KC, 24], BF16)
        self.load_w("pool", Wg[:], OFF_GATE, 24, "Wg")
        for qc in range(4):
            p5 = self.psb(5)
            for k in range(KC):
                self.mm(p5[0:24, :], Wg[:, k, :], self.hT[:, k, qc * 512:(qc + 1) * 512], k == 0, k == KC - 1,
                        ["Wg", ("hT", qc)], [("ps", 5)])
            self.ACT(gateT[:, qc * 512:(qc + 1) * 512], p5[0:24, :], AF.Sigmoid, [("ps", 5)], [("gateT", qc)])

        for g in range(2):
            mg = self.mark()
            kselT = self.sb("kselT", [128, NLOC], BF16)
            vsel = self.sb("vsel", [128, 32, 128], BF16)
            kwinT = self.sb("kwinT", [128, 2560], BF16)
            vwin = self.sb("vwin", [128, 20, 128], BF16)
            qT = self.sb("qT", [128, 4, NOWN], BF16)
            kcT = self.sb("kcT", [128, 256], BF16)
            vc = self.sb("vc", [128, 2, 128], BF16)
            mA = self.mark()
            kcmpT = self.sb("kcmpT", [128, 2, NLOC], BF16)
            mB = self.mark()
            Wkv = self.sb("Wkv", [128, 6, KC, 128], BF16)
            self.pfbuf = [self.sb("pfbuf%d" % i, [128, KC, 512], BF16) for i in range(2)]
            self.pf_i = 0
            for i in range(6):
                self.load_w("pool", Wkv[:, i], OFF_KV + (i * 2 + g) * 128, 128, ("Wkv", i))
            for tc in range(8):
                self.pc(1)
                hf_, hkeys = self.hchunk(tc)
                for i in (0, 1, 2, 4):
                    if i == 4 and tc < 3:
                        continue
                    bk = 5 + (i // 2) % 2
                    p5 = self.psb(bk)
                    pk = ("ps", bk)
                    for k in range(KC):
                        self.mm(p5[:, :], Wkv[:, i, k, :], hf_(k), k == 0, k == KC - 1, [("Wkv", i)] + hkeys, [pk])
                    if i < 2:
                        self.ACT(kcmpT[:, i, tc * 512:(tc + 1) * 512], p5[:, :], AF.Copy, [pk], [("kcmpT", i)])
                    elif i == 2:
                        self.norm_fm(p5[:, :], pk, kselT[:, tc * 512:(tc + 1) * 512], [("kselT", tc)], gkt[:, 1:2], "gkt", 512)
                    else:
                        self.norm_fm(p5[:, :], pk, kwinT[:, (tc - 3) * 512:(tc - 2) * 512], [("kwinT", tc - 3)], gkt[:, 2:3], "gkt", 512)
                for ti in range(4):
                    tt = tc * 4 + ti
                    p6 = self.psb(6)
                    units = (3, 5) if tc >= 3 else (3,)
                    for ui, i in enumerate(units):
                        for k in range(KC):
                            self.mm(p6[:, ui * 128:(ui + 1) * 128], hf_(k, ti * 128, (ti + 1) * 128), Wkv[:, i, k, :],
                                    k == 0, k == KC - 1, [("Wkv", i)] + hkeys, [("ps", 6)])
                    self.DVE("tensor_copy", [("ps", 6)], [("vsel", tt)], out=vsel[:, tt, :], in_=p6[:, 0:128])
                    if tc >= 3:
                        self.DVE("tensor_copy", [("ps", 6)], [("vwin", tt - 12)], out=vwin[:, tt - 12, :], in_=p6[:, 128:256])
            self.release(mB)
            Wq = self.sb("Wq", [128, 4, KC, 128], BF16)
            for r in range(4):
                self.load_w("pool", Wq[:, r], (4 * g + r) * 128, 128, ("Wq", r))
            for tc in range(4, 8):
                hf_, hkeys = self.hchunk(tc)
                for r in range(4):
                    p5 = self.psb(5)
                    for k in range(KC):
                        self.mm(p5[:, :], Wq[:, r, k, :], hf_(k), k == 0, k == KC - 1, [("Wq", r)] + hkeys, [("ps", 5)])
                    self.norm_fm(p5[:, :], ("ps", 5), qT[:, r, (tc - 4) * 512:(tc - 3) * 512], [("qT", r, tc - 4)],
                                 gqs[:, 0:1], "gqs", 512)
            self.release(mB)
            posT = self.sb("posT", [128, 2, 32], BF16)
            W1 = self.sb("W1", [128, 2, 32, 128], BF16)
            W2 = self.sb("W2", [128, 2, 128], BF16)
            hid = self.sb("hid", [128, 2, 256], BF16)
            S.dma("pool", posT[:], posT_in.ap(), w=["posT"])
            for j in range(2):
                S.dma("pool", W1[:, j], w1_in.ap()[j].rearrange("(l d) o -> d l o", d=128), w=[("W1", j)])
                S.dma("pool", W2[:, j, :], w2_in.ap()[j], w=[("W2", j)])
            self.POOL("memset", [], [("hid", 0), ("hid", 1)], hid[:], 0.0)
            if g == 0:
                for j in range(2):
                    p5 = self.psb(5)
                    for l in range(32):
                        self.mm(p5[:, 0:1], W1[:, j, l, :], posT[:, j, l:l + 1], l == 0, l == 31, [("W1", j), "posT"], [("ps", 5)])
                    self.DVE("tensor_copy", [("ps", 5)], [("posterm", j)], out=posterm[:, j:j + 1], in_=p5[:, 0:1])
            for j in range(2):
                p5 = self.psb(5)
                for l in range(32):
                    self.mm(p5[:, 0:255], W1[:, j, l, :], kcmpT[:, j, l:l + 16 * 254 + 1:16], l == 0, l == 31,
                            [("W1", j), ("kcmpT", j)], [("ps", 5)])
                self.ACT(hid[:, j, 0:255], p5[:, 0:255], AF.Gelu_apprx_tanh, [("ps", 5), ("posterm", j)], [("hid", j)],
                         bias=posterm[:, j:j + 1])
            p5 = self.psb(5)
            self.mm(p5[:, 0:256], W2[:, 0, :], hid[:, 0, :], True, True, [("W2", 0), ("hid", 0)], [("ps", 5)])
            self.norm_fm(p5[:, 0:256], ("ps", 5), kcT[:, :], ["kcT"], gkt[:, 0:1], "gkt", 256)
            for ct in range(2):
                p6 = self.psb(6)
                self.mm(p6[:, 0:128], hid[:, 1, ct * 128:(ct + 1) * 128], W2[:, 1, :], True, True,
                        [("W2", 1), ("hid", 1)], [("ps", 6)])
                self.DVE("tensor_copy", [("ps", 6)], [("vc", ct)], out=vc[:, ct, :], in_=p6[:, 0:128])
            self.release(mA)

            ovl = self.sb("ovl", [128, 2, 64], BF16)
            eall = self.sb("eall", [64, 32, 128], BF16)
            selg = self.sb("selg", [24, 24, 128], BF16)
            mmask = self.sb("mmask", [128, 16, 64], F32)
            amask = self.sb("amask", [128, 16, 64], F32)
            S.dma("pool", ovl[:], ovl_in.ap(), w=["ovl"])
            S.dma("pool", eall[:], eall_in.ap(), w=["eall"])
            S.dma("pool", selg[:], selg_in.ap(), w=["selg"])
            S.dma("sp", mmask[:], mmask_in.ap(), w=["mmask"])
            S.dma("sp", amask[:], amask_in.ap(), w=["amask"])
            strip = self.sb("strip", [128, 8064], BF16)
            selbT = self.sb("selbT", [64, NOWN], BF16)
            ycmp = self.sb("ycmp", [128, 4, 512], F32)
            impacc = self.sb("impacc", [64, 512], F32)
            rz = self.sb("rz", [128, 512], F32)
            wgt = self.sb("wgt", [128, 512], F32)
            tmpf = self.sb("tmpf", [128, 512], F32)
            yacc = self.sb("yacc", [128, 512], F32)
            ybuf = [self.sb("ybuf%d" % i, [128, 512], BF16) for i in range(2)]
            sc = self.sb("sc", [128, 4, 64], F32)
            wk = self.sb("wk", [128, 64], F32)
            m8 = self.sb("m8", [128, 16], F32)
            selb = self.sb("selb", [128, 4, 64], BF16)
            yb_i = 0
            for qc in range(4):
                qb = NOWN + 512 * qc
                qsl = slice(qc * 512, (qc + 1) * 512)
                for r in range(4):
                    h = 4 * g + r
                    S.dma("pool", strip[:, 3968:8064], strips_in.ap()[h][:, 3968:8064], w=["strip_c"])
                    tiles = []
                    for ct in range(2):
                        c0 = qb - 2048 * ct
                        tiles.append(dict(kT=kcT[:, ct * 128:(ct + 1) * 128], kkeys=["kcT"], v=vc[:, ct, :], vkeys=[("vc", ct)],
                                          adds=[(self.ident_b[:], strip[:, 3968 + c0:3968 + c0 + 512], ["ident_b", "strip_c"])],
                                          bias=(self.pfxb[:, 0:1] if ct == 0 else self.zero1[:, 0:1]),
                                          bkey=("pfxb" if ct == 0 else "zero1")))
                    self.attn(tiles, qT[:, r, qsl], [("qT", r, qc)], 512, 2, 3, imp=(lambda i: ovl[:, i, :], 4))
                    O = self.psb(2); Z = self.psb(3); I = self.psb(4)
                    self.DVE("tensor_scalar", [("ps", 3)], ["rz"], out=rz[:], in0=Z[:, :], scalar1=1e-30, scalar2=None, op0=ALU.max)
                    self.DVE("reciprocal", ["rz"], ["rz"], out=rz[:], in_=rz[:])
                    self.DVE("tensor_tensor", [("ps", 2), "rz"], [("ycmp", r)], out=ycmp[:, r, :], in0=O[:, :], in1=rz[:], op=ALU.mult)
                    if r == 0:
                        self.DVE("tensor_tensor", [("ps", 4), "rz"], ["impacc"], out=impacc[:], in0=I[0:64, :], in1=rz[0:64, :], op=ALU.mult)
                    else:
                        self.DVE("tensor_tensor", [("ps", 4), "rz"], ["tmpf"], out=tmpf[0:64, :], in0=I[0:64, :], in1=rz[0:64, :], op=ALU.mult)
                        self.DVE("tensor_tensor", ["tmpf", "impacc"], ["impacc"], out=impacc[:], in0=impacc[:], in1=tmpf[0:64, :], op=ALU.add)
                p5 = self.psb(5)
                for i in range(4):
                    self.mm(p5[:, i * 64:(i + 1) * 64], impacc[:, i * 128:(i + 1) * 128], self.ident_f[0:64, 0:64], True, True,
                            ["impacc", "ident_f"], [("ps", 5)])
                scv = sc[:].rearrange("p a b -> p (a b)")
                self.DVE("tensor_tensor", [("ps", 5), "mmask"], ["sc"], out=scv, in0=p5[:, 0:256],
                         in1=mmask[:, qc * 4:(qc + 1) * 4, :].rearrange("p a b -> p (a b)"), op=ALU.mult)
                self.DVE("tensor_tensor", ["sc", "amask"], ["sc"], out=scv, in0=scv,
                         in1=amask[:, qc * 4:(qc + 1) * 4, :].rearrange("p a b -> p (a b)"), op=ALU.add)
                for i in range(4):
                    self.DVE("max", ["sc"], ["m8"], out=m8[:, 0:8], in_=sc[:, i, :])
                    self.DVE("match_replace", ["sc", "m8"], ["wk"], out=wk[:], in_to_replace=m8[:, 0:8], in_values=sc[:, i, :], imm_value=-1e30)
                    self.DVE("max", ["wk"], ["m8"], out=m8[:, 8:16], in_=wk[:])
                    self.DVE("tensor_scalar", ["sc", "m8"], [("selb", i)], out=selb[:, i, :], in0=sc[:, i, :], scalar1=m8[:, 15:16],
                             scalar2=NEGB, op0=ALU.is_lt, op1=ALU.mult)
                    p6 = self.psb(6, BF16)
                    self.tr(p6[0:64, 0:128], selb[:, i, :], self.ident_b[:], [("selb", i), "ident_b"], [("ps", 6)])
                    self.ACT(selbT[:, qc * 512 + i * 128:qc * 512 + (i + 1) * 128], p6[0:64, 0:128], AF.Copy, [("ps", 6)], [("selbT", qc)])
                for r in range(4):
                    h = 4 * g + r
                    self.pc(1)
                    S.dma("pool", strip[:, 0:3968], strips_in.ap()[h][:, 0:3968], w=["strip_sw"])
                    G = self.psb(4)
                    self.mm(G[:, :], selg[:, 3 * h + 0, :], gateT[:, qsl], True, True, ["selg", ("gateT", qc)], [("ps", 4)])
                    self.DVE("tensor_tensor", [("ps", 4), ("ycmp", r)], ["yacc"], out=yacc[:], in0=ycmp[:, r, :], in1=G[:, :], op=ALU.mult)
                    for br in (1, 2):
                        tiles = []
                        if br == 1:
                            kts = range(0, 16 + 4 * qc + 4)
                        else:
                            kts = range(16 + 4 * qc - 4, 16 + 4 * qc + 4)
                        for kt in kts:
                            dlt = qb - 128 * kt
                            bias = self.pfxb[:, 0:1] if kt < 16 else self.zero1[:, 0:1]
                            bkey = "pfxb" if kt < 16 else "zero1"
                            if br == 1:
                                c0 = min(dlt + 384, 2048)
                                adds = [(eall[:, kt, :], selbT[:, qsl], ["eall", ("selbT", qc)]),
                                        (self.ident_b[:], strip[:, c0:c0 + 512], ["ident_b", "strip_sw"])]
                                tiles.append(dict(kT=kselT[:, kt * 128:(kt + 1) * 128], kkeys=[("kselT", kt // 4)],
                                                  v=vsel[:, kt, :], vkeys=[("vsel", kt)], adds=adds, bias=bias, bkey=bkey))
                            else:
                                c0 = 2560 + dlt + 384
                                adds = [(self.ident_b[:], strip[:, c0:c0 + 512], ["ident_b", "strip_sw"])]
                                tiles.append(dict(kT=kwinT[:, (kt - 12) * 128:(kt - 11) * 128], kkeys=[("kwinT", (kt - 12) // 4)],
                                                  v=vwin[:, kt - 12, :], vkeys=[("vwin", kt - 12)], adds=adds, bias=bias, bkey=bkey))
                        self.attn(tiles, qT[:, r, qsl], [("qT", r, qc)], 512, 2, 3)
                        O = self.psb(2); Z = self.psb(3)
                        self.mm(G[:, :], selg[:, 3 * h + br, :], gateT[:, qsl], True, True, ["selg", ("gateT", qc)], [("ps", 4)])
                        self.DVE("reciprocal", [("ps", 3)], ["rz"], out=rz[:], in_=Z[:, :])
                        self.DVE("tensor_tensor", [("ps", 4), "rz"], ["wgt"], out=wgt[:], in0=rz[:], in1=G[:, :], op=ALU.mult)
                        self.DVE("tensor_tensor", [("ps", 2), "wgt"], ["tmpf"], out=tmpf[:], in0=wgt[:], in1=O[:, :], op=ALU.mult)
                        if br == 1:
                            self.DVE("tensor_tensor", ["tmpf", "yacc"], ["yacc"], out=yacc[:], in0=yacc[:], in1=tmpf[:], op=ALU.add)
                        else:
                            yb = yb_i % 2
                            yb_i += 1
                            self.DVE("tensor_tensor", ["tmpf", "yacc"], [("ybuf", yb)], out=ybuf[yb][:], in0=yacc[:], in1=tmpf[:], op=ALU.add)
                            S.dma("sp", self.ynsa_d.ap()[h][:, qsl], ybuf[yb][:], r=[("ybuf", yb)], w=[("ynsa_d", h, qc)])
            self.release(mg)
        if "ynsa" in self.dbg:
            o = self.dout("dbg_ynsa", [8, 128, NOWN], BF16)
            S.dma("sp", o.ap(), self.ynsa_d.ap())
        self.release(m)

    def phase3(self):
        S = self.S
        gq = self.din("dil_q_gain_fm", [128, 1])
        gk = self.din("dil_k_gain_fm", [128, 1])
        dstr_in = self.din("dil_strips", [12, 128, 1024])
        self.ydil_d = self.dscratch("ydil_d", [4, 128, NOWN], BF16)
        m = self.mark()
        hTp = self.sb("hTp", [128, KC, NOWN], BF16)
        for c in range(4):
            S.dma("sp", hTp[:, :, c * 512:(c + 1) * 512], self.hT_d.ap().rearrange("k p t -> p k t")[:, :, c * 512:(c + 1) * 512],
                  w=[("hTp", c)])
        gqs = self.sb("dgqs", [128, 1], F32)
        gks = self.sb("dgks", [128, 1], F32)
        S.dma("sp", gqs[:], gq.ap(), w=["dgqs"])
        S.dma("sp", gks[:], gk.ap(), w=["dgks"])
        self.DVE("tensor_scalar", ["dgqs"], ["dgqs"], out=gqs[:], in0=gqs[:], scalar1=SCALE, scalar2=None, op0=ALU.mult)
        dacc = self.sb("dacc", [128, NOWN], F32)
        zacc = self.sb("zacc", [128, NOWN], F32)
        ybuf = self.sb("dybuf", [128, NOWN], BF16)
        W3 = self.sb("W3", [128, 3, KC, 128], BF16)
        qT = self.sb("dqT", [128, NOWN], BF16)
        kT = self.sb("dkT", [128, NLOC], BF16)
        vt = self.sb("dvt", [128, 32, 128], BF16)
        strip = self.sb("dstrip", [128, 1024], BF16)
        for p in range(4):
            for gi, (win, dil) in enumerate(DIL_CFG):
                self.pc(1)
                hd = gi * 4 + p
                for i in range(3):
                    self.load_w("pool", W3[:, i], OFF_DIL + (i * 12 + hd) * 128, 128, ("W3", i))
                S.dma("pool", strip[:], dstr_in.ap()[hd], w=["dstrip"])
                for tc in range(4):
                    p5 = self.psb(5)
                    for k in range(KC):
                        self.mm(p5[:, :], W3[:, 0, k, :], self.hT[:, k, tc * 512:(tc + 1) * 512], k == 0, k == KC - 1,
                                [("W3", 0), ("hT", tc)], [("ps", 5)])
                    self.norm_fm(p5[:, :], ("ps", 5), qT[:, tc * 512:(tc + 1) * 512], [("dqT", tc)], gqs[:, 0:1], "dgqs", 512)
                    p6 = self.psb(6)
                    for k in range(KC):
                        self.mm(p6[:, :], W3[:, 1, k, :], self.hT[:, k, tc * 512:(tc + 1) * 512], k == 0, k == KC - 1,
                                [("W3", 1), ("hT", tc)], [("ps", 6)])
                    self.norm_fm(p6[:, :], ("ps", 6), kT[:, NOWN + tc * 512:NOWN + (tc + 1) * 512], ["dkT"], gks[:, 0:1], "dgks", 512)
                pl = 128 * dil
                pieces = []
                t0 = NOWN - pl
                while t0 < NOWN:
                    n = min(512, NOWN - t0)
                    pieces.append((t0, n))
                    t0 += n
                for (t0, n) in pieces:
                    p6 = self.psb(6)
                    for k in range(KC):
                        self.mm(p6[:, :n], W3[:, 1, k, :], hTp[:, k, t0:t0 + n], k == 0, k == KC - 1,
                                [("W3", 1), ("hTp", t0 // 512)], [("ps", 6)])
                    self.norm_fm(p6[:, :n], ("ps", 6), kT[:, t0:t0 + n], ["dkT"], gks[:, 0:1], "dgks", n)
                ntile = 16 // dil + 1
                for r in range(dil):
                    for mt in range(ntile):
                        p6 = self.psb(6)
                        if mt == 0:
                            st = NOWN - pl + r
                            src = lambda k: hTp[:, k, st:st + dil * 127 + 1:dil]
                            keys = [("hTp", c) for c in range((NOWN - pl) // 512, 4)]
                        else:
                            st = r + dil * 128 * (mt - 1)
                            src = lambda k: self.hT[:, k, st:st + dil * 127 + 1:dil]
                            keys = [("hT", c) for c in range(st // 512, (st + dil * 127) // 512 + 1)]
                        for k in range(KC):
                            self.mm(p6[:, 0:128], src(k), W3[:, 2, k, :], k == 0, k == KC - 1, [("W3", 2)] + keys, [("ps", 6)])
                        self.DVE("tensor_copy", [("ps", 6)], [("dvt", r * ntile + mt)], out=vt[:, r * ntile + mt, :], in_=p6[:, 0:128])
                nq = min(512, NOWN // dil)
                nch = (NOWN // dil) // nq
                for r in range(dil):
                    for ci in range(nch):
                        qs = r + dil * ci * nq
                        q_rhs = qT[:, qs:qs + dil * (nq - 1) + 1:dil]
                        tiles = []
                        for mt in range(ci * nq // 128, ci * nq // 128 + nq // 128 + 1):
                            delta = (128 + ci * nq) - 128 * mt
                            c0 = delta + 384
                            ks = NOWN - pl + r + dil * 128 * mt
                            tiles.append(dict(kT=kT[:, ks:ks + dil * 127 + 1:dil], kkeys=["dkT"], v=vt[:, r * ntile + mt, :],
                                              vkeys=[("dvt", r * ntile + mt)],
                                              adds=[(self.ident_b[:], strip[:, c0:c0 + nq], ["ident_b", "dstrip"])],
                                              bias=(self.pfxb[:, 0:1] if mt == 0 else self.zero1[:, 0:1]),
                                              bkey=("pfxb" if mt == 0 else "zero1")))
                        self.attn(tiles, q_rhs, [("dqT", c) for c in range(4)], nq, 2, 3)
                        O = self.psb(2)[:, :nq]; Z = self.psb(3)[:, :nq]
                        dsl = slice(qs, qs + dil * (nq - 1) + 1, dil)
                        if gi == 0:
                            self.DVE("tensor_copy", [("ps", 2)], ["dacc"], out=dacc[:, dsl], in_=O)
                            self.DVE("tensor_copy", [("ps", 3)], ["zacc"], out=zacc[:, dsl], in_=Z)
                        else:
                            self.DVE("tensor_tensor", [("ps", 2), "dacc"], ["dacc"], out=dacc[:, dsl], in0=dacc[:, dsl], in1=O, op=ALU.add)
                            self.DVE("tensor_tensor", [("ps", 3), "zacc"], ["zacc"], out=zacc[:, dsl], in0=zacc[:, dsl], in1=Z, op=ALU.add)
            self.DVE("reciprocal", ["zacc"], ["zacc"], out=zacc[:], in_=zacc[:])
            self.DVE("tensor_tensor", ["zacc", "dacc"], ["dybuf"], out=ybuf[:], in0=dacc[:], in1=zacc[:], op=ALU.mult)
            S.dma("sp", self.ydil_d.ap()[p], ybuf[:], r=["dybuf"], w=[("ydil_d", p)])
        if "ydil" in self.dbg:
            o = self.dout("dbg_ydil", [4, 128, NOWN], BF16)
            S.dma("sp", o.ap(), self.ydil_d.ap(), r=[("ydil_d", p) for p in range(4)])
        self.release(m)

    def row_bcast(self, dst, col0):
        diag = self.sb("diag", [128, 128], F32)
        for k in range(KC):
            self.DVE("tensor_scalar", ["ident_f", "modT"], ["diag"], out=diag[:], in0=self.ident_f[:],
                     scalar1=self.modT[:, col0 + k:col0 + k + 1], scalar2=None, op0=ALU.mult)
            p5 = self.psb(5)
            self.mm(p5[:, 0:128], self.ones_f[:], diag[:], True, True, ["ones_f", "diag"], [("ps", 5)])
            self.ACT(dst[:, k * 128:(k + 1) * 128], p5[:, 0:128], AF.Copy, [("ps", 5)], ["rowb"])

    def phase4(self):
        S = self.S
        wbn_in = self.din("w_br_nsa", [1024, D])
        wbd_in = self.din("w_br_dil", [512, D])
        wout_in = self.din("w_out", [D, D])
        self.y = self.dout("y", [NOWN, D])
        self.h2T_d = self.dscratch("h2T_d", [KC, 128, NOWN], BF16)
        m = self.mark()
        g1b = self.sb("g1b", [128, D], F32)
        self.row_bcast(g1b, 32)
        yn = self.sb("yn", [128, 8, 512], BF16)
        yd = self.sb("yd", [128, 4, 512], BF16)
        Wbn2 = [self.sb("Wbn%d" % i, [128, 8, 128], BF16) for i in range(2)]
        Wbd2 = [self.sb("Wbd%d" % i, [128, 4, 128], BF16) for i in range(2)]
        Wm2 = [self.sb("Wm%d" % i, [128, 2, KC, 128], BF16) for i in range(2)]
        mg = self.sb("mg", [128, KC, 512], BF16)
        Wo = self.sb("Wo", [128, KC, 512], BF16)
        xt = [self.sb("x4_%d" % i, [128, D], F32) for i in range(4)]
        xn = self.sb("xn4", [128, D], BF16)
        h2g = self.sb("h2g", [128, KC, 512], BF16)
        s1 = self.sb("s1", [128, 512], F32)
        s2 = self.sb("s2", [128, 512], F32)
        t1 = self.sb("t1", [128, 512], F32)
        st = self.sb("st4", [128, 3], F32)
        for qc in range(4):
            qsl = slice(qc * 512, (qc + 1) * 512)
            S.dma("sp", yn[:], self.ynsa_d.ap().rearrange("h p t -> p h t")[:, :, qsl], w=["yn"])
            S.dma("sp", yd[:], self.ydil_d.ap().rearrange("h p t -> p h t")[:, :, qsl], w=["yd"])
            for ti in range(4):
                tt = qc * 4 + ti
                S.dma("sp", xt[ti][:], self.x_in.ap()[NOWN + tt * 128:NOWN + (tt + 1) * 128, :], w=[("x4", ti)])
            for fc in range(KC):
                fsl = slice(fc * 128, (fc + 1) * 128)
                wb = fc % 2
                Wbn, Wbd, Wm = Wbn2[wb], Wbd2[wb], Wm2[wb]
                S.dma("pool", Wbn[:], wbn_in.ap().rearrange("(h d) f -> d h f", d=128)[:, :, fsl], w=[("Wbn", wb)])
                S.dma("pool", Wbd[:], wbd_in.ap().rearrange("(h d) f -> d h f", d=128)[:, :, fsl], w=[("Wbd", wb)])
                self.load_w("pool", Wm[:, 0], OFF_MERGE + fc * 128, 128, ("Wm", 0, wb))
                self.load_w("pool", Wm[:, 1], OFF_MERGE + D + fc * 128, 128, ("Wm", 1, wb))
                A = self.psb(2); Bm = self.psb(3); G1 = self.psb(0); G2 = self.psb(1)
                for h in range(8):
                    self.mm(A[:, :], Wbn[:, h, :], yn[:, h, :], h == 0, h == 7, [("Wbn", wb), "yn"], [("ps", 2)])
                for p in range(4):
                    self.mm(Bm[:, :], Wbd[:, p, :], yd[:, p, :], p == 0, p == 3, [("Wbd", wb), "yd"], [("ps", 3)])
                for k in range(KC):
                    self.mm(G1[:, :], Wm[:, 0, k, :], self.hT[:, k, qsl], k == 0, k == KC - 1, [("Wm", 0, wb), ("hT", qc)], [("ps", 0)])
                for k in range(KC):
                    self.mm(G2[:, :], Wm[:, 1, k, :], self.hT[:, k, qsl], k == 0, k == KC - 1, [("Wm", 1, wb), ("hT", qc)], [("ps", 1)])
                self.ACT(s1[:], G1[:, :], AF.Sigmoid, [("ps", 0)], ["s1"])
                self.ACT(s2[:], G2[:, :], AF.Sigmoid, [("ps", 1)], ["s2"])
                self.DVE("tensor_tensor", [("ps", 2), "s1"], ["s1"], out=s1[:], in0=s1[:], in1=A[:, :], op=ALU.mult)
                self.DVE("tensor_tensor", [("ps", 3), "s2"], ["s2"], out=s2[:], in0=s2[:], in1=Bm[:, :], op=ALU.mult)
                self.DVE("tensor_tensor", ["s1", "s2"], [("mg", fc)], out=mg[:, fc, :], in0=s1[:], in1=s2[:], op=ALU.add)
            if "merged" in self.dbg:
                if qc == 0:
                    self.dbg_merged = self.dout("dbg_merged", [128, KC, NOWN], BF16)
                S.dma("sp", self.dbg_merged.ap()[:, :, qsl], mg[:], r=[("mg", fc) for fc in range(KC)])
            for fo in range(4):
                fos = slice(fo * 512, (fo + 1) * 512)
                S.dma("pool", Wo[:], wout_in.ap().rearrange("(k p) f -> p k f", p=128)[:, :, fos], w=["Wo"])
                for ti in range(4):
                    po = self.psb(4 + ti % 2)
                    for fc in range(KC):
                        self.mm(po[:, :], mg[:, fc, ti * 128:(ti + 1) * 128], Wo[:, fc, :], fc == 0, fc == KC - 1,
                                [("mg", fc), "Wo"], [("ps", 4 + ti % 2)])
                    self.DVE("tensor_tensor", [("ps", 4 + ti % 2), "rowb"], ["t1"], out=t1[:], in0=g1b[:, fos], in1=po[:, :], op=ALU.mult)
                    self.DVE("tensor_tensor", ["t1", ("x4", ti)], [("x4", ti)], out=xt[ti][:, fos], in0=xt[ti][:, fos], in1=t1[:], op=ALU.add)
            for ti in range(4):
                tt = qc * 4 + ti
                S.dma("sp", self.y.ap()[tt * 128:(tt + 1) * 128, :], xt[ti][:], r=[("x4", ti)], w=[("y", tt)])
                self.ACT(xn[:], xt[ti][:], AF.Square, [("x4", ti)], ["xn4", "st4"], accum_out=st[:, 0:1])
                self.ACT(st[:, 1:2], st[:, 0:1], AF.Sqrt, ["st4"], ["st4"], scale=1.0 / D, bias=EPS)
                self.DVE("reciprocal", ["st4"], ["st4"], out=st[:, 2:3], in_=st[:, 1:2])
                self.DVE("tensor_scalar", [("x4", ti), "st4"], ["xn4"], out=xn[:], in0=xt[ti][:], scalar1=st[:, 2:3], scalar2=None, op0=ALU.mult)
                for half in range(2):
                    pst = self.psb(6 + half, BF16)
                    for kk in range(8):
                        k = half * 8 + kk
                        self.tr(pst[:, kk * 128:(kk + 1) * 128], xn[:, k * 128:(k + 1) * 128], self.ident_b[:],
                                ["xn4", "ident_b"], [("ps", 6 + half)])
                    for kk in range(8):
                        k = half * 8 + kk
                        dst = h2g[:, k, ti * 128:(ti + 1) * 128]
                        src = pst[:, kk * 128:(kk + 1) * 128]
                        if kk % 2 == 0:
                            self.DVE("tensor_scalar", [("ps", 6 + half), "A2", "modT"], [("h2g", ti)], out=dst, in0=src,
                                     scalar1=self.A2[:, k:k + 1], scalar2=self.modT[:, 48 + k:49 + k], op0=ALU.mult, op1=ALU.add)
                        else:
                            self.ACT(dst, src, AF.Identity, [("ps", 6 + half), "A2", "modT"], [("h2g", ti)],
                                     scale=self.A2[:, k:k + 1], bias=self.modT[:, 48 + k:49 + k])
            S.dma("sp", self.h2T_d.ap().rearrange("k p t -> p k t")[:, :, qsl], h2g[:], r=[("h2g", ti) for ti in range(4)],
                  w=[("h2Td", qc)])
        if "h2T" in self.dbg:
            o = self.dout("dbg_h2T", [KC, 128, NOWN], BF16)
            S.dma("sp", o.ap(), self.h2T_d.ap(), r=[("h2Td", q) for q in range(4)])
        self.release(m)

    def peer_precast(self):
        S = self.S
        self.uT_in = self.din("peer_uT", [D, 16384])
        self.v_in = self.din("peer_v", [16384, D])
        self.uT_b = self.dscratch("uT_b", [64, 128, KC, 256], BF16)
        self.v_b = self.dscratch("v_b", [16384, D], BF16)
        self.pcq = []
        for i in range(32):
            k_, p0 = i // 2, (i % 2) * 64
            self.pcq.append((self.uT_b.ap()[:, p0:p0 + 64, k_, :].rearrange("c p e -> p c e"),
                             self.uT_in.ap()[i * 64:(i + 1) * 64, :].rearrange("p (c e) -> p c e", e=256), ("uTb", i // 2)))
        for i in range(32):
            self.pcq.append((self.v_b.ap()[i * 512:(i + 1) * 512, :], self.v_in.ap()[i * 512:(i + 1) * 512, :], ("vb", i // 2)))
        self.pcq.reverse()

    def pc(self, n=1):
        for _ in range(n):
            if getattr(self, "pcq", None):
                o, i_, key = self.pcq.pop()
                self.S.dma("pool", o, i_, w=[(key, len(self.pcq))])

    def phase5(self):
        S = self.S
        wq_in = self.din("peer_w_q", [D, D])
        gain_in = self.din("peer_q_gain_row", [1, D])
        skT_in = self.din("peer_skT", [128, 16, 128])
        self.sc_d = self.dscratch("sc_d", [NOWN, D], F32)
        self.pc(64)
        self.release(self.m_hT)
        m0 = self.mark()
        g2b = self.sb("g2b", [128, D], F32)
        self.row_bcast(g2b, 80)
        m = self.mark()
        Wq = self.sb("pWq", [128, KC, D], BF16)
        for k in range(KC):
            S.dma("pool", Wq[:, k, :], wq_in.ap()[k * 128:(k + 1) * 128, :], w=[("pWq", k)])
        gqb = self.sb("gqb", [128, D], F32)
        S.dma("sp", gqb[:], gain_in.ap().partition_broadcast(128), w=["gqb"])
        skT = self.sb("skT", [128, 16, 128], BF16)
        S.dma("pool", skT[:], skT_in.ap(), w=["skT"])
        h2t = [self.sb("h2t%d" % i, [128, KC, 128], BF16) for i in range(2)]
        sq = self.sb("psq", [128, 512], F32)
        ss = self.sb("pss", [128, 16], F32)
        qn = self.sb("pqn", [128, D], BF16)
        tq = self.sb("ptq", [128, 512], F32)
        qnT = self.sb("pqnT", [128, 16, 128], BF16)
        sct = [self.sb("psct%d" % i, [128, D], F32) for i in range(2)]
        for tt in range(16):
            b = tt % 2
            S.dma("sp", h2t[b][:], self.h2T_d.ap().rearrange("k p t -> p k t")[:, :, tt * 128:(tt + 1) * 128], w=[("h2t", b)])
            for fo in range(4):
                pq = self.psb(fo)
                for k in range(KC):
                    self.mm(pq[:, :], h2t[b][:, k, :], Wq[:, k, fo * 512:(fo + 1) * 512], k == 0, k == KC - 1,
                            [("h2t", b), ("pWq", k)], [("ps", fo)])
                self.ACT(sq[:], pq[:, :], AF.Square, [("ps", fo)], ["psq"])
                self.DVE("reduce_sum", ["psq"], [("pss", fo)], out=ss[:, fo * 4:(fo + 1) * 4],
                         in_=sq[:].rearrange("p (a b) -> p a b", b=128), axis=AX.X)
            self.ACT(ss[:], ss[:], AF.Sqrt, [("pss", f) for f in range(4)], [("pss", f) for f in range(4)], scale=1.0 / 128, bias=EPS)
            self.DVE("reciprocal", [("pss", f) for f in range(4)], [("pss", f) for f in range(4)], out=ss[:], in_=ss[:])
            for fo in range(4):
                pq = self.psb(fo)
                self.DVE("tensor_tensor", [("ps", fo), ("pss", 0)], ["ptq"], out=tq[:].rearrange("p (a b) -> p a b", b=128),
                         in0=pq[:, :].rearrange("p (a b) -> p a b", b=128),
                         in1=ss[:, fo * 4:(fo + 1) * 4].unsqueeze(2).to_broadcast([128, 4, 128]), op=ALU.mult)
                self.DVE("tensor_tensor", ["ptq", "gqb"], [("pqn", fo)], out=qn[:, fo * 512:(fo + 1) * 512], in0=tq[:],
                         in1=gqb[:, fo * 512:(fo + 1) * 512], op=ALU.mult)
            for half in range(2):
                pst = self.psb(4 + half, BF16)
                for kk in range(8):
                    hp = half * 8 + kk
                    self.tr(pst[:, kk * 128:(kk + 1) * 128], qn[:, hp * 128:(hp + 1) * 128], self.ident_b[:],
                            [("pqn", hp // 4), "ident_b"], [("ps", 4 + half)])
                self.ACT(qnT[:, half * 8:(half + 1) * 8, :].rearrange("p a b -> p (a b)"), pst[:, :], AF.Copy, [("ps", 4 + half)], [("pqnT", half)])
            for fo in range(4):
                pq = self.psb(fo)
                for i in range(4):
                    hp = fo * 4 + i
                    self.mm(pq[:, i * 128:(i + 1) * 128], qnT[:, hp, :], skT[:, hp, :], True, True,
                            [("pqnT", hp // 8), "skT"], [("ps", fo)])
                self.ACT(sct[b][:, fo * 512:(fo + 1) * 512], pq[:, :], AF.Copy, [("ps", fo)], [("psct", b)])
            S.dma("sp", self.sc_d.ap()[tt * 128:(tt + 1) * 128, :], sct[b][:], r=[("psct", b)], w=[("scd", tt)])
        self.release(m)

        EC = 256
        NB = 4
        NEC = 16384 // EC
        coef = [self.sb("coef%d" % i, [128, 16384], BF16) for i in range(2)]
        Ef = [self.sb("Ef%d" % i, [128, 8, 128], F32) for i in range(2)]
        C = [self.sb("C%d" % i, [128, 1024], BF16) for i in range(2)]
        ub = [self.sb("ub%d" % i, [128, KC, EC], BF16) for i in range(NB)]
        vb = [self.sb("vb%d" % i, [128, EC // 128, D], BF16) for i in range(NB)]
        h2t = [self.sb("h2tc%d" % i, [128, KC, 128], BF16) for i in range(2)]
        sct = self.sb("sctc", [128, D], F32)
        e12 = [self.sb("e12_%d" % i, [128, 8, 2, 128], F32) for i in range(2)]
        x1c = self.sb("x1c", [128, 512], F32)
        Gb = [self.sb("Gb%d" % i, [128, EC], BF16) for i in range(2)]
        Wb = [self.sb("Wb%d" % i, [128, EC], BF16) for i in range(2)]
        WT = [self.sb("WT%d" % i, [128, EC // 128, 128], BF16) for i in range(2)]
        t16 = self.sb("t16", [128, 2, 16], F32)
        wk = self.sb("pwk", [128, 256], F32)
        cand = self.sb("cand", [128, 16, 16], F32)
        c16 = self.sb("c16", [128, 16], F32)
        e16 = self.sb("e16", [128, 16], F32)
        scal = [self.sb("scal%d" % i, [128, 8, 8], F32) for i in range(2)]
        tf = self.sb("ptf", [128, 512], F32)
        cnt = {"s3": 0, "pendD": None}

        def load_tile(tt):
            sbi = tt % 2
            S.dma("sp", h2t[sbi][:], self.h2T_d.ap().rearrange("k p t -> p k t")[:, :, tt * 128:(tt + 1) * 128], w=[("h2tc", sbi)])

        def load_scores(tt):
            S.dma("sp", sct[:], self.sc_d.ap()[tt * 128:(tt + 1) * 128, :], r=[("scd", tt)], w=["sctc"])

        def topk(sbi, h):
            sl = scal[sbi]
            sk = ("scal", sbi, h)
            for pi in range(2):
                sv = sct[:, (2 * h + pi) * 128:(2 * h + pi + 1) * 128]
                self.DVE("max", ["sctc"], ["t16"], out=t16[:, pi, 0:8], in_=sv)
                self.DVE("match_replace", ["sctc", "t16"], ["pwk"], out=wk[:, 0:128], in_to_replace=t16[:, pi, 0:8], in_values=sv, imm_value=-1e30)
                self.DVE("max", ["pwk"], ["t16"], out=t16[:, pi, 8:16], in_=wk[:, 0:128])
            self.DVE("tensor_tensor", ["t16"], ["cand"], out=cand[:], in0=t16[:, 0, :].unsqueeze(2).to_broadcast([128, 16, 16]),
                     in1=t16[:, 1, :].unsqueeze(1).to_broadcast([128, 16, 16]), op=ALU.add)
            cf = cand[:].rearrange("p a b -> p (a b)")
            self.DVE("max", ["cand"], ["c16"], out=c16[:, 0:8], in_=cf)
            self.DVE("match_replace", ["cand", "c16"], ["pwk"], out=wk[:], in_to_replace=c16[:, 0:8], in_values=cf, imm_value=-1e30)
            self.DVE("max", ["pwk"], ["c16"], out=c16[:, 8:16], in_=wk[:])
            self.DVE("tensor_scalar", ["c16"], [sk], out=sl[:, h, 0:1], in0=c16[:, 0:1], scalar1=-1.0, scalar2=None, op0=ALU.mult)
            self.ACT(e16[:], c16[:], AF.Exp, ["c16", sk], ["e16", sk], bias=sl[:, h, 0:1], accum_out=sl[:, h, 1:2])
            self.ACT(sl[:, h, 2:3], sl[:, h, 1:2], AF.Ln, [sk], [sk])
            self.DVE("tensor_tensor", [sk], [sk], out=sl[:, h, 3:4], in0=sl[:, h, 0:1], in1=sl[:, h, 2:3], op=ALU.subtract)
            self.DVE("scalar_tensor_tensor", [sk, "t16"], [sk], out=sl[:, h, 5:6], in0=t16[:, 0, 0:1], scalar=-1.0, in1=sl[:, h, 2:3],
                     op0=ALU.mult, op1=ALU.subtract)
            self.DVE("tensor_scalar", ["t16"], [sk], out=sl[:, h, 6:7], in0=t16[:, 1, 0:1], scalar1=-1.0, scalar2=None, op0=ALU.mult)
            self.ACT(sl[:, h, 7:8], c16[:, 15:16], AF.Exp, ["c16", sk], [sk], bias=sl[:, h, 3:4])
            self.DVE("tensor_scalar", [sk], [sk], out=sl[:, h, 4:5], in0=sl[:, h, 7:8], scalar1=0.9995, scalar2=None, op0=ALU.mult)
            self.ACT(e12[sbi][:, h, 0, :], sct[:, (2 * h) * 128:(2 * h + 1) * 128], AF.Exp, ["sctc", sk], [("e12", sbi, h)], bias=sl[:, h, 5:6])
            self.ACT(e12[sbi][:, h, 1, :], sct[:, (2 * h + 1) * 128:(2 * h + 2) * 128], AF.Exp, ["sctc", sk], [("e12", sbi, h)], bias=sl[:, h, 6:7])

        def coef_step(sbi, a8, h):
            sl = scal[sbi]
            bi = cnt["s3"] % 2
            eng = "pool"
            cnt["s3"] += 1
            self.V(eng, "tensor_tensor", [("e12", sbi, h)], [("Ef", bi)], out=Ef[bi][:],
                   in0=e12[sbi][:, h, 0, a8 * 8:(a8 + 1) * 8].unsqueeze(2).to_broadcast([128, 8, 128]),
                   in1=e12[sbi][:, h, 1, :].unsqueeze(1).to_broadcast([128, 8, 128]), op=ALU.mult)
            eff = Ef[bi][:].rearrange("p a b -> p (a b)")
            self.DVE("scalar_tensor_tensor", [("Ef", bi), ("scal", sbi, h)], [("C", bi)], out=C[bi][:], in0=eff,
                     scalar=sl[:, h, 4:5], in1=eff, op0=ALU.is_ge, op1=ALU.mult)
            for j in range(2):
                self.mm(self.psb(1 + 2 * j)[:, :], self.ident_b[:], C[bi][:, j * 512:(j + 1) * 512], h == 0, h == 7,
                        ["ident_b", ("C", bi)], [("ps", 1 + 2 * j)])
            if h == 7:
                for j in range(2):
                    self.ACT(coef[sbi][:, a8 * 1024 + j * 512:a8 * 1024 + (j + 1) * 512], self.psb(1 + 2 * j)[:, :], AF.Copy,
                             [("ps", 1 + 2 * j)], [("coef", sbi, a8)])

        def flushD():
            pass

        def load_chunk(ec):
            b = ec % NB
            S.dma("sp", ub[b][:], self.uT_b.ap()[ec], r=[("uTb", i) for i in range(16)], w=[("ub", b)])
            S.dma("sp", vb[b][:], self.v_b.ap()[ec * EC:(ec + 1) * EC, :].rearrange("(s p) d -> p s d", p=128),
                  r=[("vb", (ec * EC) // 1024)], w=[("vbuf", b)])

        def act_mm(sbi, ec):
            b = ec % NB
            pa = self.psb(0)[:, (ec % 2) * EC:(ec % 2 + 1) * EC]
            for k in range(KC):
                self.mm(pa, h2t[sbi][:, k, :], ub[b][:, k, :], k == 0, k == KC - 1, [("h2tc", sbi), ("ub", b)], [("ps", 0)])

        seq = [(tt, ec) for tt in range(16) for ec in range(NEC)]
        PF = NB - 1
        load_tile(0)
        load_scores(0)
        for h in range(8):
            topk(0, h)
        for a8 in range(16):
            for h in range(8):
                coef_step(0, a8, h)
        load_scores(1)
        for i in range(PF):
            load_chunk(seq[i][1])
        act_mm(0, 0)
        for si, (tt, ec) in enumerate(seq):
            sbi = tt % 2
            nxt = (tt + 1) % 2
            if ec == 0 and tt + 1 < 16:
                load_tile(tt + 1)
            if si + PF < len(seq):
                load_chunk(seq[si + PF][1])
            b = ec % NB
            p2 = ec % 2
            pa = self.psb(0)[:, p2 * EC:(p2 + 1) * EC]
            self.ACT(Gb[p2][:], pa, AF.Gelu_apprx_tanh, [("ps", 0)], [("Gb", p2)])
            if si + 1 < len(seq):
                act_mm(seq[si + 1][0] % 2, seq[si + 1][1])
            self.DVE("tensor_tensor", [("Gb", p2), ("coef", sbi, (ec * EC) // 1024)], [("Wb", p2)], out=Wb[p2][:], in0=Gb[p2][:],
                     in1=coef[sbi][:, ec * EC:(ec + 1) * EC], op=ALU.mult)
            pt = self.psb(2, BF16)[:, p2 * EC:(p2 + 1) * EC]
            nsub = EC // 128
            for i in range(nsub):
                self.tr(pt[:, i * 128:(i + 1) * 128], Wb[p2][:, i * 128:(i + 1) * 128], self.ident_b[:], [("Wb", p2), "ident_b"], [("ps", 2)])
            self.ACT(WT[p2][:].rearrange("p a b -> p (a b)"), pt, AF.Copy, [("ps", 2)], [("WT", p2)])
            for i in range(nsub):
                for dc in range(4):
                    self.mm(self.psb(4 + dc)[:, :], WT[p2][:, i, :], vb[b][:, i, dc * 512:(dc + 1) * 512],
                            ec == 0 and i == 0, ec == NEC - 1 and i == nsub - 1, [("WT", p2), ("vbuf", b)], [("ps", 4 + dc)])
            if tt + 1 < 16:
                if ec == 0:
                    for h_ in range(8):
                        topk(nxt, h_)
                for idx in (2 * ec, 2 * ec + 1):
                    coef_step(nxt, idx // 8, idx % 8)
            if ec == NEC - 1:
                flushD()
                if tt + 2 < 16:
                    load_scores(tt + 2)
                for dc in range(4):
                    dsl = slice(dc * 512, (dc + 1) * 512)
                    S.dma("sp", x1c[:], self.y.ap()[tt * 128:(tt + 1) * 128, dsl], r=[("y", tt)], w=["x1c"])
                    self.DVE("tensor_tensor", [("ps", 4 + dc), "rowb"], ["ptf"], out=tf[:], in0=g2b[:, dsl], in1=self.psb(4 + dc)[:, :], op=ALU.mult)
                    self.DVE("tensor_tensor", ["ptf", "x1c"], ["x1c"], out=x1c[:], in0=x1c[:], in1=tf[:], op=ALU.add)
                    S.dma("sp", self.y.ap()[tt * 128:(tt + 1) * 128, dsl], x1c[:], r=["x1c"], w=[("y2", tt, dc)])
        self.release(m0)

    def finish(self):
        S = self.S
        S.add("sp", lambda e: e.nop(), extra_deps=[o.idx for o in S.ops if o.isdma])
        with ExitStack() as st:
            S.emit(st)
        return self.nc


def build(stop_after=None, dbg=()):
    B = Builder(stop_after, dbg)
    B.phase0()
    if stop_after != 1 and "nopeer" not in dbg:
        B.peer_precast()
    B.phase1()
    if stop_after != 1:
        if "skip2" not in dbg:
            B.phase2()
        else:
            B.w_in = B.din("w_in", [D, IN_COLS])
            B.Pbuf = [B.sb("P%d" % i, [128, 512], BF16) for i in range(3)]
            B.P_i = 0
            B.nb_kf = B.sb("nb_kf", [128, 512], F32)
            B.nb_sq = B.sb("nb_sq", [128, 512], BF16)
            B.nb_rs = B.sb("nb_rs", [128, 512], F32)
        B.phase3()
        B.phase4()
        if "nopeer" not in dbg:
            B.phase5()
    B.finish()
    return B


def _t5_bucket(n):
    n = np.maximum(n, 0)
    nf = np.maximum(n, 16).astype(np.float32)
    large = 16 + (np.log(nf / np.float32(16)) / np.float32(math.log(128.0)) * np.float32(16)).astype(np.int32)
    large = np.minimum(large, 31)
    return np.where(n < 16, n, large).astype(np.int64)


def _gather_strip(tbl_row, dist, valid):
    tb = np.concatenate([np.array([NEGB], np.float32), tbl_row.astype(np.float32)])
    idx = np.where(valid, _t5_bucket(dist) + 1, 0)
    return tb[idx]


def host_shared(inputs):
    m = {}
    rb = inputs["rel_bias"]
    p = np.arange(128)[:, None]
    strips = np.zeros((8, 128, 8064), np.float32)
    j = np.arange(2560)[None, :]
    r_sel = j - p - 384
    j2 = np.arange(1408)[None, :]
    r_win = j2 - p - 384
    j3 = np.arange(4096)[None, :]
    r_cmp = j3 - 16 * p - 31
    for h in range(8):
        strips[h, :, 0:2560] = _gather_strip(rb[h], r_sel, r_sel >= 0)
        strips[h, :, 2560:3968] = _gather_strip(rb[h], r_win, (r_win >= 0) & (r_win <= 511))
        strips[h, :, 3968:8064] = _gather_strip(rb[h], r_cmp, r_cmp >= 0)
    m["nsa_strips"] = strips
    dstrips = np.zeros((12, 128, 1024), np.float32)
    j4 = np.arange(1024)[None, :]
    r_d = j4 - p - 384
    for gi, (win, dil) in enumerate(DIL_CFG):
        for ps_ in range(4):
            hd = gi * 4 + ps_
            dstrips[hd] = _gather_strip(rb[8 + hd], r_d * dil, (r_d >= 0) & (r_d <= win // dil))
    m["dil_strips"] = dstrips
    c = (np.arange(2)[None, :, None] * 128 + np.arange(128)[:, None, None])
    jj = np.arange(64)[None, None, :]
    m["ovl"] = (((16 * c) < (64 * jj + 64)) & ((16 * c + 32) > (64 * jj))).astype(np.float32)
    jrow = np.arange(64)[:, None, None]
    kt = np.arange(32)[None, :, None]
    pp = np.arange(128)[None, None, :]
    m["e_all"] = (jrow == (2 * kt + pp // 64)).astype(np.float32)
    m["selg"] = np.ascontiguousarray(np.broadcast_to(np.eye(24, dtype=np.float32)[:, :, None], (24, 24, 128)))
    m["w_ada"] = inputs["w_ada"][0]
    m["b_ada_fm"] = np.ascontiguousarray(inputs["b_ada"][0].reshape(96, 128).T)
    m["norm1_g_fm"] = np.ascontiguousarray(inputs["norm1_g"][0].reshape(16, 128).T)
    m["norm2_g_fm"] = np.ascontiguousarray(inputs["norm2_g"][0].reshape(16, 128).T)
    m["w_in"] = inputs["w_in"][0]
    m["nsa_q_gain_fm"] = np.ascontiguousarray(inputs["nsa_q_gain"][0].reshape(128, 1))
    m["nsa_k_gain_fm"] = np.ascontiguousarray(inputs["nsa_k_gain"][0].T)
    m["cmp_posT"] = np.ascontiguousarray(inputs["cmp_pos"][0].transpose(2, 0, 1))
    m["cmp_w1"] = inputs["cmp_w1"][0]
    m["cmp_w2"] = inputs["cmp_w2"][0]
    m["dil_q_gain_fm"] = np.ascontiguousarray(inputs["dil_q_gain"][0].reshape(128, 1))
    m["dil_k_gain_fm"] = np.ascontiguousarray(inputs["dil_k_gain"][0].reshape(128, 1))
    m["w_br_nsa"] = inputs["w_br_nsa"][0]
    m["w_br_dil"] = inputs["w_br_dil"][0]
    m["w_out"] = inputs["w_out"][0]
    m["peer_w_q"] = inputs["peer_w_q"][0]
    m["peer_q_gain_row"] = np.ascontiguousarray(inputs["peer_q_gain"][0].reshape(1, 2048))
    m["peer_skT"] = np.ascontiguousarray(inputs["peer_sub_keys"][0].reshape(16, 128, 128).transpose(2, 0, 1))
    m["peer_uT"] = np.ascontiguousarray(inputs["peer_u"][0].T)
    m["peer_v"] = inputs["peer_v"][0]
    for hf in range(2):
        qi = np.arange(NOWN)
        cur = (qi + NOWN * hf) // 64
        jl = np.arange(64)[None, :]
        jg = jl - 32 + 32 * hf
        curc = cur[:, None]
        forced = (jg == 0) | (jg == curc) | (jg == curc - 1)
        bad = (jg > curc) | (jg < 0)
        add = np.where(forced & ~(jg < 0), 1e4, np.where(bad, -1e4, 0.0)).astype(np.float32)
        mult = np.where((forced & ~(jg < 0)) | bad, 0.0, 1.0).astype(np.float32)
        m["sel_add%d" % hf] = np.ascontiguousarray(add.reshape(16, 128, 64).transpose(1, 0, 2))
        m["sel_mult%d" % hf] = np.ascontiguousarray(mult.reshape(16, 128, 64).transpose(1, 0, 2))
    return m


def host_inputs(inputs, core, shared=None):
    if shared is None:
        shared = host_shared(inputs)
    b, hf = core // 2, core % 2
    x = inputs["x"]
    m = dict(shared)
    xl = np.zeros((NLOC, D), np.float32)
    if hf == 1:
        xl[:] = x[b]
    else:
        xl[NOWN:] = x[b, :NOWN]
    m["x_loc"] = xl
    m["pfx_bias"] = np.full((128, 1), 0.0 if hf == 1 else NEGB, np.float32)
    m["c_fm"] = np.ascontiguousarray(inputs["c"][b].reshape(16, 128).T)
    m["sel_add"] = shared["sel_add%d" % hf]
    m["sel_mult"] = shared["sel_mult%d" % hf]
    return m


def kernel(**inputs):
    inputs = {k: np.asarray(v) for k, v in inputs.items()}
    B = build()
    shared = host_shared(inputs)
    in_maps = []
    for c in range(8):
        hm = host_inputs(inputs, c, shared)
        in_maps.append({k: hm[k] for k in B.inputs})
    res = run_bass_kernel_spmd(B.nc, in_maps, core_ids=list(range(8)))
    out = np.zeros((4, 4096, D), np.float32)
    for c in range(8):
        out[c // 2, (c % 2) * NOWN:(c % 2 + 1) * NOWN] = res.results[c]["y"]
    return out
```

```python
import math
from contextlib import ExitStack
import numpy as np
import concourse.bass as bass
import concourse.mybir as mybir
from concourse.bass_utils import run_bass_kernel_spmd

F32 = mybir.dt.float32
BF16 = mybir.dt.bfloat16
I32 = mybir.dt.int32
U32 = mybir.dt.uint32
AF = mybir.ActivationFunctionType
ALU = mybir.AluOpType
AX = mybir.AxisListType

D = 2048
KC = 16
NLOC = 4096
NOWN = 2048
DH = 128
OFF_KV = 1024
OFF_GATE = OFF_KV + 1536
OFF_DIL = OFF_GATE + 24
OFF_MERGE = OFF_DIL + 4608
IN_COLS = OFF_MERGE + 4096
DIL_CFG = ((128, 1), (512, 4), (2048, 16))
EPS = 1e-6
NEGB = -30000.0
SCALE = DH ** -0.5
SB_BASE = 16640
SB_END = 229344


class Op:
    __slots__ = ("eng", "fn", "deps", "sig", "need", "isdma", "idx")


class Sched:
    COMPUTE = ("pe", "act", "dve", "pool")
    RING = 8

    def __init__(self, nc, same_engine_sync=True):
        self.nc = nc
        self.ops = []
        self.last_w = {}
        self.readers = {}
        self.ses = same_engine_sync
        self.last_on = {}
        self.dma_pend = []

    def add(self, eng, fn, r=(), w=(), dma=False, extra_deps=()):
        op = Op()
        op.eng = eng; op.fn = fn; op.isdma = dma; op.need = dma; op.sig = None
        op.idx = len(self.ops)
        deps = set(extra_deps)
        for k in r:
            lw = self.last_w.get(k)
            if lw is not None:
                deps.add(lw)
        for k in w:
            lw = self.last_w.get(k)
            if lw is not None:
                deps.add(lw)
            for rd in self.readers.get(k, ()):
                deps.add(rd)
        deps.discard(op.idx)
        fd = []
        for d in deps:
            o = self.ops[d]
            if (not o.isdma) and o.eng == eng and (eng == "pe" or not self.ses):
                continue
            o.need = True
            fd.append(d)
        op.deps = sorted(fd)
        self.ops.append(op)
        for k in w:
            self.last_w[k] = op.idx
            self.readers[k] = []
        for k in r:
            lst = self.readers.setdefault(k, [])
            if not dma:
                lst[:] = [x for x in lst if self.ops[x].isdma or self.ops[x].eng != eng]
            lst.append(op.idx)
        if not dma:
            self.last_on[eng] = op.idx
        else:
            self.dma_pend.append(op.idx)
        return op.idx

    def dma(self, eng, out, in_, r=(), w=(), **kw):
        return self.add(eng, lambda e: e.dma_start(out=out, in_=in_, **kw), r=r, w=w, dma=True)

    def barrier(self):
        lasts = dict(self.last_on)
        engs = ["pe", "act", "dve", "pool", "sp"]
        deps = list(lasts.values()) + list(self.dma_pend)
        for e in engs:
            self.add(e, lambda en: en.nop(), extra_deps=[d for d in deps])
        self.dma_pend = []
        self.last_w = {}
        self.readers = {}

    def emit(self, stack):
        nc = self.nc
        engs = sorted({o.eng for o in self.ops})
        csem = {}
        for e in engs:
            csem[e] = stack.enter_context(nc.semaphore("c_" + e))
        dring = {}
        for e in engs:
            if any(o.isdma and o.eng == e for o in self.ops):
                dring[e] = [stack.enter_context(nc.semaphore("d_%s%d" % (e, i))) for i in range(self.RING)]
        ccount = {e: 0 for e in engs}
        dcount = {e: 0 for e in engs}
        ringtot = {e: [0] * self.RING for e in engs}
        pre = {}
        for o in self.ops:
            if o.isdma:
                n = dcount[o.eng]; dcount[o.eng] += 1
                slot = n % self.RING
                prev = ringtot[o.eng][slot]
                ringtot[o.eng][slot] += 16
                pre[o.idx] = (dring[o.eng][slot], prev)
                o.sig = (dring[o.eng][slot], prev + 16, 16)
            elif o.need:
                ccount[o.eng] += 1
                o.sig = (csem[o.eng], ccount[o.eng], 1)
        per = {e: [o for o in self.ops if o.eng == e] for e in engs}
        self.stats = {e: (len(per[e]), ccount[e], dcount[e]) for e in engs}
        ops = self.ops
        blk = stack.enter_context(nc.Block())

        def runner(e):
            def body(engine):
                seen = {}
                for o in per[e]:
                    waits = []
                    if o.isdma:
                        s, v = pre[o.idx]
                        if v > 0:
                            waits.append((s, v))
                    for d in o.deps:
                        s, v, _ = ops[d].sig
                        waits.append((s, v))
                    for s, v in waits:
                        key = id(s)
                        if seen.get(key, 0) < v:
                            engine.wait_ge(s, v)
                            seen[key] = v
                    ins = o.fn(engine)
                    if o.sig is not None:
                        ins.then_inc(o.sig[0], o.sig[2])
            return body

        for e in engs:
            reg = {"pe": blk.tensor, "act": blk.scalar, "dve": blk.vector,
                   "pool": blk.gpsimd, "sp": blk.sync}[e]
            reg(runner(e))


def _dtsize(dt):
    return 2 if dt == BF16 else 4


class Builder:
    def __init__(self, stop_after=None, dbg=()):
        self.nc = bass.Bass("TRN2", target_bir_lowering=False)
        self.S = Sched(self.nc)
        self.off = SB_BASE
        self.cnt = 0
        self.stop_after = stop_after
        self.dbg = dbg
        self.inputs = {}
        self.outs = {}
        nc = self.nc
        self.psf = [nc.alloc_psum_tensor("psb%d" % i, [128, 512], F32) for i in range(8)]

    def sb(self, name, shape, dt):
        nbytes = int(np.prod(shape[1:])) * _dtsize(dt)
        self.cnt += 1
        t = self.nc.alloc_sbuf_tensor_at("%s_%d" % (name, self.cnt), list(shape), dt, offset=self.off)
        self.off += (nbytes + 63) // 64 * 64
        self.maxoff = max(getattr(self, "maxoff", 0), self.off)
        assert self.off <= SB_END, ("SBUF overflow", name, self.off)
        return t

    def mark(self):
        return self.off

    def release(self, m):
        self.S.barrier()
        self.off = m

    def din(self, name, shape, dt=F32):
        t = self.nc.dram_tensor(name, list(shape), dt, kind="ExternalInput")
        self.inputs[name] = t
        return t

    def dout(self, name, shape, dt=F32):
        t = self.nc.dram_tensor(name, list(shape), dt, kind="ExternalOutput")
        self.outs[name] = t
        return t

    def dscratch(self, name, shape, dt):
        return self.nc.dram_tensor(name, list(shape), dt)

    def psb(self, i, dt=F32):
        a = self.psf[i][:]
        if dt == BF16:
            a = a.bitcast(BF16)
        return a

    def PE(self, fn, r, w):
        return self.S.add("pe", fn, r=r, w=w)

    def mm(self, out, lhsT, rhs, start, stop, r, w):
        return self.S.add("pe", lambda e: e.matmul(out, lhsT=lhsT, rhs=rhs, start=start, stop=stop), r=r, w=w)

    def tr(self, out, in_, ident, r, w):
        return self.S.add("pe", lambda e: e.transpose(out, in_, ident), r=r, w=w)

    def ACT(self, out, in_, func, r, w, **kw):
        return self.S.add("act", lambda e: e.activation(out=out, in_=in_, func=func, **kw), r=r, w=w)

    def V(self, eng, name, r, w, *a, **kw):
        return self.S.add(eng, lambda e: getattr(e, name)(*a, **kw), r=r, w=w)

    def DVE(self, name, r, w, *a, **kw):
        return self.V("dve", name, r, w, *a, **kw)

    def POOL(self, name, r, w, *a, **kw):
        return self.V("pool", name, r, w, *a, **kw)

    def dump(self, name, src_tensor, shape, dt, rkeys):
        o = self.dout("dbg_" + name, shape, dt)
        self.S.dma("sp", o.ap(), src_tensor[:], r=rkeys)

    def phase0(self):
        S = self.S
        self.ident_f = self.sb("ident_f", [128, 128], F32)
        self.ident_b = self.sb("ident_b", [128, 128], BF16)
        self.ones_b = self.sb("ones_b", [128, 128], BF16)
        self.ones_f = self.sb("ones_f", [128, 128], F32)
        self.zero1 = self.sb("zero1", [128, 1], F32)
        self.pfxb = self.sb("pfxb", [128, 1], F32)
        idf = self.ident_f
        self.POOL("memset", [], ["ident_f"], idf[:], 0.0)
        S.add("pool", lambda e: e.affine_select(out=idf[:], in_=idf[:], pattern=[[-1, 128]],
                                                compare_op=ALU.not_equal, fill=1.0, base=0,
                                                channel_multiplier=1), r=["ident_f"], w=["ident_f"])
        self.DVE("tensor_copy", ["ident_f"], ["ident_b"], out=self.ident_b[:], in_=idf[:])
        self.POOL("memset", [], ["ones_b"], self.ones_b[:], 1.0)
        self.POOL("memset", [], ["ones_f"], self.ones_f[:], 1.0)
        self.POOL("memset", [], ["zero1"], self.zero1[:], 0.0)
        pf = self.din("pfx_bias", [128, 1])
        S.dma("sp", self.pfxb[:], pf.ap(), w=["pfxb"])

        c_in = self.din("c_fm", [128, 16])
        w_ada = self.din("w_ada", [D, 6 * D])
        b_ada = self.din("b_ada_fm", [128, 96])
        n1g = self.din("norm1_g_fm", [128, 16])
        n2g = self.din("norm2_g_fm", [128, 16])
        self.modT = self.sb("modT", [128, 96], F32)
        self.A1 = self.sb("A1", [128, 16], F32)
        self.A2 = self.sb("A2", [128, 16], F32)
        m = self.mark()
        cT = self.sb("cT", [128, 16], F32)
        sg = self.sb("sg", [128, 16], F32)
        condT = self.sb("condT", [128, 16], F32)
        bT = self.sb("bT", [128, 96], F32)
        g1T = self.sb("g1T", [128, 16], F32)
        g2T = self.sb("g2T", [128, 16], F32)
        wa = [self.sb("wa%d" % i, [128, 6 * D], BF16) for i in range(3)]
        condB = self.sb("condB", [128, 16], BF16)
        S.dma("sp", cT[:], c_in.ap(), w=["cT"])
        S.dma("sp", bT[:], b_ada.ap(), w=["bT"])
        S.dma("sp", g1T[:], n1g.ap(), w=["g1T"])
        S.dma("sp", g2T[:], n2g.ap(), w=["g2T"])
        self.ACT(condT[:], cT[:], AF.Silu, ["cT"], ["condT"])
        self.DVE("tensor_copy", ["condT"], ["condB"], out=condB[:], in_=condT[:])
        ps = self.psb(0)
        for k in range(KC):
            wk = wa[k % 3]
            key = "wa%d" % (k % 3)
            for hh in range(4):
                cs = slice(hh * 3072, (hh + 1) * 3072)
                S.dma("pool", wk[:, cs], w_ada.ap()[k * 128:(k + 1) * 128, cs], w=[key + "_%d" % hh])
            for j in range(96):
                self.mm(ps[:, j:j + 1], wk[:, j * 128:(j + 1) * 128], condB[:, k:k + 1], True, True,
                        [key + "_%d" % (j // 24), "condB"], [("ps", 0)])
            if k == 0:
                self.DVE("tensor_copy", [("ps", 0)], ["modT"], out=self.modT[:], in_=ps[:, 0:96])
            else:
                self.DVE("tensor_tensor", [("ps", 0), "modT"], ["modT"], out=self.modT[:], in0=self.modT[:],
                         in1=ps[:, 0:96], op=ALU.add)
        self.DVE("tensor_tensor", ["modT", "bT"], ["modT"], out=self.modT[:], in0=self.modT[:], in1=bT[:], op=ALU.add)
        self.DVE("scalar_tensor_tensor", ["modT", "g1T"], ["A1"], out=self.A1[:], in0=self.modT[:, 16:32],
                 scalar=1.0, in1=g1T[:], op0=ALU.add, op1=ALU.mult)
        self.DVE("scalar_tensor_tensor", ["modT", "g2T"], ["A2"], out=self.A2[:], in0=self.modT[:, 64:80],
                 scalar=1.0, in1=g2T[:], op0=ALU.add, op1=ALU.mult)
        if "mod" in self.dbg:
            self.dump("mod", self.modT, [128, 96], F32, ["modT"])
        self.release(m)

    def phase1(self):
        S = self.S
        x_in = self.din("x_loc", [NLOC, D])
        self.x_in = x_in
        self.hT_d = self.dscratch("hT_pfx", [KC, 128, NOWN], BF16)
        self.m_hT = self.mark()
        self.hT = self.sb("hT", [128, KC, NOWN], BF16)
        m = self.mark()
        xt = [self.sb("xt%d" % i, [128, D], F32) for i in range(2)]
        xn = [self.sb("xn%d" % i, [128, D], BF16) for i in range(2)]
        junk = self.sb("junk", [128, D], BF16)
        st = self.sb("st", [128, 3 * 32], F32)
        hg = [self.sb("hg%d" % i, [128, KC, 512], BF16) for i in range(2)]
        def stage1(tt):
            b = tt % 2
            S.dma("sp", xt[b][:], x_in.ap()[tt * 128:(tt + 1) * 128, :], w=["xt%d" % b])
            self.ACT(junk[:], xt[b][:], AF.Square, ["xt%d" % b], ["junk", ("st", tt)], accum_out=st[:, tt:tt + 1])
            self.ACT(st[:, 32 + tt:33 + tt], st[:, tt:tt + 1], AF.Sqrt, [("st", tt)], [("st", tt)],
                     scale=1.0 / D, bias=EPS)
            self.DVE("reciprocal", [("st", tt)], [("st", tt)], out=st[:, 64 + tt:65 + tt], in_=st[:, 32 + tt:33 + tt])
            self.DVE("tensor_scalar", ["xt%d" % b, ("st", tt)], ["xn%d" % b], out=xn[b][:], in0=xt[b][:],
                     scalar1=st[:, 64 + tt:65 + tt], scalar2=None, op0=ALU.mult)

        def stage2(tt):
            g, i = tt // 4, tt % 4
            b = tt % 2
            for half in range(2):
                pst = self.psb(half, BF16)
                for kk in range(8):
                    k = half * 8 + kk
                    self.tr(pst[:, kk * 128:(kk + 1) * 128], xn[b][:, k * 128:(k + 1) * 128], self.ident_b[:],
                            ["xn%d" % b, "ident_b"], [("ps", half)])
                for kk in range(8):
                    k = half * 8 + kk
                    if g < 4:
                        dst = hg[g % 2][:, k, i * 128:(i + 1) * 128]
                        wkey = [("hg", g % 2, i)]
                    else:
                        dst = self.hT[:, k, (g - 4) * 512 + i * 128:(g - 4) * 512 + (i + 1) * 128]
                        wkey = [("hT", g - 4)]
                    src = pst[:, kk * 128:(kk + 1) * 128]
                    if half == 0:
                        self.DVE("tensor_scalar", [("ps", half), "A1", "modT"], wkey, out=dst, in0=src,
                                 scalar1=self.A1[:, k:k + 1], scalar2=self.modT[:, k:k + 1], op0=ALU.mult, op1=ALU.add)
                    else:
                        self.ACT(dst, src, AF.Identity, [("ps", half), "A1", "modT"], wkey,
                                 scale=self.A1[:, k:k + 1], bias=self.modT[:, k:k + 1])
            if g < 4 and i == 3:
                S.dma("sp", self.hT_d.ap().rearrange("k p t -> p k t")[:, :, g * 512:(g + 1) * 512], hg[g % 2][:],
                      r=[("hg", g % 2, ii) for ii in range(4)], w=[("hTd", g)])

        stage1(0)
        for tt in range(32):
            if tt + 1 < 32:
                stage1(tt + 1)
            stage2(tt)
        if "hT" in self.dbg:
            self.dump("hT", self.hT, [128, KC, NOWN], BF16, [("hT", q) for q in range(4)])
        self.release(m)

    def load_w(self, eng, dst, col0, ncols, wkey):
        src = self.w_in.ap().rearrange("(k p) c -> p k c", p=128)[:, :, col0:col0 + ncols]
        self.S.dma("pool", dst, src, w=[wkey])

    def hchunk(self, tc):
        if tc >= 4:
            base = (tc - 4) * 512
            return (lambda k, lo=0, hi=512, st=1: self.hT[:, k, base + lo:base + hi:st]), [("hT", tc - 4)]
        b = self.pf_i % 2
        self.pf_i += 1
        buf = self.pfbuf[b]
        key = ("pfbuf", b)
        self.S.dma("sp", buf[:], self.hT_d.ap().rearrange("k p t -> p k t")[:, :, tc * 512:(tc + 1) * 512],
                   r=[("hTd", tc)], w=[key])
        return (lambda k, lo=0, hi=512, st=1: buf[:, k, lo:hi:st]), [key]

    def norm_fm(self, ps, pskey, dst, dkeys, gain_ap, gkey, n):
        kf, sq, rs = self.nb_kf, self.nb_sq, self.nb_rs
        self.ACT(kf[:, :n], ps, AF.Copy, [pskey], ["nb_kf"])
        self.ACT(sq[:, :n], ps, AF.Square, [pskey], ["nb_sq"])
        p7 = self.psb(7)
        self.mm(p7[:, :n], self.ones_b[:], sq[:, :n], True, True, ["ones_b", "nb_sq"], [("ps", 7)])
        self.ACT(rs[:, :n], p7[:, :n], AF.Sqrt, [("ps", 7)], ["nb_rs"], scale=1.0 / DH, bias=EPS)
        self.DVE("reciprocal", ["nb_rs"], ["nb_rs"], out=rs[:, :n], in_=rs[:, :n])
        self.DVE("scalar_tensor_tensor", ["nb_kf", "nb_rs", gkey], dkeys, out=dst, in0=kf[:, :n], scalar=gain_ap,
                 in1=rs[:, :n], op0=ALU.mult, op1=ALU.mult)

    def attn(self, tiles, q_rhs, qkeys, nq, ob, zb, imp=None):
        O = self.psb(ob)[:, :nq]
        Z = self.psb(zb)[:, :nq]
        n = len(tiles)

        def emit_S(i):
            t = tiles[i]
            sb_ = i % 2
            Sps = self.psb(sb_)[:, :nq]
            adds = t.get("adds", [])
            self.mm(Sps, t["kT"], q_rhs, True, len(adds) == 0, t["kkeys"] + qkeys, [("ps", sb_)])
            for ai, (l, r_, ks) in enumerate(adds):
                self.mm(Sps, l, r_, False, ai == len(adds) - 1, ks, [("ps", sb_)])

        emit_S(0)
        for i in range(n):
            if i + 1 < n:
                emit_S(i + 1)
            t = tiles[i]
            pb = self.P_i % 3
            self.P_i += 1
            P = self.Pbuf[pb][:, :nq]
            self.ACT(P, self.psb(i % 2)[:, :nq], AF.Exp, [("ps", i % 2), t["bkey"]], [("P", pb)], bias=t["bias"])
            self.mm(O, t["v"], P, i == 0, i == n - 1, t["vkeys"] + [("P", pb)], [("ps", ob)])
            self.mm(Z, self.ones_b[:], P, i == 0, i == n - 1, ["ones_b", ("P", pb)], [("ps", zb)])
            if imp is not None:
                lfn, ib = imp
                self.mm(self.psb(ib)[0:64, :nq], lfn(i), P, i == 0, i == n - 1, ["ovl", ("P", pb)], [("ps", ib)])

    def phase2(self):
        S = self.S
        self.w_in = self.din("w_in", [D, IN_COLS])
        gq = self.din("nsa_q_gain_fm", [128, 1])
        gk = self.din("nsa_k_gain_fm", [128, 3])
        posT_in = self.din("cmp_posT", [128, 2, 32])
        w1_in = self.din("cmp_w1", [2, 4096, 128])
        w2_in = self.din("cmp_w2", [2, 128, 128])
        strips_in = self.din("nsa_strips", [8, 128, 8064])
        ovl_in = self.din("ovl", [128, 2, 64])
        eall_in = self.din("e_all", [64, 32, 128])
        selg_in = self.din("selg", [24, 24, 128])
        mmask_in = self.din("sel_mult", [128, 16, 64])
        amask_in = self.din("sel_add", [128, 16, 64])
        self.ynsa_d = self.dscratch("ynsa_d", [8, 128, NOWN], BF16)

        self.Pbuf = [self.sb("P%d" % i, [128, 512], BF16) for i in range(3)]
        self.P_i = 0
        self.nb_kf = self.sb("nb_kf", [128, 512], F32)
        self.nb_sq = self.sb("nb_sq", [128, 512], BF16)
        self.nb_rs = self.sb("nb_rs", [128, 512], F32)
        m = self.mark()
        gqs = self.sb("gqs", [128, 1], F32)
        gkt = self.sb("gkt", [128, 3], F32)
        S.dma("sp", gqs[:], gq.ap(), w=["gqs"])
        S.dma("sp", gkt[:], gk.ap(), w=["gkt"])
        self.DVE("tensor_scalar", ["gqs"], ["gqs"], out=gqs[:], in0=gqs[:], scalar1=SCALE, scalar2=None, op0=ALU.mult)
        posterm = self.sb("posterm", [128, 2], F32)
        gateT = self.sb("gateT", [24, NOWN], BF16)
        Wg = self.sb("Wg", [128, KC, 24], BF16)
        self.load_w("pool", Wg[:], OFF_GATE, 24, "Wg")
        for qc in range(4):
            p5 = self.psb(5)
            for k in range(KC):
                self.mm(p5[0:24, :], Wg[:, k, :], self.hT[:, k, qc * 512:(qc + 1) * 512], k == 0, k == KC - 1,
                        ["Wg", ("hT", qc)], [("ps", 5)])
            self.ACT(gateT[:, qc * 512:(qc + 1) * 512], p5[0:24, :], AF.Sigmoid, [("ps", 5)], [("gateT", qc)])

        for g in range(2):
            mg = self.mark()
            kselT = self.sb("kselT", [128, NLOC], BF16)
            vsel = self.sb("vsel", [128, 32, 128], BF16)
            kwinT = self.sb("kwinT", [128, 2560], BF16)
            vwin = self.sb("vwin", [128, 20, 128], BF16)
            qT = self.sb("qT", [128, 4, NOWN], BF16)
            kcT = self.sb("kcT", [128, 256], BF16)
            vc = self.sb("vc", [128, 2, 128], BF16)
            mA = self.mark()
            kcmpT = self.sb("kcmpT", [128, 2, NLOC], BF16)
            mB = self.mark()
            Wkv = self.sb("Wkv", [128, 6, KC, 128], BF16)
            self.pfbuf = [self.sb("pfbuf%d" % i, [128, KC, 512], BF16) for i in range(2)]
            self.pf_i = 0
            for i in range(6):
                self.load_w("pool", Wkv[:, i], OFF_KV + (i * 2 + g) * 128, 128, ("Wkv", i))
            for tc in range(8):
                self.pc(1)
                hf_, hkeys = self.hchunk(tc)
                for i in (0, 1, 2, 4):
                    if i == 4 and tc < 3:
                        continue
                    bk = 5 + (i // 2) % 2
                    p5 = self.psb(bk)
                    pk = ("ps", bk)
                    for k in range(KC):
                        self.mm(p5[:, :], Wkv[:, i, k, :], hf_(k), k == 0, k == KC - 1, [("Wkv", i)] + hkeys, [pk])
                    if i < 2:
                        self.ACT(kcmpT[:, i, tc * 512:(tc + 1) * 512], p5[:, :], AF.Copy, [pk], [("kcmpT", i)])
                    elif i == 2:
                        self.norm_fm(p5[:, :], pk, kselT[:, tc * 512:(tc + 1) * 512], [("kselT", tc)], gkt[:, 1:2], "gkt", 512)
                    else:
                        self.norm_fm(p5[:, :], pk, kwinT[:, (tc - 3) * 512:(tc - 2) * 512], [("kwinT", tc - 3)], gkt[:, 2:3], "gkt", 512)
                for ti in range(4):
                    tt = tc * 4 + ti
                    p6 = self.psb(6)
                    units = (3, 5) if tc >= 3 else (3,)
                    for ui, i in enumerate(units):
                        for k in range(KC):
                            self.mm(p6[:, ui * 128:(ui + 1) * 128], hf_(k, ti * 128, (ti + 1) * 128), Wkv[:, i, k, :],
                                    k == 0, k == KC - 1, [("Wkv", i)] + hkeys, [("ps", 6)])
                    self.DVE("tensor_copy", [("ps", 6)], [("vsel", tt)], out=vsel[:, tt, :], in_=p6[:, 0:128])
                    if tc >= 3:
                        self.DVE("tensor_copy", [("ps", 6)], [("vwin", tt - 12)], out=vwin[:, tt - 12, :], in_=p6[:, 128:256])
            self.release(mB)
            Wq = self.sb("Wq", [128, 4, KC, 128], BF16)
            for r in range(4):
                self.load_w("pool", Wq[:, r], (4 * g + r) * 128, 128, ("Wq", r))
            for tc in range(4, 8):
                hf_, hkeys = self.hchunk(tc)
                for r in range(4):
                    p5 = self.psb(5)
                    for k in range(KC):
                        self.mm(p5[:, :], Wq[:, r, k, :], hf_(k), k == 0, k == KC - 1, [("Wq", r)] + hkeys, [("ps", 5)])
                    self.norm_fm(p5[:, :], ("ps", 5), qT[:, r, (tc - 4) * 512:(tc - 3) * 512], [("qT", r, tc - 4)],
                                 gqs[:, 0:1], "gqs", 512)
            self.release(mB)
            posT = self.sb("posT", [128, 2, 32], BF16)
            W1 = self.sb("W1", [128, 2, 32, 128], BF16)
            W2 = self.sb("W2", [128, 2, 128], BF16)
            hid = self.sb("hid", [128, 2, 256], BF16)
            S.dma("pool", posT[:], posT_in.ap(), w=["posT"])
            for j in range(2):
                S.dma("pool", W1[:, j], w1_in.ap()[j].rearrange("(l d) o -> d l o", d=128), w=[("W1", j)])
                S.dma("pool", W2[:, j, :], w2_in.ap()[j], w=[("W2", j)])
            self.POOL("memset", [], [("hid", 0), ("hid", 1)], hid[:], 0.0)
            if g == 0:
                for j in range(2):
                    p5 = self.psb(5)
                    for l in range(32):
                        self.mm(p5[:, 0:1], W1[:, j, l, :], posT[:, j, l:l + 1], l == 0, l == 31, [("W1", j), "posT"], [("ps", 5)])
                    self.DVE("tensor_copy", [("ps", 5)], [("posterm", j)], out=posterm[:, j:j + 1], in_=p5[:, 0:1])
            for j in range(2):
                p5 = self.psb(5)
                for l in range(32):
                    self.mm(p5[:, 0:255], W1[:, j, l, :], kcmpT[:, j, l:l + 16 * 254 + 1:16], l == 0, l == 31,
                            [("W1", j), ("kcmpT", j)], [("ps", 5)])
                self.ACT(hid[:, j, 0:255], p5[:, 0:255], AF.Gelu_apprx_tanh, [("ps", 5), ("posterm", j)], [("hid", j)],
                         bias=posterm[:, j:j + 1])
            p5 = self.psb(5)
            self.mm(p5[:, 0:256], W2[:, 0, :], hid[:, 0, :], True, True, [("W2", 0), ("hid", 0)], [("ps", 5)])
            self.norm_fm(p5[:, 0:256], ("ps", 5), kcT[:, :], ["kcT"], gkt[:, 0:1], "gkt", 256)
            for ct in range(2):
                p6 = self.psb(6)
                self.mm(p6[:, 0:128], hid[:, 1, ct * 128:(ct + 1) * 128], W2[:, 1, :], True, True,
                        [("W2", 1), ("hid", 1)], [("ps", 6)])
                self.DVE("tensor_copy", [("ps", 6)], [("vc", ct)], out=vc[:, ct, :], in_=p6[:, 0:128])
            self.release(mA)

            ovl = self.sb("ovl", [128, 2, 64], BF16)
            eall = self.sb("eall", [64, 32, 128], BF16)
            selg = self.sb("selg", [24, 24, 128], BF16)
            mmask = self.sb("mmask", [128, 16, 64], F32)
            amask = self.sb("amask", [128, 16, 64], F32)
            S.dma("pool", ovl[:], ovl_in.ap(), w=["ovl"])
            S.dma("pool", eall[:], eall_in.ap(), w=["eall"])
            S.dma("pool", selg[:], selg_in.ap(), w=["selg"])
            S.dma("sp", mmask[:], mmask_in.ap(), w=["mmask"])
            S.dma("sp", amask[:], amask_in.ap(), w=["amask"])
            strip = self.sb("strip", [128, 8064], BF16)
            selbT = self.sb("selbT", [64, NOWN], BF16)
            ycmp = self.sb("ycmp", [128, 4, 512], F32)
            impacc = self.sb("impacc", [64, 512], F32)
            rz = self.sb("rz", [128, 512], F32)
            wgt = self.sb("wgt", [128, 512], F32)
            tmpf = self.sb("tmpf", [128, 512], F32)
            yacc = self.sb("yacc", [128, 512], F32)
            ybuf = [self.sb("ybuf%d" % i, [128, 512], BF16) for i in range(2)]
            sc = self.sb("sc", [128, 4, 64], F32)
            wk = self.sb("wk", [128, 64], F32)
            m8 = self.sb("m8", [128, 16], F32)
            selb = self.sb("selb", [128, 4, 64], BF16)
            yb_i = 0
            for qc in range(4):
                qb = NOWN + 512 * qc
                qsl = slice(qc * 512, (qc + 1) * 512)
                for r in range(4):
                    h = 4 * g + r
                    S.dma("pool", strip[:, 3968:8064], strips_in.ap()[h][:, 3968:8064], w=["strip_c"])
                    tiles = []
                    for ct in range(2):
                        c0 = qb - 2048 * ct
                        tiles.append(dict(kT=kcT[:, ct * 128:(ct + 1) * 128], kkeys=["kcT"], v=vc[:, ct, :], vkeys=[("vc", ct)],
                                          adds=[(self.ident_b[:], strip[:, 3968 + c0:3968 + c0 + 512], ["ident_b", "strip_c"])],
                                          bias=(self.pfxb[:, 0:1] if ct == 0 else self.zero1[:, 0:1]),
                                          bkey=("pfxb" if ct == 0 else "zero1")))
                    self.attn(tiles, qT[:, r, qsl], [("qT", r, qc)], 512, 2, 3, imp=(lambda i: ovl[:, i, :], 4))
                    O = self.psb(2); Z = self.psb(3); I = self.psb(4)
                    self.DVE("tensor_scalar", [("ps", 3)], ["rz"], out=rz[:], in0=Z[:, :], scalar1=1e-30, scalar2=None, op0=ALU.max)
                    self.DVE("reciprocal", ["rz"], ["rz"], out=rz[:], in_=rz[:])
                    self.DVE("tensor_tensor", [("ps", 2), "rz"], [("ycmp", r)], out=ycmp[:, r, :], in0=O[:, :], in1=rz[:], op=ALU.mult)
                    if r == 0:
                        self.DVE("tensor_tensor", [("ps", 4), "rz"], ["impacc"], out=impacc[:], in0=I[0:64, :], in1=rz[0:64, :], op=ALU.mult)
                    else:
                        self.DVE("tensor_tensor", [("ps", 4), "rz"], ["tmpf"], out=tmpf[0:64, :], in0=I[0:64, :], in1=rz[0:64, :], op=ALU.mult)
                        self.DVE("tensor_tensor", ["tmpf", "impacc"], ["impacc"], out=impacc[:], in0=impacc[:], in1=tmpf[0:64, :], op=ALU.add)
                p5 = self.psb(5)
                for i in range(4):
                    self.mm(p5[:, i * 64:(i + 1) * 64], impacc[:, i * 128:(i + 1) * 128], self.ident_f[0:64, 0:64], True, True,
                            ["impacc", "ident_f"], [("ps", 5)])
                scv = sc[:].rearrange("p a b -> p (a b)")
                self.DVE("tensor_tensor", [("ps", 5), "mmask"], ["sc"], out=scv, in0=p5[:, 0:256],
                         in1=mmask[:, qc * 4:(qc + 1) * 4, :].rearrange("p a b -> p (a b)"), op=ALU.mult)
                self.DVE("tensor_tensor", ["sc", "amask"], ["sc"], out=scv, in0=scv,
                         in1=amask[:, qc * 4:(qc + 1) * 4, :].rearrange("p a b -> p (a b)"), op=ALU.add)
                for i in range(4):
                    self.DVE("max", ["sc"], ["m8"], out=m8[:, 0:8], in_=sc[:, i, :])
                    self.DVE("match_replace", ["sc", "m8"], ["wk"], out=wk[:], in_to_replace=m8[:, 0:8], in_values=sc[:, i, :], imm_value=-1e30)
                    self.DVE("max", ["wk"], ["m8"], out=m8[:, 8:16], in_=wk[:])
                    self.DVE("tensor_scalar", ["sc", "m8"], [("selb", i)], out=selb[:, i, :], in0=sc[:, i, :], scalar1=m8[:, 15:16],
                             scalar2=NEGB, op0=ALU.is_lt, op1=ALU.mult)
                    p6 = self.psb(6, BF16)
                    self.tr(p6[0:64, 0:128], selb[:, i, :], self.ident_b[:], [("selb", i), "ident_b"], [("ps", 6)])
                    self.ACT(selbT[:, qc * 512 + i * 128:qc * 512 + (i + 1) * 128], p6[0:64, 0:128], AF.Copy, [("ps", 6)], [("selbT", qc)])
                for r in range(4):
                    h = 4 * g + r
                    self.pc(1)
                    S.dma("pool", strip[:, 0:3968], strips_in.ap()[h][:, 0:3968], w=["strip_sw"])
                    G = self.psb(4)
                    self.mm(G[:, :], selg[:, 3 * h + 0, :], gateT[:, qsl], True, True, ["selg", ("gateT", qc)], [("ps", 4)])
                    self.DVE("tensor_tensor", [("ps", 4), ("ycmp", r)], ["yacc"], out=yacc[:], in0=ycmp[:, r, :], in1=G[:, :], op=ALU.mult)
                    for br in (1, 2):
                        tiles = []
                        if br == 1:
                            kts = range(0, 16 + 4 * qc + 4)
                        else:
                            kts = range(16 + 4 * qc - 4, 16 + 4 * qc + 4)
                        for kt in kts:
                            dlt = qb - 128 * kt
                            bias = self.pfxb[:, 0:1] if kt < 16 else self.zero1[:, 0:1]
                            bkey = "pfxb" if kt < 16 else "zero1"
                            if br == 1:
                                c0 = min(dlt + 384, 2048)
                                adds = [(eall[:, kt, :], selbT[:, qsl], ["eall", ("selbT", qc)]),
                                        (self.ident_b[:], strip[:, c0:c0 + 512], ["ident_b", "strip_sw"])]
                                tiles.append(dict(kT=kselT[:, kt * 128:(kt + 1) * 128], kkeys=[("kselT", kt // 4)],
                                                  v=vsel[:, kt, :], vkeys=[("vsel", kt)], adds=adds, bias=bias, bkey=bkey))
                            else:
                                c0 = 2560 + dlt + 384
                                adds = [(self.ident_b[:], strip[:, c0:c0 + 512], ["ident_b", "strip_sw"])]
                                tiles.append(dict(kT=kwinT[:, (kt - 12) * 128:(kt - 11) * 128], kkeys=[("kwinT", (kt - 12) // 4)],
                                                  v=vwin[:, kt - 12, :], vkeys=[("vwin", kt - 12)], adds=adds, bias=bias, bkey=bkey))
                        self.attn(tiles, qT[:, r, qsl], [("qT", r, qc)], 512, 2, 3)
                        O = self.psb(2); Z = self.psb(3)
                        self.mm(G[:, :], selg[:, 3 * h + br, :], gateT[:, qsl], True, True, ["selg", ("gateT", qc)], [("ps", 4)])
                        self.DVE("reciprocal", [("ps", 3)], ["rz"], out=rz[:], in_=Z[:, :])
                        self.DVE("tensor_tensor", [("ps", 4), "rz"], ["wgt"], out=wgt[:], in0=rz[:], in1=G[:, :], op=ALU.mult)
                        self.DVE("tensor_tensor", [("ps", 2), "wgt"], ["tmpf"], out=tmpf[:], in0=wgt[:], in1=O[:, :], op=ALU.mult)
                        if br == 1:
                            self.DVE("tensor_tensor", ["tmpf", "yacc"], ["yacc"], out=yacc[:], in0=yacc[:], in1=tmpf[:], op=ALU.add)
                        else:
                            yb = yb_i % 2
                            yb_i += 1
                            self.DVE("tensor_tensor", ["tmpf", "yacc"], [("ybuf", yb)], out=ybuf[yb][:], in0=yacc[:], in1=tmpf[:], op=ALU.add)
                            S.dma("sp", self.ynsa_d.ap()[h][:, qsl], ybuf[yb][:], r=[("ybuf", yb)], w=[("ynsa_d", h, qc)])
            self.release(mg)
        if "ynsa" in self.dbg:
            o = self.dout("dbg_ynsa", [8, 128, NOWN], BF16)
            S.dma("sp", o.ap(), self.ynsa_d.ap())
        self.release(m)

    def phase3(self):
        S = self.S
        gq = self.din("dil_q_gain_fm", [128, 1])
        gk = self.din("dil_k_gain_fm", [128, 1])
        dstr_in = self.din("dil_strips", [12, 128, 1024])
        self.ydil_d = self.dscratch("ydil_d", [4, 128, NOWN], BF16)
        m = self.mark()
        hTp = self.sb("hTp", [128, KC, NOWN], BF16)
        for c in range(4):
            S.dma("sp", hTp[:, :, c * 512:(c + 1) * 512], self.hT_d.ap().rearrange("k p t -> p k t")[:, :, c * 512:(c + 1) * 512],
                  w=[("hTp", c)])
        gqs = self.sb("dgqs", [128, 1], F32)
        gks = self.sb("dgks", [128, 1], F32)
        S.dma("sp", gqs[:], gq.ap(), w=["dgqs"])
        S.dma("sp", gks[:], gk.ap(), w=["dgks"])
        self.DVE("tensor_scalar", ["dgqs"], ["dgqs"], out=gqs[:], in0=gqs[:], scalar1=SCALE, scalar2=None, op0=ALU.mult)
        dacc = self.sb("dacc", [128, NOWN], F32)
        zacc = self.sb("zacc", [128, NOWN], F32)
        ybuf = self.sb("dybuf", [128, NOWN], BF16)
        W3 = self.sb("W3", [128, 3, KC, 128], BF16)
        qT = self.sb("dqT", [128, NOWN], BF16)
        kT = self.sb("dkT", [128, NLOC], BF16)
        vt = self.sb("dvt", [128, 32, 128], BF16)
        strip = self.sb("dstrip", [128, 1024], BF16)
        for p in range(4):
            for gi, (win, dil) in enumerate(DIL_CFG):
                self.pc(1)
                hd = gi * 4 + p
                for i in range(3):
                    self.load_w("pool", W3[:, i], OFF_DIL + (i * 12 + hd) * 128, 128, ("W3", i))
                S.dma("pool", strip[:], dstr_in.ap()[hd], w=["dstrip"])
                for tc in range(4):
                    p5 = self.psb(5)
                    for k in range(KC):
                        self.mm(p5[:, :], W3[:, 0, k, :], self.hT[:, k, tc * 512:(tc + 1) * 512], k == 0, k == KC - 1,
                                [("W3", 0), ("hT", tc)], [("ps", 5)])
                    self.norm_fm(p5[:, :], ("ps", 5), qT[:, tc * 512:(tc + 1) * 512], [("dqT", tc)], gqs[:, 0:1], "dgqs", 512)
                    p6 = self.psb(6)
                    for k in range(KC):
                        self.mm(p6[:, :], W3[:, 1, k, :], self.hT[:, k, tc * 512:(tc + 1) * 512], k == 0, k == KC - 1,
                                [("W3", 1), ("hT", tc)], [("ps", 6)])
                    self.norm_fm(p6[:, :], ("ps", 6), kT[:, NOWN + tc * 512:NOWN + (tc + 1) * 512], ["dkT"], gks[:, 0:1], "dgks", 512)
                pl = 128 * dil
                pieces = []
                t0 = NOWN - pl
                while t0 < NOWN:
                    n = min(512, NOWN - t0)
                    pieces.append((t0, n))
                    t0 += n
                for (t0, n) in pieces:
                    p6 = self.psb(6)
                    for k in range(KC):
                        self.mm(p6[:, :n], W3[:, 1, k, :], hTp[:, k, t0:t0 + n], k == 0, k == KC - 1,
                                [("W3", 1), ("hTp", t0 // 512)], [("ps", 6)])
                    self.norm_fm(p6[:, :n], ("ps", 6), kT[:, t0:t0 + n], ["dkT"], gks[:, 0:1], "dgks", n)
                ntile = 16 // dil + 1
                for r in range(dil):
                    for mt in range(ntile):
                        p6 = self.psb(6)
                        if mt == 0:
                            st = NOWN - pl + r
                            src = lambda k: hTp[:, k, st:st + dil * 127 + 1:dil]
                            keys = [("hTp", c) for c in range((NOWN - pl) // 512, 4)]
                        else:
                            st = r + dil * 128 * (mt - 1)
                            src = lambda k: self.hT[:, k, st:st + dil * 127 + 1:dil]
                            keys = [("hT", c) for c in range(st // 512, (st + dil * 127) // 512 + 1)]
                        for k in range(KC):
                            self.mm(p6[:, 0:128], src(k), W3[:, 2, k, :], k == 0, k == KC - 1, [("W3", 2)] + keys, [("ps", 6)])
                        self.DVE("tensor_copy", [("ps", 6)], [("dvt", r * ntile + mt)], out=vt[:, r * ntile + mt, :], in_=p6[:, 0:128])
                nq = min(512, NOWN // dil)
                nch = (NOWN // dil) // nq
                for r in range(dil):
                    for ci in range(nch):
                        qs = r + dil * ci * nq
                        q_rhs = qT[:, qs:qs + dil * (nq - 1) + 1:dil]
                        tiles = []
                        for mt in range(ci * nq // 128, ci * nq // 128 + nq // 128 + 1):
                            delta = (128 + ci * nq) - 128 * mt
                            c0 = delta + 384
                            ks = NOWN - pl + r + dil * 128 * mt
                            tiles.append(dict(kT=kT[:, ks:ks + dil * 127 + 1:dil], kkeys=["dkT"], v=vt[:, r * ntile + mt, :],
                                              vkeys=[("dvt", r * ntile + mt)],
                                              adds=[(self.ident_b[:], strip[:, c0:c0 + nq], ["ident_b", "dstrip"])],
                                              bias=(self.pfxb[:, 0:1] if mt == 0 else self.zero1[:, 0:1]),
                                              bkey=("pfxb" if mt == 0 else "zero1")))
                        self.attn(tiles, q_rhs, [("dqT", c) for c in range(4)], nq, 2, 3)
                        O = self.psb(2)[:, :nq]; Z = self.psb(3)[:, :nq]
                        dsl = slice(qs, qs + dil * (nq - 1) + 1, dil)
                        if gi == 0:
                            self.DVE("tensor_copy", [("ps", 2)], ["dacc"], out=dacc[:, dsl], in_=O)
                            self.DVE("tensor_copy", [("ps", 3)], ["zacc"], out=zacc[:, dsl], in_=Z)
                        else:
                            self.DVE("tensor_tensor", [("ps", 2), "dacc"], ["dacc"], out=dacc[:, dsl], in0=dacc[:, dsl], in1=O, op=ALU.add)
                            self.DVE("tensor_tensor", [("ps", 3), "zacc"], ["zacc"], out=zacc[:, dsl], in0=zacc[:, dsl], in1=Z, op=ALU.add)
            self.DVE("reciprocal", ["zacc"], ["zacc"], out=zacc[:], in_=zacc[:])
            self.DVE("tensor_tensor", ["zacc", "dacc"], ["dybuf"], out=ybuf[:], in0=dacc[:], in1=zacc[:], op=ALU.mult)
            S.dma("sp", self.ydil_d.ap()[p], ybuf[:], r=["dybuf"], w=[("ydil_d", p)])
        if "ydil" in self.dbg:
            o = self.dout("dbg_ydil", [4, 128, NOWN], BF16)
            S.dma("sp", o.ap(), self.ydil_d.ap(), r=[("ydil_d", p) for p in range(4)])
        self.release(m)

    def row_bcast(self, dst, col0):
        diag = self.sb("diag", [128, 128], F32)
        for k in range(KC):
            self.DVE("tensor_scalar", ["ident_f", "modT"], ["diag"], out=diag[:], in0=self.ident_f[:],
                     scalar1=self.modT[:, col0 + k:col0 + k + 1], scalar2=None, op0=ALU.mult)
            p5 = self.psb(5)
            self.mm(p5[:, 0:128], self.ones_f[:], diag[:], True, True, ["ones_f", "diag"], [("ps", 5)])
            self.ACT(dst[:, k * 128:(k + 1) * 128], p5[:, 0:128], AF.Copy, [("ps", 5)], ["rowb"])

    def phase4(self):
        S = self.S
        wbn_in = self.din("w_br_nsa", [1024, D])
        wbd_in = self.din("w_br_dil", [512, D])
        wout_in = self.din("w_out", [D, D])
        self.y = self.dout("y", [NOWN, D])
        self.h2T_d = self.dscratch("h2T_d", [KC, 128, NOWN], BF16)
        m = self.mark()
        g1b = self.sb("g1b", [128, D], F32)
        self.row_bcast(g1b, 32)
        yn = self.sb("yn", [128, 8, 512], BF16)
        yd = self.sb("yd", [128, 4, 512], BF16)
        Wbn2 = [self.sb("Wbn%d" % i, [128, 8, 128], BF16) for i in range(2)]
        Wbd2 = [self.sb("Wbd%d" % i, [128, 4, 128], BF16) for i in range(2)]
        Wm2 = [self.sb("Wm%d" % i, [128, 2, KC, 128], BF16) for i in range(2)]
        mg = self.sb("mg", [128, KC, 512], BF16)
        Wo = self.sb("Wo", [128, KC, 512], BF16)
        xt = [self.sb("x4_%d" % i, [128, D], F32) for i in range(4)]
        xn = self.sb("xn4", [128, D], BF16)
        h2g = self.sb("h2g", [128, KC, 512], BF16)
        s1 = self.sb("s1", [128, 512], F32)
        s2 = self.sb("s2", [128, 512], F32)
        t1 = self.sb("t1", [128, 512], F32)
        st = self.sb("st4", [128, 3], F32)
        for qc in range(4):
            qsl = slice(qc * 512, (qc + 1) * 512)
            S.dma("sp", yn[:], self.ynsa_d.ap().rearrange("h p t -> p h t")[:, :, qsl], w=["yn"])
            S.dma("sp", yd[:], self.ydil_d.ap().rearrange("h p t -> p h t")[:, :, qsl], w=["yd"])
            for ti in range(4):
                tt = qc * 4 + ti
                S.dma("sp", xt[ti][:], self.x_in.ap()[NOWN + tt * 128:NOWN + (tt + 1) * 128, :], w=[("x4", ti)])
            for fc in range(KC):
                fsl = slice(fc * 128, (fc + 1) * 128)
                wb = fc % 2
                Wbn, Wbd, Wm = Wbn2[wb], Wbd2[wb], Wm2[wb]
                S.dma("pool", Wbn[:], wbn_in.ap().rearrange("(h d) f -> d h f", d=128)[:, :, fsl], w=[("Wbn", wb)])
                S.dma("pool", Wbd[:], wbd_in.ap().rearrange("(h d) f -> d h f", d=128)[:, :, fsl], w=[("Wbd", wb)])
                self.load_w("pool", Wm[:, 0], OFF_MERGE + fc * 128, 128, ("Wm", 0, wb))
                self.load_w("pool", Wm[:, 1], OFF_MERGE + D + fc * 128, 128, ("Wm", 1, wb))
                A = self.psb(2); Bm = self.psb(3); G1 = self.psb(0); G2 = self.psb(1)
                for h in range(8):
                    self.mm(A[:, :], Wbn[:, h, :], yn[:, h, :], h == 0, h == 7, [("Wbn", wb), "yn"], [("ps", 2)])
                for p in range(4):
                    self.mm(Bm[:, :], Wbd[:, p, :], yd[:, p, :], p == 0, p == 3, [("Wbd", wb), "yd"], [("ps", 3)])
                for k in range(KC):
                    self.mm(G1[:, :], Wm[:, 0, k, :], self.hT[:, k, qsl], k == 0, k == KC - 1, [("Wm", 0, wb), ("hT", qc)], [("ps", 0)])
                for k in range(KC):
                    self.mm(G2[:, :], Wm[:, 1, k, :], self.hT[:, k, qsl], k == 0, k == KC - 1, [("Wm", 1, wb), ("hT", qc)], [("ps", 1)])
                self.ACT(s1[:], G1[:, :], AF.Sigmoid, [("ps", 0)], ["s1"])
                self.ACT(s2[:], G2[:, :], AF.Sigmoid, [("ps", 1)], ["s2"])
                self.DVE("tensor_tensor", [("ps", 2), "s1"], ["s1"], out=s1[:], in0=s1[:], in1=A[:, :], op=ALU.mult)
                self.DVE("tensor_tensor", [("ps", 3), "s2"], ["s2"], out=s2[:], in0=s2[:], in1=Bm[:, :], op=ALU.mult)
                self.DVE("tensor_tensor", ["s1", "s2"], [("mg", fc)], out=mg[:, fc, :], in0=s1[:], in1=s2[:], op=ALU.add)
            if "merged" in self.dbg:
                if qc == 0:
                    self.dbg_merged = self.dout("dbg_merged", [128, KC, NOWN], BF16)
                S.dma("sp", self.dbg_merged.ap()[:, :, qsl], mg[:], r=[("mg", fc) for fc in range(KC)])
            for fo in range(4):
                fos = slice(fo * 512, (fo + 1) * 512)
                S.dma("pool", Wo[:], wout_in.ap().rearrange("(k p) f -> p k f", p=128)[:, :, fos], w=["Wo"])
                for ti in range(4):
                    po = self.psb(4 + ti % 2)
                    for fc in range(KC):
                        self.mm(po[:, :], mg[:, fc, ti * 128:(ti + 1) * 128], Wo[:, fc, :], fc == 0, fc == KC - 1,
                                [("mg", fc), "Wo"], [("ps", 4 + ti % 2)])
                    self.DVE("tensor_tensor", [("ps", 4 + ti % 2), "rowb"], ["t1"], out=t1[:], in0=g1b[:, fos], in1=po[:, :], op=ALU.mult)
                    self.DVE("tensor_tensor", ["t1", ("x4", ti)], [("x4", ti)], out=xt[ti][:, fos], in0=xt[ti][:, fos], in1=t1[:], op=ALU.add)
            for ti in range(4):
                tt = qc * 4 + ti
                S.dma("sp", self.y.ap()[tt * 128:(tt + 1) * 128, :], xt[ti][:], r=[("x4", ti)], w=[("y", tt)])
                self.ACT(xn[:], xt[ti][:], AF.Square, [("x4", ti)], ["xn4", "st4"], accum_out=st[:, 0:1])
                self.ACT(st[:, 1:2], st[:, 0:1], AF.Sqrt, ["st4"], ["st4"], scale=1.0 / D, bias=EPS)
                self.DVE("reciprocal", ["st4"], ["st4"], out=st[:, 2:3], in_=st[:, 1:2])
                self.DVE("tensor_scalar", [("x4", ti), "st4"], ["xn4"], out=xn[:], in0=xt[ti][:], scalar1=st[:, 2:3], scalar2=None, op0=ALU.mult)
                for half in range(2):
                    pst = self.psb(6 + half, BF16)
                    for kk in range(8):
                        k = half * 8 + kk
                        self.tr(pst[:, kk * 128:(kk + 1) * 128], xn[:, k * 128:(k + 1) * 128], self.ident_b[:],
                                ["xn4", "ident_b"], [("ps", 6 + half)])
                    for kk in range(8):
                        k = half * 8 + kk
                        dst = h2g[:, k, ti * 128:(ti + 1) * 128]
                        src = pst[:, kk * 128:(kk + 1) * 128]
                        if half == 0:
                            self.DVE("tensor_scalar", [("ps", 6 + half), "A2", "modT"], [("h2g", ti)], out=dst, in0=src,
                                     scalar1=self.A2[:, k:k + 1], scalar2=self.modT[:, 48 + k:49 + k], op0=ALU.mult, op1=ALU.add)
                        else:
                            self.ACT(dst, src, AF.Identity, [("ps", 6 + half), "A2", "modT"], [("h2g", ti)],
                                     scale=self.A2[:, k:k + 1], bias=self.modT[:, 48 + k:49 + k])
            S.dma("sp", self.h2T_d.ap().rearrange("k p t -> p k t")[:, :, qsl], h2g[:], r=[("h2g", ti) for ti in range(4)],
                  w=[("h2Td", qc)])
        if "h2T" in self.dbg:
            o = self.dout("dbg_h2T", [KC, 128, NOWN], BF16)
            S.dma("sp", o.ap(), self.h2T_d.ap(), r=[("h2Td", q) for q in range(4)])
        self.release(m)

    def peer_precast(self):
        S = self.S
        self.uT_in = self.din("peer_uT", [D, 16384])
        self.v_in = self.din("peer_v", [16384, D])
        self.uT_b = self.dscratch("uT_b", [64, 128, KC, 256], BF16)
        self.v_b = self.dscratch("v_b", [16384, D], BF16)
        self.pcq = []
        for i in range(32):
            k_, p0 = i // 2, (i % 2) * 64
            self.pcq.append((self.uT_b.ap()[:, p0:p0 + 64, k_, :].rearrange("c p e -> p c e"),
                             self.uT_in.ap()[i * 64:(i + 1) * 64, :].rearrange("p (c e) -> p c e", e=256), ("uTb", i // 2)))
        for i in range(32):
            self.pcq.append((self.v_b.ap()[i * 512:(i + 1) * 512, :], self.v_in.ap()[i * 512:(i + 1) * 512, :], ("vb", i // 2)))
        self.pcq.reverse()

    def pc(self, n=1):
        for _ in range(n):
            if getattr(self, "pcq", None):
                o, i_, key = self.pcq.pop()
                self.S.dma("pool", o, i_, w=[(key, len(self.pcq))])

    def phase5(self):
        S = self.S
        wq_in = self.din("peer_w_q", [D, D])
        gain_in = self.din("peer_q_gain_row", [1, D])
        skT_in = self.din("peer_skT", [128, 16, 128])
        self.sc_d = self.dscratch("sc_d", [NOWN, D], F32)
        self.pc(64)
        self.release(self.m_hT)
        m0 = self.mark()
        g2b = self.sb("g2b", [128, D], F32)
        self.row_bcast(g2b, 80)
        m = self.mark()
        Wq = self.sb("pWq", [128, KC, D], BF16)
        for k in range(KC):
            S.dma("pool", Wq[:, k, :], wq_in.ap()[k * 128:(k + 1) * 128, :], w=[("pWq", k)])
        gqb = self.sb("gqb", [128, D], F32)
        S.dma("sp", gqb[:], gain_in.ap().partition_broadcast(128), w=["gqb"])
        skT = self.sb("skT", [128, 16, 128], BF16)
        S.dma("pool", skT[:], skT_in.ap(), w=["skT"])
        h2t = [self.sb("h2t%d" % i, [128, KC, 128], BF16) for i in range(2)]
        sq = self.sb("psq", [128, 512], F32)
        ss = self.sb("pss", [128, 16], F32)
        qn = self.sb("pqn", [128, D], BF16)
        tq = self.sb("ptq", [128, 512], F32)
        qnT = self.sb("pqnT", [128, 16, 128], BF16)
        sct = [self.sb("psct%d" % i, [128, D], F32) for i in range(2)]
        for tt in range(16):
            b = tt % 2
            S.dma("sp", h2t[b][:], self.h2T_d.ap().rearrange("k p t -> p k t")[:, :, tt * 128:(tt + 1) * 128], w=[("h2t", b)])
            for fo in range(4):
                pq = self.psb(fo)
                for k in range(KC):
                    self.mm(pq[:, :], h2t[b][:, k, :], Wq[:, k, fo * 512:(fo + 1) * 512], k == 0, k == KC - 1,
                            [("h2t", b), ("pWq", k)], [("ps", fo)])
                self.ACT(sq[:], pq[:, :], AF.Square, [("ps", fo)], ["psq"])
                self.DVE("reduce_sum", ["psq"], [("pss", fo)], out=ss[:, fo * 4:(fo + 1) * 4],
                         in_=sq[:].rearrange("p (a b) -> p a b", b=128), axis=AX.X)
            self.ACT(ss[:], ss[:], AF.Sqrt, [("pss", f) for f in range(4)], [("pss", f) for f in range(4)], scale=1.0 / 128, bias=EPS)
            self.DVE("reciprocal", [("pss", f) for f in range(4)], [("pss", f) for f in range(4)], out=ss[:], in_=ss[:])
            for fo in range(4):
                pq = self.psb(fo)
                self.DVE("tensor_tensor", [("ps", fo), ("pss", 0)], ["ptq"], out=tq[:].rearrange("p (a b) -> p a b", b=128),
                         in0=pq[:, :].rearrange("p (a b) -> p a b", b=128),
                         in1=ss[:, fo * 4:(fo + 1) * 4].unsqueeze(2).to_broadcast([128, 4, 128]), op=ALU.mult)
                self.DVE("tensor_tensor", ["ptq", "gqb"], [("pqn", fo)], out=qn[:, fo * 512:(fo + 1) * 512], in0=tq[:],
                         in1=gqb[:, fo * 512:(fo + 1) * 512], op=ALU.mult)
            for half in range(2):
                pst = self.psb(4 + half, BF16)
                for kk in range(8):
                    hp = half * 8 + kk
                    self.tr(pst[:, kk * 128:(kk + 1) * 128], qn[:, hp * 128:(hp + 1) * 128], self.ident_b[:],
                            [("pqn", hp // 4), "ident_b"], [("ps", 4 + half)])
                self.ACT(qnT[:, half * 8:(half + 1) * 8, :].rearrange("p a b -> p (a b)"), pst[:, :], AF.Copy, [("ps", 4 + half)], [("pqnT", half)])
            for fo in range(4):
                pq = self.psb(fo)
                for i in range(4):
                    hp = fo * 4 + i
                    self.mm(pq[:, i * 128:(i + 1) * 128], qnT[:, hp, :], skT[:, hp, :], True, True,
                            [("pqnT", hp // 8), "skT"], [("ps", fo)])
                self.ACT(sct[b][:, fo * 512:(fo + 1) * 512], pq[:, :], AF.Copy, [("ps", fo)], [("psct", b)])
            S.dma("sp", self.sc_d.ap()[tt * 128:(tt + 1) * 128, :], sct[b][:], r=[("psct", b)], w=[("scd", tt)])
        self.release(m)

        EC = 256
        NB = 4
        NEC = 16384 // EC
        coef = [self.sb("coef%d" % i, [128, 16384], BF16) for i in range(2)]
        Ef = [self.sb("Ef%d" % i, [128, 8, 128], F32) for i in range(2)]
        C = [self.sb("C%d" % i, [128, 1024], BF16) for i in range(2)]
        ub = [self.sb("ub%d" % i, [128, KC, EC], BF16) for i in range(NB)]
        vb = [self.sb("vb%d" % i, [128, EC // 128, D], BF16) for i in range(NB)]
        h2t = [self.sb("h2tc%d" % i, [128, KC, 128], BF16) for i in range(2)]
        sct = self.sb("sctc", [128, D], F32)
        e12 = [self.sb("e12_%d" % i, [128, 8, 2, 128], F32) for i in range(2)]
        x1c = self.sb("x1c", [128, 512], F32)
        Gb = [self.sb("Gb%d" % i, [128, EC], BF16) for i in range(2)]
        Wb = [self.sb("Wb%d" % i, [128, EC], BF16) for i in range(2)]
        WT = [self.sb("WT%d" % i, [128, EC // 128, 128], BF16) for i in range(2)]
        t16 = self.sb("t16", [128, 2, 16], F32)
        wk = self.sb("pwk", [128, 256], F32)
        cand = self.sb("cand", [128, 16, 16], F32)
        c16 = self.sb("c16", [128, 16], F32)
        e16 = self.sb("e16", [128, 16], F32)
        scal = [self.sb("scal%d" % i, [128, 8, 8], F32) for i in range(2)]
        tf = self.sb("ptf", [128, 512], F32)
        cnt = {"s3": 0, "pendD": None}

        def load_tile(tt):
            sbi = tt % 2
            S.dma("sp", h2t[sbi][:], self.h2T_d.ap().rearrange("k p t -> p k t")[:, :, tt * 128:(tt + 1) * 128], w=[("h2tc", sbi)])

        def load_scores(tt):
            S.dma("sp", sct[:], self.sc_d.ap()[tt * 128:(tt + 1) * 128, :], r=[("scd", tt)], w=["sctc"])

        def topk(sbi, h):
            sl = scal[sbi]
            sk = ("scal", sbi, h)
            for pi in range(2):
                sv = sct[:, (2 * h + pi) * 128:(2 * h + pi + 1) * 128]
                self.DVE("max", ["sctc"], ["t16"], out=t16[:, pi, 0:8], in_=sv)
                self.DVE("match_replace", ["sctc", "t16"], ["pwk"], out=wk[:, 0:128], in_to_replace=t16[:, pi, 0:8], in_values=sv, imm_value=-1e30)
                self.DVE("max", ["pwk"], ["t16"], out=t16[:, pi, 8:16], in_=wk[:, 0:128])
            self.DVE("tensor_tensor", ["t16"], ["cand"], out=cand[:], in0=t16[:, 0, :].unsqueeze(2).to_broadcast([128, 16, 16]),
                     in1=t16[:, 1, :].unsqueeze(1).to_broadcast([128, 16, 16]), op=ALU.add)
            cf = cand[:].rearrange("p a b -> p (a b)")
            self.DVE("max", ["cand"], ["c16"], out=c16[:, 0:8], in_=cf)
            self.DVE("match_replace", ["cand", "c16"], ["pwk"], out=wk[:], in_to_replace=c16[:, 0:8], in_values=cf, imm_value=-1e30)
            self.DVE("max", ["pwk"], ["c16"], out=c16[:, 8:16], in_=wk[:])
            self.DVE("tensor_scalar", ["c16"], [sk], out=sl[:, h, 0:1], in0=c16[:, 0:1], scalar1=-1.0, scalar2=None, op0=ALU.mult)
            self.ACT(e16[:], c16[:], AF.Exp, ["c16", sk], ["e16", sk], bias=sl[:, h, 0:1], accum_out=sl[:, h, 1:2])
            self.ACT(sl[:, h, 2:3], sl[:, h, 1:2], AF.Ln, [sk], [sk])
            self.DVE("tensor_tensor", [sk], [sk], out=sl[:, h, 3:4], in0=sl[:, h, 0:1], in1=sl[:, h, 2:3], op=ALU.subtract)
            self.DVE("scalar_tensor_tensor", [sk, "t16"], [sk], out=sl[:, h, 5:6], in0=t16[:, 0, 0:1], scalar=-1.0, in1=sl[:, h, 2:3],
                     op0=ALU.mult, op1=ALU.subtract)
            self.DVE("tensor_scalar", ["t16"], [sk], out=sl[:, h, 6:7], in0=t16[:, 1, 0:1], scalar1=-1.0, scalar2=None, op0=ALU.mult)
            self.ACT(sl[:, h, 7:8], c16[:, 15:16], AF.Exp, ["c16", sk], [sk], bias=sl[:, h, 3:4])
            self.DVE("tensor_scalar", [sk], [sk], out=sl[:, h, 4:5], in0=sl[:, h, 7:8], scalar1=0.9995, scalar2=None, op0=ALU.mult)
            self.ACT(e12[sbi][:, h, 0, :], sct[:, (2 * h) * 128:(2 * h + 1) * 128], AF.Exp, ["sctc", sk], [("e12", sbi, h)], bias=sl[:, h, 5:6])
            self.ACT(e12[sbi][:, h, 1, :], sct[:, (2 * h + 1) * 128:(2 * h + 2) * 128], AF.Exp, ["sctc", sk], [("e12", sbi, h)], bias=sl[:, h, 6:7])

        def coef_step(sbi, a8, h):
            sl = scal[sbi]
            bi = cnt["s3"] % 2
            eng = "dve" if cnt["s3"] % 8 == 7 else "pool"
            cnt["s3"] += 1
            self.V(eng, "tensor_tensor", [("e12", sbi, h)], [("Ef", bi)], out=Ef[bi][:],
                   in0=e12[sbi][:, h, 0, a8 * 8:(a8 + 1) * 8].unsqueeze(2).to_broadcast([128, 8, 128]),
                   in1=e12[sbi][:, h, 1, :].unsqueeze(1).to_broadcast([128, 8, 128]), op=ALU.mult)
            eff = Ef[bi][:].rearrange("p a b -> p (a b)")
            self.DVE("scalar_tensor_tensor", [("Ef", bi), ("scal", sbi, h)], [("C", bi)], out=C[bi][:], in0=eff,
                     scalar=sl[:, h, 4:5], in1=eff, op0=ALU.is_ge, op1=ALU.mult)
            for j in range(2):
                self.mm(self.psb(1 + 2 * j)[:, :], self.ident_b[:], C[bi][:, j * 512:(j + 1) * 512], h == 0, h == 7,
                        ["ident_b", ("C", bi)], [("ps", 1 + 2 * j)])
            if h == 7:
                for j in range(2):
                    self.ACT(coef[sbi][:, a8 * 1024 + j * 512:a8 * 1024 + (j + 1) * 512], self.psb(1 + 2 * j)[:, :], AF.Copy,
                             [("ps", 1 + 2 * j)], [("coef", sbi, a8)])

        def flushD():
            pass

        def load_chunk(ec):
            b = ec % NB
            S.dma("sp", ub[b][:], self.uT_b.ap()[ec], r=[("uTb", i) for i in range(16)], w=[("ub", b)])
            S.dma("sp", vb[b][:], self.v_b.ap()[ec * EC:(ec + 1) * EC, :].rearrange("(s p) d -> p s d", p=128),
                  r=[("vb", (ec * EC) // 1024)], w=[("vbuf", b)])

        def act_mm(sbi, ec):
            b = ec % NB
            pa = self.psb(0)[:, (ec % 2) * EC:(ec % 2 + 1) * EC]
            for k in range(KC):
                self.mm(pa, h2t[sbi][:, k, :], ub[b][:, k, :], k == 0, k == KC - 1, [("h2tc", sbi), ("ub", b)], [("ps", 0)])

        seq = [(tt, ec) for tt in range(16) for ec in range(NEC)]
        PF = NB - 1
        load_tile(0)
        load_scores(0)
        for h in range(8):
            topk(0, h)
        for a8 in range(16):
            for h in range(8):
                coef_step(0, a8, h)
        load_scores(1)
        for i in range(PF):
            load_chunk(seq[i][1])
        act_mm(0, 0)
        for si, (tt, ec) in enumerate(seq):
            sbi = tt % 2
            nxt = (tt + 1) % 2
            if ec == 0 and tt + 1 < 16:
                load_tile(tt + 1)
            if si + PF < len(seq):
                load_chunk(seq[si + PF][1])
            b = ec % NB
            p2 = ec % 2
            pa = self.psb(0)[:, p2 * EC:(p2 + 1) * EC]
            self.ACT(Gb[p2][:], pa, AF.Gelu_apprx_tanh, [("ps", 0)], [("Gb", p2)])
            if si + 1 < len(seq):
                act_mm(seq[si + 1][0] % 2, seq[si + 1][1])
            self.DVE("tensor_tensor", [("Gb", p2), ("coef", sbi, (ec * EC) // 1024)], [("Wb", p2)], out=Wb[p2][:], in0=Gb[p2][:],
                     in1=coef[sbi][:, ec * EC:(ec + 1) * EC], op=ALU.mult)
            pt = self.psb(2, BF16)[:, p2 * EC:(p2 + 1) * EC]
            nsub = EC // 128
            for i in range(nsub):
                self.tr(pt[:, i * 128:(i + 1) * 128], Wb[p2][:, i * 128:(i + 1) * 128], self.ident_b[:], [("Wb", p2), "ident_b"], [("ps", 2)])
            self.ACT(WT[p2][:].rearrange("p a b -> p (a b)"), pt, AF.Copy, [("ps", 2)], [("WT", p2)])
            for i in range(nsub):
                for dc in range(4):
                    self.mm(self.psb(4 + dc)[:, :], WT[p2][:, i, :], vb[b][:, i, dc * 512:(dc + 1) * 512],
                            ec == 0 and i == 0, ec == NEC - 1 and i == nsub - 1, [("WT", p2), ("vbuf", b)], [("ps", 4 + dc)])
            if tt + 1 < 16:
                if ec == 0:
                    for h_ in range(8):
                        topk(nxt, h_)
                for idx in (2 * ec, 2 * ec + 1):
                    coef_step(nxt, idx // 8, idx % 8)
            if ec == NEC - 1:
                flushD()
                if tt + 2 < 16:
                    load_scores(tt + 2)
                for dc in range(4):
                    dsl = slice(dc * 512, (dc + 1) * 512)
                    S.dma("sp", x1c[:], self.y.ap()[tt * 128:(tt + 1) * 128, dsl], r=[("y", tt)], w=["x1c"])
                    self.DVE("tensor_tensor", [("ps", 4 + dc), "rowb"], ["ptf"], out=tf[:], in0=g2b[:, dsl], in1=self.psb(4 + dc)[:, :], op=ALU.mult)
                    self.DVE("tensor_tensor", ["ptf", "x1c"], ["x1c"], out=x1c[:], in0=x1c[:], in1=tf[:], op=ALU.add)
                    S.dma("sp", self.y.ap()[tt * 128:(tt + 1) * 128, dsl], x1c[:], r=["x1c"], w=[("y2", tt, dc)])
        self.release(m0)

    def finish(self):
        S = self.S
        S.add("sp", lambda e: e.nop(), extra_deps=[o.idx for o in S.ops if o.isdma])
        with ExitStack() as st:
            S.emit(st)
        return self.nc


def build(stop_after=None, dbg=()):
    B = Builder(stop_after, dbg)
    B.phase0()
    if stop_after != 1 and "nopeer" not in dbg:
        B.peer_precast()
    B.phase1()
    if stop_after != 1:
        if "skip2" not in dbg:
            B.phase2()
        else:
            B.w_in = B.din("w_in", [D, IN_COLS])
            B.Pbuf = [B.sb("P%d" % i, [128, 512], BF16) for i in range(3)]
            B.P_i = 0
            B.nb_kf = B.sb("nb_kf", [128, 512], F32)
            B.nb_sq = B.sb("nb_sq", [128, 512], BF16)
            B.nb_rs = B.sb("nb_rs", [128, 512], F32)
        B.phase3()
        B.phase4()
        if "nopeer" not in dbg:
            B.phase5()
    B.finish()
    return B


def _t5_bucket(n):
    n = np.maximum(n, 0)
    nf = np.maximum(n, 16).astype(np.float32)
    large = 16 + (np.log(nf / np.float32(16)) / np.float32(math.log(128.0)) * np.float32(16)).astype(np.int32)
    large = np.minimum(large, 31)
    return np.where(n < 16, n, large).astype(np.int64)


def _gather_strip(tbl_row, dist, valid):
    tb = np.concatenate([np.array([NEGB], np.float32), tbl_row.astype(np.float32)])
    idx = np.where(valid, _t5_bucket(dist) + 1, 0)
    return tb[idx]


def host_shared(inputs):
    m = {}
    rb = inputs["rel_bias"]
    p = np.arange(128)[:, None]
    strips = np.zeros((8, 128, 8064), np.float32)
    j = np.arange(2560)[None, :]
    r_sel = j - p - 384
    j2 = np.arange(1408)[None, :]
    r_win = j2 - p - 384
    j3 = np.arange(4096)[None, :]
    r_cmp = j3 - 16 * p - 31
    for h in range(8):
        strips[h, :, 0:2560] = _gather_strip(rb[h], r_sel, r_sel >= 0)
        strips[h, :, 2560:3968] = _gather_strip(rb[h], r_win, (r_win >= 0) & (r_win <= 511))
        strips[h, :, 3968:8064] = _gather_strip(rb[h], r_cmp, r_cmp >= 0)
    m["nsa_strips"] = strips
    dstrips = np.zeros((12, 128, 1024), np.float32)
    j4 = np.arange(1024)[None, :]
    r_d = j4 - p - 384
    for gi, (win, dil) in enumerate(DIL_CFG):
        for ps_ in range(4):
            hd = gi * 4 + ps_
            dstrips[hd] = _gather_strip(rb[8 + hd], r_d * dil, (r_d >= 0) & (r_d <= win // dil))
    m["dil_strips"] = dstrips
    c = (np.arange(2)[None, :, None] * 128 + np.arange(128)[:, None, None])
    jj = np.arange(64)[None, None, :]
    m["ovl"] = (((16 * c) < (64 * jj + 64)) & ((16 * c + 32) > (64 * jj))).astype(np.float32)
    jrow = np.arange(64)[:, None, None]
    kt = np.arange(32)[None, :, None]
    pp = np.arange(128)[None, None, :]
    m["e_all"] = (jrow == (2 * kt + pp // 64)).astype(np.float32)
    m["selg"] = np.ascontiguousarray(np.broadcast_to(np.eye(24, dtype=np.float32)[:, :, None], (24, 24, 128)))
    m["w_ada"] = inputs["w_ada"][0]
    m["b_ada_fm"] = np.ascontiguousarray(inputs["b_ada"][0].reshape(96, 128).T)
    m["norm1_g_fm"] = np.ascontiguousarray(inputs["norm1_g"][0].reshape(16, 128).T)
    m["norm2_g_fm"] = np.ascontiguousarray(inputs["norm2_g"][0].reshape(16, 128).T)
    m["w_in"] = inputs["w_in"][0]
    m["nsa_q_gain_fm"] = np.ascontiguousarray(inputs["nsa_q_gain"][0].reshape(128, 1))
    m["nsa_k_gain_fm"] = np.ascontiguousarray(inputs["nsa_k_gain"][0].T)
    m["cmp_posT"] = np.ascontiguousarray(inputs["cmp_pos"][0].transpose(2, 0, 1))
    m["cmp_w1"] = inputs["cmp_w1"][0]
    m["cmp_w2"] = inputs["cmp_w2"][0]
    m["dil_q_gain_fm"] = np.ascontiguousarray(inputs["dil_q_gain"][0].reshape(128, 1))
    m["dil_k_gain_fm"] = np.ascontiguousarray(inputs["dil_k_gain"][0].reshape(128, 1))
    m["w_br_nsa"] = inputs["w_br_nsa"][0]
    m["w_br_dil"] = inputs["w_br_dil"][0]
    m["w_out"] = inputs["w_out"][0]
    m["peer_w_q"] = inputs["peer_w_q"][0]
    m["peer_q_gain_row"] = np.ascontiguousarray(inputs["peer_q_gain"][0].reshape(1, 2048))
    m["peer_skT"] = np.ascontiguousarray(inputs["peer_sub_keys"][0].reshape(16, 128, 128).transpose(2, 0, 1))
    m["peer_uT"] = np.ascontiguousarray(inputs["peer_u"][0].T)
    m["peer_v"] = inputs["peer_v"][0]
    for hf in range(2):
        qi = np.arange(NOWN)
        cur = (qi + NOWN * hf) // 64
        jl = np.arange(64)[None, :]
        jg = jl - 32 + 32 * hf
        curc = cur[:, None]
        forced = (jg == 0) | (jg == curc) | (jg == curc - 1)
        bad = (jg > curc) | (jg < 0)
        add = np.where(forced & ~(jg < 0), 1e4, np.where(bad, -1e4, 0.0)).astype(np.float32)
        mult = np.where((forced & ~(jg < 0)) | bad, 0.0, 1.0).astype(np.float32)
        m["sel_add%d" % hf] = np.ascontiguousarray(add.reshape(16, 128, 64).transpose(1, 0, 2))
        m["sel_mult%d" % hf] = np.ascontiguousarray(mult.reshape(16, 128, 64).transpose(1, 0, 2))
    return m


def host_inputs(inputs, core, shared=None):
    if shared is None:
        shared = host_shared(inputs)
    b, hf = core // 2, core % 2
    x = inputs["x"]
    m = dict(shared)
    xl = np.zeros((NLOC, D), np.float32)
    if hf == 1:
        xl[:] = x[b]
    else:
        xl[NOWN:] = x[b, :NOWN]
    m["x_loc"] = xl
    m["pfx_bias"] = np.full((128, 1), 0.0 if hf == 1 else NEGB, np.float32)
    m["c_fm"] = np.ascontiguousarray(inputs["c"][b].reshape(16, 128).T)
    m["sel_add"] = shared["sel_add%d" % hf]
    m["sel_mult"] = shared["sel_mult%d" % hf]
    return m


def kernel(**inputs):
    inputs = {k: np.asarray(v) for k, v in inputs.items()}
    B = build()
    shared = host_shared(inputs)
    in_maps = []
    for c in range(8):
        hm = host_inputs(inputs, c, shared)
        in_maps.append({k: hm[k] for k in B.inputs})
    res = run_bass_kernel_spmd(B.nc, in_maps, core_ids=list(range(8)))
    out = np.zeros((4, 4096, D), np.float32)
    for c in range(8):
        out[c // 2, (c % 2) * NOWN:(c % 2 + 1) * NOWN] = res.results[c]["y"]
    return out
```

```python
import math
from contextlib import ExitStack
import numpy as np
import concourse.bass as bass
import concourse.mybir as mybir
from concourse.bass_utils import run_bass_kernel_spmd

F32 = mybir.dt.float32
BF16 = mybir.dt.bfloat16
I32 = mybir.dt.int32
U32 = mybir.dt.uint32
AF = mybir.ActivationFunctionType
ALU = mybir.AluOpType
AX = mybir.AxisListType

D = 2048
KC = 16
NLOC = 4096
NOWN = 2048
DH = 128
OFF_KV = 1024
OFF_GATE = OFF_KV + 1536
OFF_DIL = OFF_GATE + 24
OFF_MERGE = OFF_DIL + 4608
IN_COLS = OFF_MERGE + 4096
DIL_CFG = ((128, 1), (512, 4), (2048, 16))
EPS = 1e-6
NEGB = -30000.0
SCALE = DH ** -0.5
SB_BASE = 16640
SB_END = 229344


class Op:
    __slots__ = ("eng", "fn", "deps", "sig", "need", "isdma", "idx")


class Sched:
    COMPUTE = ("pe", "act", "dve", "pool")
    RING = 8

    def __init__(self, nc, same_engine_sync=True):
        self.nc = nc
        self.ops = []
        self.last_w = {}
        self.readers = {}
        self.ses = same_engine_sync
        self.last_on = {}
        self.dma_pend = []

    def add(self, eng, fn, r=(), w=(), dma=False, extra_deps=()):
        op = Op()
        op.eng = eng; op.fn = fn; op.isdma = dma; op.need = dma; op.sig = None
        op.idx = len(self.ops)
        deps = set(extra_deps)
        for k in r:
            lw = self.last_w.get(k)
            if lw is not None:
                deps.add(lw)
        for k in w:
            lw = self.last_w.get(k)
            if lw is not None:
                deps.add(lw)
            for rd in self.readers.get(k, ()):
                deps.add(rd)
        deps.discard(op.idx)
        fd = []
        for d in deps:
            o = self.ops[d]
            if (not o.isdma) and o.eng == eng and (eng == "pe" or not self.ses):
                continue
            o.need = True
            fd.append(d)
        op.deps = sorted(fd)
        self.ops.append(op)
        for k in w:
            self.last_w[k] = op.idx
            self.readers[k] = []
        for k in r:
            lst = self.readers.setdefault(k, [])
            if not dma:
                lst[:] = [x for x in lst if self.ops[x].isdma or self.ops[x].eng != eng]
            lst.append(op.idx)
        if not dma:
            self.last_on[eng] = op.idx
        else:
            self.dma_pend.append(op.idx)
        return op.idx

    def dma(self, eng, out, in_, r=(), w=(), **kw):
        return self.add(eng, lambda e: e.dma_start(out=out, in_=in_, **kw), r=r, w=w, dma=True)

    def barrier(self):
        lasts = dict(self.last_on)
        engs = ["pe", "act", "dve", "pool", "sp"]
        deps = list(lasts.values()) + list(self.dma_pend)
        for e in engs:
            self.add(e, lambda en: en.nop(), extra_deps=[d for d in deps])
        self.dma_pend = []
        self.last_w = {}
        self.readers = {}

    def emit(self, stack):
        nc = self.nc
        engs = sorted({o.eng for o in self.ops})
        csem = {}
        for e in engs:
            csem[e] = stack.enter_context(nc.semaphore("c_" + e))
        dring = {}
        for e in engs:
            if any(o.isdma and o.eng == e for o in self.ops):
                dring[e] = [stack.enter_context(nc.semaphore("d_%s%d" % (e, i))) for i in range(self.RING)]
        ccount = {e: 0 for e in engs}
        dcount = {e: 0 for e in engs}
        ringtot = {e: [0] * self.RING for e in engs}
        pre = {}
        for o in self.ops:
            if o.isdma:
                n = dcount[o.eng]; dcount[o.eng] += 1
                slot = n % self.RING
                prev = ringtot[o.eng][slot]
                ringtot[o.eng][slot] += 16
                pre[o.idx] = (dring[o.eng][slot], prev)
                o.sig = (dring[o.eng][slot], prev + 16, 16)
            elif o.need:
                ccount[o.eng] += 1
                o.sig = (csem[o.eng], ccount[o.eng], 1)
        per = {e: [o for o in self.ops if o.eng == e] for e in engs}
        self.stats = {e: (len(per[e]), ccount[e], dcount[e]) for e in engs}
        ops = self.ops
        blk = stack.enter_context(nc.Block())

        def runner(e):
            def body(engine):
                seen = {}
                for o in per[e]:
                    waits = []
                    if o.isdma:
                        s, v = pre[o.idx]
                        if v > 0:
                            waits.append((s, v))
                    for d in o.deps:
                        s, v, _ = ops[d].sig
                        waits.append((s, v))
                    for s, v in waits:
                        key = id(s)
                        if seen.get(key, 0) < v:
                            engine.wait_ge(s, v)
                            seen[key] = v
                    ins = o.fn(engine)
                    if o.sig is not None:
                        ins.then_inc(o.sig[0], o.sig[2])
            return body

        for e in engs:
            reg = {"pe": blk.tensor, "act": blk.scalar, "dve": blk.vector,
                   "pool": blk.gpsimd, "sp": blk.sync}[e]
            reg(runner(e))


def _dtsize(dt):
    return 2 if dt == BF16 else 4


class Builder:
    def __init__(self, stop_after=None, dbg=()):
        self.nc = bass.Bass("TRN2", target_bir_lowering=False)
        self.S = Sched(self.nc)
        self.off = SB_BASE
        self.cnt = 0
        self.stop_after = stop_after
        self.dbg = dbg
        self.inputs = {}
        self.outs = {}
        nc = self.nc
        self.psf = [nc.alloc_psum_tensor("psb%d" % i, [128, 512], F32) for i in range(8)]

    def sb(self, name, shape, dt):
        nbytes = int(np.prod(shape[1:])) * _dtsize(dt)
        self.cnt += 1
        t = self.nc.alloc_sbuf_tensor_at("%s_%d" % (name, self.cnt), list(shape), dt, offset=self.off)
        self.off += (nbytes + 63) // 64 * 64
        self.maxoff = max(getattr(self, "maxoff", 0), self.off)
        assert self.off <= SB_END, ("SBUF overflow", name, self.off)
        return t

    def mark(self):
        return self.off

    def release(self, m):
        self.S.barrier()
        self.off = m

    def din(self, name, shape, dt=F32):
        t = self.nc.dram_tensor(name, list(shape), dt, kind="ExternalInput")
        self.inputs[name] = t
        return t

    def dout(self, name, shape, dt=F32):
        t = self.nc.dram_tensor(name, list(shape), dt, kind="ExternalOutput")
        self.outs[name] = t
        return t

    def dscratch(self, name, shape, dt):
        return self.nc.dram_tensor(name, list(shape), dt)

    def psb(self, i, dt=F32):
        a = self.psf[i][:]
        if dt == BF16:
            a = a.bitcast(BF16)
        return a

    def PE(self, fn, r, w):
        return self.S.add("pe", fn, r=r, w=w)

    def mm(self, out, lhsT, rhs, start, stop, r, w):
        return self.S.add("pe", lambda e: e.matmul(out, lhsT=lhsT, rhs=rhs, start=start, stop=stop), r=r, w=w)

    def tr(self, out, in_, ident, r, w):
        return self.S.add("pe", lambda e: e.transpose(out, in_, ident), r=r, w=w)

    def ACT(self, out, in_, func, r, w, **kw):
        return self.S.add("act", lambda e: e.activation(out=out, in_=in_, func=func, **kw), r=r, w=w)

    def V(self, eng, name, r, w, *a, **kw):
        return self.S.add(eng, lambda e: getattr(e, name)(*a, **kw), r=r, w=w)

    def DVE(self, name, r, w, *a, **kw):
        return self.V("dve", name, r, w, *a, **kw)

    def POOL(self, name, r, w, *a, **kw):
        return self.V("pool", name, r, w, *a, **kw)

    def dump(self, name, src_tensor, shape, dt, rkeys):
        o = self.dout("dbg_" + name, shape, dt)
        self.S.dma("sp", o.ap(), src_tensor[:], r=rkeys)

    def phase0(self):
        S = self.S
        self.ident_f = self.sb("ident_f", [128, 128], F32)
        self.ident_b = self.sb("ident_b", [128, 128], BF16)
        self.ones_b = self.sb("ones_b", [128, 128], BF16)
        self.ones_f = self.sb("ones_f", [128, 128], F32)
        self.zero1 = self.sb("zero1", [128, 1], F32)
        self.pfxb = self.sb("pfxb", [128, 1], F32)
        idf = self.ident_f
        self.POOL("memset", [], ["ident_f"], idf[:], 0.0)
        S.add("pool", lambda e: e.affine_select(out=idf[:], in_=idf[:], pattern=[[-1, 128]],
                                                compare_op=ALU.not_equal, fill=1.0, base=0,
                                                channel_multiplier=1), r=["ident_f"], w=["ident_f"])
        self.DVE("tensor_copy", ["ident_f"], ["ident_b"], out=self.ident_b[:], in_=idf[:])
        self.POOL("memset", [], ["ones_b"], self.ones_b[:], 1.0)
        self.POOL("memset", [], ["ones_f"], self.ones_f[:], 1.0)
        self.POOL("memset", [], ["zero1"], self.zero1[:], 0.0)
        pf = self.din("pfx_bias", [128, 1])
        S.dma("sp", self.pfxb[:], pf.ap(), w=["pfxb"])

        c_in = self.din("c_fm", [128, 16])
        w_ada = self.din("w_ada", [D, 6 * D])
        b_ada = self.din("b_ada_fm", [128, 96])
        n1g = self.din("norm1_g_fm", [128, 16])
        n2g = self.din("norm2_g_fm", [128, 16])
        self.modT = self.sb("modT", [128, 96], F32)
        self.A1 = self.sb("A1", [128, 16], F32)
        self.A2 = self.sb("A2", [128, 16], F32)
        m = self.mark()
        cT = self.sb("cT", [128, 16], F32)
        sg = self.sb("sg", [128, 16], F32)
        condT = self.sb("condT", [128, 16], F32)
        bT = self.sb("bT", [128, 96], F32)
        g1T = self.sb("g1T", [128, 16], F32)
        g2T = self.sb("g2T", [128, 16], F32)
        wa = [self.sb("wa%d" % i, [128, 6 * D], BF16) for i in range(3)]
        condB = self.sb("condB", [128, 16], BF16)
        S.dma("sp", cT[:], c_in.ap(), w=["cT"])
        S.dma("sp", bT[:], b_ada.ap(), w=["bT"])
        S.dma("sp", g1T[:], n1g.ap(), w=["g1T"])
        S.dma("sp", g2T[:], n2g.ap(), w=["g2T"])
        self.ACT(condT[:], cT[:], AF.Silu, ["cT"], ["condT"])
        self.DVE("tensor_copy", ["condT"], ["condB"], out=condB[:], in_=condT[:])
        ps = self.psb(0)
        for k in range(KC):
            wk = wa[k % 3]
            key = "wa%d" % (k % 3)
            for hh in range(4):
                cs = slice(hh * 3072, (hh + 1) * 3072)
                S.dma("pool", wk[:, cs], w_ada.ap()[k * 128:(k + 1) * 128, cs], w=[key + "_%d" % hh])
            for j in range(96):
                self.mm(ps[:, j:j + 1], wk[:, j * 128:(j + 1) * 128], condB[:, k:k + 1], True, True,
                        [key + "_%d" % (j // 24), "condB"], [("ps", 0)])
            if k == 0:
                self.DVE("tensor_copy", [("ps", 0)], ["modT"], out=self.modT[:], in_=ps[:, 0:96])
            else:
                self.DVE("tensor_tensor", [("ps", 0), "modT"], ["modT"], out=self.modT[:], in0=self.modT[:],
                         in1=ps[:, 0:96], op=ALU.add)
        self.DVE("tensor_tensor", ["modT", "bT"], ["modT"], out=self.modT[:], in0=self.modT[:], in1=bT[:], op=ALU.add)
        self.DVE("scalar_tensor_tensor", ["modT", "g1T"], ["A1"], out=self.A1[:], in0=self.modT[:, 16:32],
                 scalar=1.0, in1=g1T[:], op0=ALU.add, op1=ALU.mult)
        self.DVE("scalar_tensor_tensor", ["modT", "g2T"], ["A2"], out=self.A2[:], in0=self.modT[:, 64:80],
                 scalar=1.0, in1=g2T[:], op0=ALU.add, op1=ALU.mult)
        if "mod" in self.dbg:
            self.dump("mod", self.modT, [128, 96], F32, ["modT"])
        self.release(m)

    def phase1(self):
        S = self.S
        x_in = self.din("x_loc", [NLOC, D])
        self.x_in = x_in
        self.hT_d = self.dscratch("hT_pfx", [KC, 128, NOWN], BF16)
        self.m_hT = self.mark()
        self.hT = self.sb("hT", [128, KC, NOWN], BF16)
        m = self.mark()
        xt = [self.sb("xt%d" % i, [128, D], F32) for i in range(2)]
        xn = [self.sb("xn%d" % i, [128, D], BF16) for i in range(2)]
        junk = self.sb("junk", [128, D], BF16)
        st = self.sb("st", [128, 3 * 32], F32)
        hg = [self.sb("hg%d" % i, [128, KC, 512], BF16) for i in range(2)]
        def stage1(tt):
            b = tt % 2
            S.dma("sp", xt[b][:], x_in.ap()[tt * 128:(tt + 1) * 128, :], w=["xt%d" % b])
            self.ACT(junk[:], xt[b][:], AF.Square, ["xt%d" % b], ["junk", ("st", tt)], accum_out=st[:, tt:tt + 1])
            self.ACT(st[:, 32 + tt:33 + tt], st[:, tt:tt + 1], AF.Sqrt, [("st", tt)], [("st", tt)],
                     scale=1.0 / D, bias=EPS)
            self.DVE("reciprocal", [("st", tt)], [("st", tt)], out=st[:, 64 + tt:65 + tt], in_=st[:, 32 + tt:33 + tt])
            self.DVE("tensor_scalar", ["xt%d" % b, ("st", tt)], ["xn%d" % b], out=xn[b][:], in0=xt[b][:],
                     scalar1=st[:, 64 + tt:65 + tt], scalar2=None, op0=ALU.mult)

        def stage2(tt):
            g, i = tt // 4, tt % 4
            b = tt % 2
            for half in range(2):
                pst = self.psb(half, BF16)
                for kk in range(8):
                    k = half * 8 + kk
                    self.tr(pst[:, kk * 128:(kk + 1) * 128], xn[b][:, k * 128:(k + 1) * 128], self.ident_b[:],
                            ["xn%d" % b, "ident_b"], [("ps", half)])
                for kk in range(8):
                    k = half * 8 + kk
                    if g < 4:
                        dst = hg[g % 2][:, k, i * 128:(i + 1) * 128]
                        wkey = [("hg", g % 2, i)]
                    else:
                        dst = self.hT[:, k, (g - 4) * 512 + i * 128:(g - 4) * 512 + (i + 1) * 128]
                        wkey = [("hT", g - 4)]
                    src = pst[:, kk * 128:(kk + 1) * 128]
                    if half == 0:
                        self.DVE("tensor_scalar", [("ps", half), "A1", "modT"], wkey, out=dst, in0=src,
                                 scalar1=self.A1[:, k:k + 1], scalar2=self.modT[:, k:k + 1], op0=ALU.mult, op1=ALU.add)
                    else:
                        self.ACT(dst, src, AF.Identity, [("ps", half), "A1", "modT"], wkey,
                                 scale=self.A1[:, k:k + 1], bias=self.modT[:, k:k + 1])
            if g < 4 and i == 3:
                S.dma("sp", self.hT_d.ap().rearrange("k p t -> p k t")[:, :, g * 512:(g + 1) * 512], hg[g % 2][:],
                      r=[("hg", g % 2, ii) for ii in range(4)], w=[("hTd", g)])

        stage1(0)
        for tt in range(32):
            if tt + 1 < 32:
                stage1(tt + 1)
            stage2(tt)
        if "hT" in self.dbg:
            self.dump("hT", self.hT, [128, KC, NOWN], BF16, [("hT", q) for q in range(4)])
        self.release(m)

    def load_w(self, eng, dst, col0, ncols, wkey):
        src = self.w_in.ap().rearrange("(k p) c -> p k c", p=128)[:, :, col0:col0 + ncols]
        self.S.dma("pool", dst, src, w=[wkey])

    def hchunk(self, tc):
        if tc >= 4:
            base = (tc - 4) * 512
            return (lambda k, lo=0, hi=512, st=1: self.hT[:, k, base + lo:base + hi:st]), [("hT", tc - 4)]
        b = self.pf_i % 2
        self.pf_i += 1
        buf = self.pfbuf[b]
        key = ("pfbuf", b)
        self.S.dma("sp", buf[:], self.hT_d.ap().rearrange("k p t -> p k t")[:, :, tc * 512:(tc + 1) * 512],
                   r=[("hTd", tc)], w=[key])
        return (lambda k, lo=0, hi=512, st=1: buf[:, k, lo:hi:st]), [key]

    def norm_fm(self, ps, pskey, dst, dkeys, gain_ap, gkey, n):
        kf, sq, rs = self.nb_kf, self.nb_sq, self.nb_rs
        self.ACT(kf[:, :n], ps, AF.Copy, [pskey], ["nb_kf"])
        self.ACT(sq[:, :n], ps, AF.Square, [pskey], ["nb_sq"])
        p7 = self.psb(7)
        self.mm(p7[:, :n], self.ones_b[:], sq[:, :n], True, True, ["ones_b", "nb_sq"], [("ps", 7)])
        self.ACT(rs[:, :n], p7[:, :n], AF.Sqrt, [("ps", 7)], ["nb_rs"], scale=1.0 / DH, bias=EPS)
        self.DVE("reciprocal", ["nb_rs"], ["nb_rs"], out=rs[:, :n], in_=rs[:, :n])
        self.DVE("scalar_tensor_tensor", ["nb_kf", "nb_rs", gkey], dkeys, out=dst, in0=kf[:, :n], scalar=gain_ap,
                 in1=rs[:, :n], op0=ALU.mult, op1=ALU.mult)

    def attn(self, tiles, q_rhs, qkeys, nq, ob, zb, imp=None):
        O = self.psb(ob)[:, :nq]
        Z = self.psb(zb)[:, :nq]
        n = len(tiles)

        def emit_S(i):
            t = tiles[i]
            sb_ = i % 2
            Sps = self.psb(sb_)[:, :nq]
            adds = t.get("adds", [])
            self.mm(Sps, t["kT"], q_rhs, True, len(adds) == 0, t["kkeys"] + qkeys, [("ps", sb_)])
            for ai, (l, r_, ks) in enumerate(adds):
                self.mm(Sps, l, r_, False, ai == len(adds) - 1, ks, [("ps", sb_)])

        emit_S(0)
        for i in range(n):
            if i + 1 < n:
                emit_S(i + 1)
            t = tiles[i]
            pb = self.P_i % 3
            self.P_i += 1
            P = self.Pbuf[pb][:, :nq]
            self.ACT(P, self.psb(i % 2)[:, :nq], AF.Exp, [("ps", i % 2), t["bkey"]], [("P", pb)], bias=t["bias"])
            self.mm(O, t["v"], P, i == 0, i == n - 1, t["vkeys"] + [("P", pb)], [("ps", ob)])
            self.mm(Z, self.ones_b[:], P, i == 0, i == n - 1, ["ones_b", ("P", pb)], [("ps", zb)])
            if imp is not None:
                lfn, ib = imp
                self.mm(self.psb(ib)[0:64, :nq], lfn(i), P, i == 0, i == n - 1, ["ovl", ("P", pb)], [("ps", ib)])

    def phase2(self):
        S = self.S
        self.w_in = self.din("w_in", [D, IN_COLS])
        gq = self.din("nsa_q_gain_fm", [128, 1])
        gk = self.din("nsa_k_gain_fm", [128, 3])
        posT_in = self.din("cmp_posT", [128, 2, 32])
        w1_in = self.din("cmp_w1", [2, 4096, 128])
        w2_in = self.din("cmp_w2", [2, 128, 128])
        strips_in = self.din("nsa_strips", [8, 128, 8064])
        ovl_in = self.din("ovl", [128, 2, 64])
        eall_in = self.din("e_all", [64, 32, 128])
        selg_in = self.din("selg", [24, 24, 128])
        mmask_in = self.din("sel_mult", [128, 16, 64])
        amask_in = self.din("sel_add", [128, 16, 64])
        self.ynsa_d = self.dscratch("ynsa_d", [8, 128, NOWN], BF16)

        self.Pbuf = [self.sb("P%d" % i, [128, 512], BF16) for i in range(3)]
        self.P_i = 0
        self.nb_kf = self.sb("nb_kf", [128, 512], F32)
        self.nb_sq = self.sb("nb_sq", [128, 512], BF16)
        self.nb_rs = self.sb("nb_rs", [128, 512], F32)
        m = self.mark()
        gqs = self.sb("gqs", [128, 1], F32)
        gkt = self.sb("gkt", [128, 3], F32)
        S.dma("sp", gqs[:], gq.ap(), w=["gqs"])
        S.dma("sp", gkt[:], gk.ap(), w=["gkt"])
        self.DVE("tensor_scalar", ["gqs"], ["gqs"], out=gqs[:], in0=gqs[:], scalar1=SCALE, scalar2=None, op0=ALU.mult)
        posterm = self.sb("posterm", [128, 2], F32)
        gateT = self.sb("gateT", [24, NOWN], BF16)
        Wg = self.sb("Wg", [128, KC, 24], BF16)
        self.load_w("pool", Wg[:], OFF_GATE, 24, "Wg")
        for qc in range(4):
            p5 = self.psb(5)
            for k in range(KC):
                self.mm(p5[0:24, :], Wg[:, k, :], self.hT[:, k, qc * 512:(qc + 1) * 512], k == 0, k == KC - 1,
                        ["Wg", ("hT", qc)], [("ps", 5)])
            self.ACT(gateT[:, qc * 512:(qc + 1) * 512], p5[0:24, :], AF.Sigmoid, [("ps", 5)], [("gateT", qc)])

        for g in range(2):
            mg = self.mark()
            kselT = self.sb("kselT", [128, NLOC], BF16)
            vsel = self.sb("vsel", [128, 32, 128], BF16)
            kwinT = self.sb("kwinT", [128, 2560], BF16)
            vwin = self.sb("vwin", [128, 20, 128], BF16)
            qT = self.sb("qT", [128, 4, NOWN], BF16)
            kcT = self.sb("kcT", [128, 256], BF16)
            vc = self.sb("vc", [128, 2, 128], BF16)
            mA = self.mark()
            kcmpT = self.sb("kcmpT", [128, 2, NLOC], BF16)
            mB = self.mark()
            Wkv = self.sb("Wkv", [128, 6, KC, 128], BF16)
            self.pfbuf = [self.sb("pfbuf%d" % i, [128, KC, 512], BF16) for i in range(2)]
            self.pf_i = 0
            for i in range(6):
                self.load_w("pool", Wkv[:, i], OFF_KV + (i * 2 + g) * 128, 128, ("Wkv", i))
            for tc in range(8):
                self.pc(1)
                hf_, hkeys = self.hchunk(tc)
                for i in (0, 1, 2, 4):
                    if i == 4 and tc < 3:
                        continue
                    bk = 5 + (i // 2) % 2
                    p5 = self.psb(bk)
                    pk = ("ps", bk)
                    for k in range(KC):
                        self.mm(p5[:, :], Wkv[:, i, k, :], hf_(k), k == 0, k == KC - 1, [("Wkv", i)] + hkeys, [pk])
                    if i < 2:
                        self.ACT(kcmpT[:, i, tc * 512:(tc + 1) * 512], p5[:, :], AF.Copy, [pk], [("kcmpT", i)])
                    elif i == 2:
                        self.norm_fm(p5[:, :], pk, kselT[:, tc * 512:(tc + 1) * 512], [("kselT", tc)], gkt[:, 1:2], "gkt", 512)
                    else:
                        self.norm_fm(p5[:, :], pk, kwinT[:, (tc - 3) * 512:(tc - 2) * 512], [("kwinT", tc - 3)], gkt[:, 2:3], "gkt", 512)
                for ti in range(4):
                    tt = tc * 4 + ti
                    p6 = self.psb(6)
                    units = (3, 5) if tc >= 3 else (3,)
                    for ui, i in enumerate(units):
                        for k in range(KC):
                            self.mm(p6[:, ui * 128:(ui + 1) * 128], hf_(k, ti * 128, (ti + 1) * 128), Wkv[:, i, k, :],
                                    k == 0, k == KC - 1, [("Wkv", i)] + hkeys, [("ps", 6)])
                    self.DVE("tensor_copy", [("ps", 6)], [("vsel", tt)], out=vsel[:, tt, :], in_=p6[:, 0:128])
                    if tc >= 3:
                        self.DVE("tensor_copy", [("ps", 6)], [("vwin", tt - 12)], out=vwin[:, tt - 12, :], in_=p6[:, 128:256])
            self.release(mB)
            Wq = self.sb("Wq", [128, 4, KC, 128], BF16)
            for r in range(4):
                self.load_w("pool", Wq[:, r], (4 * g + r) * 128, 128, ("Wq", r))
            for tc in range(4, 8):
                hf_, hkeys = self.hchunk(tc)
                for r in range(4):
                    p5 = self.psb(5)
                    for k in range(KC):
                        self.mm(p5[:, :], Wq[:, r, k, :], hf_(k), k == 0, k == KC - 1, [("Wq", r)] + hkeys, [("ps", 5)])
                    self.norm_fm(p5[:, :], ("ps", 5), qT[:, r, (tc - 4) * 512:(tc - 3) * 512], [("qT", r, tc - 4)],
                                 gqs[:, 0:1], "gqs", 512)
            self.release(mB)
            posT = self.sb("posT", [128, 2, 32], BF16)
            W1 = self.sb("W1", [128, 2, 32, 128], BF16)
            W2 = self.sb("W2", [128, 2, 128], BF16)
            hid = self.sb("hid", [128, 2, 256], BF16)
            S.dma("pool", posT[:], posT_in.ap(), w=["posT"])
            for j in range(2):
                S.dma("pool", W1[:, j], w1_in.ap()[j].rearrange("(l d) o -> d l o", d=128), w=[("W1", j)])
                S.dma("pool", W2[:, j, :], w2_in.ap()[j], w=[("W2", j)])
            self.POOL("memset", [], [("hid", 0), ("hid", 1)], hid[:], 0.0)
            if g == 0:
                for j in range(2):
                    p5 = self.psb(5)
                    for l in range(32):
                        self.mm(p5[:, 0:1], W1[:, j, l, :], posT[:, j, l:l + 1], l == 0, l == 31, [("W1", j), "posT"], [("ps", 5)])
                    self.DVE("tensor_copy", [("ps", 5)], [("posterm", j)], out=posterm[:, j:j + 1], in_=p5[:, 0:1])
            for j in range(2):
                p5 = self.psb(5)
                for l in range(32):
                    self.mm(p5[:, 0:255], W1[:, j, l, :], kcmpT[:, j, l:l + 16 * 254 + 1:16], l == 0, l == 31,
                            [("W1", j), ("kcmpT", j)], [("ps", 5)])
                self.ACT(hid[:, j, 0:255], p5[:, 0:255], AF.Gelu_apprx_tanh, [("ps", 5), ("posterm", j)], [("hid", j)],
                         bias=posterm[:, j:j + 1])
            p5 = self.psb(5)
            self.mm(p5[:, 0:256], W2[:, 0, :], hid[:, 0, :], True, True, [("W2", 0), ("hid", 0)], [("ps", 5)])
            self.norm_fm(p5[:, 0:256], ("ps", 5), kcT[:, :], ["kcT"], gkt[:, 0:1], "gkt", 256)
            for ct in range(2):
                p6 = self.psb(6)
                self.mm(p6[:, 0:128], hid[:, 1, ct * 128:(ct + 1) * 128], W2[:, 1, :], True, True,
                        [("W2", 1), ("hid", 1)], [("ps", 6)])
                self.DVE("tensor_copy", [("ps", 6)], [("vc", ct)], out=vc[:, ct, :], in_=p6[:, 0:128])
            self.release(mA)

            ovl = self.sb("ovl", [128, 2, 64], BF16)
            eall = self.sb("eall", [64, 32, 128], BF16)
            selg = self.sb("selg", [24, 24, 128], BF16)
            mmask = self.sb("mmask", [128, 16, 64], F32)
            amask = self.sb("amask", [128, 16, 64], F32)
            S.dma("pool", ovl[:], ovl_in.ap(), w=["ovl"])
            S.dma("pool", eall[:], eall_in.ap(), w=["eall"])
            S.dma("pool", selg[:], selg_in.ap(), w=["selg"])
            S.dma("sp", mmask[:], mmask_in.ap(), w=["mmask"])
            S.dma("sp", amask[:], amask_in.ap(), w=["amask"])
            strip = self.sb("strip", [128, 8064], BF16)
            selbT = self.sb("selbT", [64, NOWN], BF16)
            ycmp = self.sb("ycmp", [128, 4, 512], F32)
            impacc = self.sb("impacc", [64, 512], F32)
            rz = self.sb("rz", [128, 512], F32)
            wgt = self.sb("wgt", [128, 512], F32)
            tmpf = self.sb("tmpf", [128, 512], F32)
            yacc = self.sb("yacc", [128, 512], F32)
            ybuf = [self.sb("ybuf%d" % i, [128, 512], BF16) for i in range(2)]
            sc = self.sb("sc", [128, 4, 64], F32)
            wk = self.sb("wk", [128, 64], F32)
            m8 = self.sb("m8", [128, 16], F32)
            selb = self.sb("selb", [128, 4, 64], BF16)
            yb_i = 0
            for qc in range(4):
                qb = NOWN + 512 * qc
                qsl = slice(qc * 512, (qc + 1) * 512)
                for r in range(4):
                    h = 4 * g + r
                    S.dma("pool", strip[:, 3968:8064], strips_in.ap()[h][:, 3968:8064], w=["strip_c"])
                    tiles = []
                    for ct in range(2):
                        c0 = qb - 2048 * ct
                        tiles.append(dict(kT=kcT[:, ct * 128:(ct + 1) * 128], kkeys=["kcT"], v=vc[:, ct, :], vkeys=[("vc", ct)],
                                          adds=[(self.ident_b[:], strip[:, 3968 + c0:3968 + c0 + 512], ["ident_b", "strip_c"])],
                                          bias=(self.pfxb[:, 0:1] if ct == 0 else self.zero1[:, 0:1]),
                                          bkey=("pfxb" if ct == 0 else "zero1")))
                    self.attn(tiles, qT[:, r, qsl], [("qT", r, qc)], 512, 2, 3, imp=(lambda i: ovl[:, i, :], 4))
                    O = self.psb(2); Z = self.psb(3); I = self.psb(4)
                    self.DVE("tensor_scalar", [("ps", 3)], ["rz"], out=rz[:], in0=Z[:, :], scalar1=1e-30, scalar2=None, op0=ALU.max)
                    self.DVE("reciprocal", ["rz"], ["rz"], out=rz[:], in_=rz[:])
                    self.DVE("tensor_tensor", [("ps", 2), "rz"], [("ycmp", r)], out=ycmp[:, r, :], in0=O[:, :], in1=rz[:], op=ALU.mult)
                    if r == 0:
                        self.DVE("tensor_tensor", [("ps", 4), "rz"], ["impacc"], out=impacc[:], in0=I[0:64, :], in1=rz[0:64, :], op=ALU.mult)
                    else:
                        self.DVE("tensor_tensor", [("ps", 4), "rz"], ["tmpf"], out=tmpf[0:64, :], in0=I[0:64, :], in1=rz[0:64, :], op=ALU.mult)
                        self.DVE("tensor_tensor", ["tmpf", "impacc"], ["impacc"], out=impacc[:], in0=impacc[:], in1=tmpf[0:64, :], op=ALU.add)
                p5 = self.psb(5)
                for i in range(4):
                    self.mm(p5[:, i * 64:(i + 1) * 64], impacc[:, i * 128:(i + 1) * 128], self.ident_f[0:64, 0:64], True, True,
                            ["impacc", "ident_f"], [("ps", 5)])
                scv = sc[:].rearrange("p a b -> p (a b)")
                self.DVE("tensor_tensor", [("ps", 5), "mmask"], ["sc"], out=scv, in0=p5[:, 0:256],
                         in1=mmask[:, qc * 4:(qc + 1) * 4, :].rearrange("p a b -> p (a b)"), op=ALU.mult)
                self.DVE("tensor_tensor", ["sc", "amask"], ["sc"], out=scv, in0=scv,
                         in1=amask[:, qc * 4:(qc + 1) * 4, :].rearrange("p a b -> p (a b)"), op=ALU.add)
                for i in range(4):
                    self.DVE("max", ["sc"], ["m8"], out=m8[:, 0:8], in_=sc[:, i, :])
                    self.DVE("match_replace", ["sc", "m8"], ["wk"], out=wk[:], in_to_replace=m8[:, 0:8], in_values=sc[:, i, :], imm_value=-1e30)
                    self.DVE("max", ["wk"], ["m8"], out=m8[:, 8:16], in_=wk[:])
                    self.DVE("tensor_scalar", ["sc", "m8"], [("selb", i)], out=selb[:, i, :], in0=sc[:, i, :], scalar1=m8[:, 15:16],
                             scalar2=NEGB, op0=ALU.is_lt, op1=ALU.mult)
                    p6 = self.psb(6, BF16)
                    self.tr(p6[0:64, 0:128], selb[:, i, :], self.ident_b[:], [("selb", i), "ident_b"], [("ps", 6)])
                    self.ACT(selbT[:, qc * 512 + i * 128:qc * 512 + (i + 1) * 128], p6[0:64, 0:128], AF.Copy, [("ps", 6)], [("selbT", qc)])
                for r in range(4):
                    h = 4 * g + r
                    self.pc(1)
                    S.dma("pool", strip[:, 0:3968], strips_in.ap()[h][:, 0:3968], w=["strip_sw"])
                    G = self.psb(4)
                    self.mm(G[:, :], selg[:, 3 * h + 0, :], gateT[:, qsl], True, True, ["selg", ("gateT", qc)], [("ps", 4)])
                    self.DVE("tensor_tensor", [("ps", 4), ("ycmp", r)], ["yacc"], out=yacc[:], in0=ycmp[:, r, :], in1=G[:, :], op=ALU.mult)
                    for br in (1, 2):
                        tiles = []
                        if br == 1:
                            kts = range(0, 16 + 4 * qc + 4)
                        else:
                            kts = range(16 + 4 * qc - 4, 16 + 4 * qc + 4)
                        for kt in kts:
                            dlt = qb - 128 * kt
                            bias = self.pfxb[:, 0:1] if kt < 16 else self.zero1[:, 0:1]
                            bkey = "pfxb" if kt < 16 else "zero1"
                            if br == 1:
                                c0 = min(dlt + 384, 2048)
                                adds = [(eall[:, kt, :], selbT[:, qsl], ["eall", ("selbT", qc)]),
                                        (self.ident_b[:], strip[:, c0:c0 + 512], ["ident_b", "strip_sw"])]
                                tiles.append(dict(kT=kselT[:, kt * 128:(kt + 1) * 128], kkeys=[("kselT", kt // 4)],
                                                  v=vsel[:, kt, :], vkeys=[("vsel", kt)], adds=adds, bias=bias, bkey=bkey))
                            else:
                                c0 = 2560 + dlt + 384
                                adds = [(self.ident_b[:], strip[:, c0:c0 + 512], ["ident_b", "strip_sw"])]
                                tiles.append(dict(kT=kwinT[:, (kt - 12) * 128:(kt - 11) * 128], kkeys=[("kwinT", (kt - 12) // 4)],
                                                  v=vwin[:, kt - 12, :], vkeys=[("vwin", kt - 12)], adds=adds, bias=bias, bkey=bkey))
                        self.attn(tiles, qT[:, r, qsl], [("qT", r, qc)], 512, 2, 3)
                        O = self.psb(2); Z = self.psb(3)
                        self.mm(G[:, :], selg[:, 3 * h + br, :], gateT[:, qsl], True, True, ["selg", ("gateT", qc)], [("ps", 4)])
                        self.DVE("reciprocal", [("ps", 3)], ["rz"], out=rz[:], in_=Z[:, :])
                        self.DVE("tensor_tensor", [("ps", 4), "rz"], ["wgt"], out=wgt[:], in0=rz[:], in1=G[:, :], op=ALU.mult)
                        self.DVE("tensor_tensor", [("ps", 2), "wgt"], ["tmpf"], out=tmpf[:], in0=wgt[:], in1=O[:, :], op=ALU.mult)
                        if br == 1:
                            self.DVE("tensor_tensor", ["tmpf", "yacc"], ["yacc"], out=yacc[:], in0=yacc[:], in1=tmpf[:], op=ALU.add)
                        else:
                            yb = yb_i % 2
                            yb_i += 1
                            self.DVE("tensor_tensor", ["tmpf", "yacc"], [("ybuf", yb)], out=ybuf[yb][:], in0=yacc[:], in1=tmpf[:], op=ALU.add)
                            S.dma("sp", self.ynsa_d.ap()[h][:, qsl], ybuf[yb][:], r=[("ybuf", yb)], w=[("ynsa_d", h, qc)])
            self.release(mg)
        if "ynsa" in self.dbg:
            o = self.dout("dbg_ynsa", [8, 128, NOWN], BF16)
            S.dma("sp", o.ap(), self.ynsa_d.ap())
        self.release(m)

    def phase3(self):
        S = self.S
        gq = self.din("dil_q_gain_fm", [128, 1])
        gk = self.din("dil_k_gain_fm", [128, 1])
        dstr_in = self.din("dil_strips", [12, 128, 1024])
        self.ydil_d = self.dscratch("ydil_d", [4, 128, NOWN], BF16)
        m = self.mark()
        hTp = self.sb("hTp", [128, KC, NOWN], BF16)
        for c in range(4):
            S.dma("sp", hTp[:, :, c * 512:(c + 1) * 512], self.hT_d.ap().rearrange("k p t -> p k t")[:, :, c * 512:(c + 1) * 512],
                  w=[("hTp", c)])
        gqs = self.sb("dgqs", [128, 1], F32)
        gks = self.sb("dgks", [128, 1], F32)
        S.dma("sp", gqs[:], gq.ap(), w=["dgqs"])
        S.dma("sp", gks[:], gk.ap(), w=["dgks"])
        self.DVE("tensor_scalar", ["dgqs"], ["dgqs"], out=gqs[:], in0=gqs[:], scalar1=SCALE, scalar2=None, op0=ALU.mult)
        dacc = self.sb("dacc", [128, NOWN], F32)
        zacc = self.sb("zacc", [128, NOWN], F32)
        ybuf = self.sb("dybuf", [128, NOWN], BF16)
        W3 = self.sb("W3", [128, 3, KC, 128], BF16)
        qT = self.sb("dqT", [128, NOWN], BF16)
        kT = self.sb("dkT", [128, NLOC], BF16)
        vt = self.sb("dvt", [128, 32, 128], BF16)
        strip = self.sb("dstrip", [128, 1024], BF16)
        for p in range(4):
            for gi, (win, dil) in enumerate(DIL_CFG):
                self.pc(1)
                hd = gi * 4 + p
                for i in range(3):
                    self.load_w("pool", W3[:, i], OFF_DIL + (i * 12 + hd) * 128, 128, ("W3", i))
                S.dma("pool", strip[:], dstr_in.ap()[hd], w=["dstrip"])
                for tc in range(4):
                    p5 = self.psb(5)
                    for k in range(KC):
                        self.mm(p5[:, :], W3[:, 0, k, :], self.hT[:, k, tc * 512:(tc + 1) * 512], k == 0, k == KC - 1,
                                [("W3", 0), ("hT", tc)], [("ps", 5)])
                    self.norm_fm(p5[:, :], ("ps", 5), qT[:, tc * 512:(tc + 1) * 512], [("dqT", tc)], gqs[:, 0:1], "dgqs", 512)
                    p6 = self.psb(6)
                    for k in range(KC):
                        self.mm(p6[:, :], W3[:, 1, k, :], self.hT[:, k, tc * 512:(tc + 1) * 512], k == 0, k == KC - 1,
                                [("W3", 1), ("hT", tc)], [("ps", 6)])
                    self.norm_fm(p6[:, :], ("ps", 6), kT[:, NOWN + tc * 512:NOWN + (tc + 1) * 512], ["dkT"], gks[:, 0:1], "dgks", 512)
                pl = 128 * dil
                pieces = []
                t0 = NOWN - pl
                while t0 < NOWN:
                    n = min(512, NOWN - t0)
                    pieces.append((t0, n))
                    t0 += n
                for (t0, n) in pieces:
                    p6 = self.psb(6)
                    for k in range(KC):
                        self.mm(p6[:, :n], W3[:, 1, k, :], hTp[:, k, t0:t0 + n], k == 0, k == KC - 1,
                                [("W3", 1), ("hTp", t0 // 512)], [("ps", 6)])
                    self.norm_fm(p6[:, :n], ("ps", 6), kT[:, t0:t0 + n], ["dkT"], gks[:, 0:1], "dgks", n)
                ntile = 16 // dil + 1
                for r in range(dil):
                    for mt in range(ntile):
                        p6 = self.psb(6)
                        if mt == 0:
                            st = NOWN - pl + r
                            src = lambda k: hTp[:, k, st:st + dil * 127 + 1:dil]
                            keys = [("hTp", c) for c in range((NOWN - pl) // 512, 4)]
                        else:
                            st = r + dil * 128 * (mt - 1)
                            src = lambda k: self.hT[:, k, st:st + dil * 127 + 1:dil]
                            keys = [("hT", c) for c in range(st // 512, (st + dil * 127) // 512 + 1)]
                        for k in range(KC):
                            self.mm(p6[:, 0:128], src(k), W3[:, 2, k, :], k == 0, k == KC - 1, [("W3", 2)] + keys, [("ps", 6)])
                        self.DVE("tensor_copy", [("ps", 6)], [("dvt", r * ntile + mt)], out=vt[:, r * ntile + mt, :], in_=p6[:, 0:128])
                nq = min(512, NOWN // dil)
                nch = (NOWN // dil) // nq
                for r in range(dil):
                    for ci in range(nch):
                        qs = r + dil * ci * nq
                        q_rhs = qT[:, qs:qs + dil * (nq - 1) + 1:dil]
                        tiles = []
                        for mt in range(ci * nq // 128, ci * nq // 128 + nq // 128 + 1):
                            delta = (128 + ci * nq) - 128 * mt
                            c0 = delta + 384
                            ks = NOWN - pl + r + dil * 128 * mt
                            tiles.append(dict(kT=kT[:, ks:ks + dil * 127 + 1:dil], kkeys=["dkT"], v=vt[:, r * ntile + mt, :],
                                              vkeys=[("dvt", r * ntile + mt)],
                                              adds=[(self.ident_b[:], strip[:, c0:c0 + nq], ["ident_b", "dstrip"])],
                                              bias=(self.pfxb[:, 0:1] if mt == 0 else self.zero1[:, 0:1]),
                                              bkey=("pfxb" if mt == 0 else "zero1")))
                        self.attn(tiles, q_rhs, [("dqT", c) for c in range(4)], nq, 2, 3)
                        O = self.psb(2)[:, :nq]; Z = self.psb(3)[:, :nq]
                        dsl = slice(qs, qs + dil * (nq - 1) + 1, dil)
                        if gi == 0:
                            self.DVE("tensor_copy", [("ps", 2)], ["dacc"], out=dacc[:, dsl], in_=O)
                            self.DVE("tensor_copy", [("ps", 3)], ["zacc"], out=zacc[:, dsl], in_=Z)
                        else:
                            self.DVE("tensor_tensor", [("ps", 2), "dacc"], ["dacc"], out=dacc[:, dsl], in0=dacc[:, dsl], in1=O, op=ALU.add)
                            self.DVE("tensor_tensor", [("ps", 3), "zacc"], ["zacc"], out=zacc[:, dsl], in0=zacc[:, dsl], in1=Z, op=ALU.add)
            self.DVE("reciprocal", ["zacc"], ["zacc"], out=zacc[:], in_=zacc[:])
            self.DVE("tensor_tensor", ["zacc", "dacc"], ["dybuf"], out=ybuf[:], in0=dacc[:], in1=zacc[:], op=ALU.mult)
            S.dma("sp", self.ydil_d.ap()[p], ybuf[:], r=["dybuf"], w=[("ydil_d", p)])
        if "ydil" in self.dbg:
            o = self.dout("dbg_ydil", [4, 128, NOWN], BF16)
            S.dma("sp", o.ap(), self.ydil_d.ap(), r=[("ydil_d", p) for p in range(4)])
        self.release(m)

    def row_bcast(self, dst, col0):
        diag = self.sb("diag", [128, 128], F32)
        for k in range(KC):
            self.DVE("tensor_scalar", ["ident_f", "modT"], ["diag"], out=diag[:], in0=self.ident_f[:],
                     scalar1=self.modT[:, col0 + k:col0 + k + 1], scalar2=None, op0=ALU.mult)
            p5 = self.psb(5)
            self.mm(p5[:, 0:128], self.ones_f[:], diag[:], True, True, ["ones_f", "diag"], [("ps", 5)])
            self.ACT(dst[:, k * 128:(k + 1) * 128], p5[:, 0:128], AF.Copy, [("ps", 5)], ["rowb"])

    def phase4(self):
        S = self.S
        wbn_in = self.din("w_br_nsa", [1024, D])
        wbd_in = self.din("w_br_dil", [512, D])
        wout_in = self.din("w_out", [D, D])
        self.y = self.dout("y", [NOWN, D])
        self.h2T_d = self.dscratch("h2T_d", [KC, 128, NOWN], BF16)
        m = self.mark()
        g1b = self.sb("g1b", [128, D], F32)
        self.row_bcast(g1b, 32)
        yn = self.sb("yn", [128, 8, 512], BF16)
        yd = self.sb("yd", [128, 4, 512], BF16)
        Wbn2 = [self.sb("Wbn%d" % i, [128, 8, 128], BF16) for i in range(2)]
        Wbd2 = [self.sb("Wbd%d" % i, [128, 4, 128], BF16) for i in range(2)]
        Wm2 = [self.sb("Wm%d" % i, [128, 2, KC, 128], BF16) for i in range(2)]
        mg = self.sb("mg", [128, KC, 512], BF16)
        Wo = self.sb("Wo", [128, KC, 512], BF16)
        xt = [self.sb("x4_%d" % i, [128, D], F32) for i in range(4)]
        xn = self.sb("xn4", [128, D], BF16)
        h2g = self.sb("h2g", [128, KC, 512], BF16)
        s1 = self.sb("s1", [128, 512], F32)
        s2 = self.sb("s2", [128, 512], F32)
        t1 = self.sb("t1", [128, 512], F32)
        st = self.sb("st4", [128, 3], F32)
        for qc in range(4):
            qsl = slice(qc * 512, (qc + 1) * 512)
            S.dma("sp", yn[:], self.ynsa_d.ap().rearrange("h p t -> p h t")[:, :, qsl], w=["yn"])
            S.dma("sp", yd[:], self.ydil_d.ap().rearrange("h p t -> p h t")[:, :, qsl], w=["yd"])
            for ti in range(4):
                tt = qc * 4 + ti
                S.dma("sp", xt[ti][:], self.x_in.ap()[NOWN + tt * 128:NOWN + (tt + 1) * 128, :], w=[("x4", ti)])
            for fc in range(KC):
                fsl = slice(fc * 128, (fc + 1) * 128)
                wb = fc % 2
                Wbn, Wbd, Wm = Wbn2[wb], Wbd2[wb], Wm2[wb]
                S.dma("pool", Wbn[:], wbn_in.ap().rearrange("(h d) f -> d h f", d=128)[:, :, fsl], w=[("Wbn", wb)])
                S.dma("pool", Wbd[:], wbd_in.ap().rearrange("(h d) f -> d h f", d=128)[:, :, fsl], w=[("Wbd", wb)])
                self.load_w("pool", Wm[:, 0], OFF_MERGE + fc * 128, 128, ("Wm", 0, wb))
                self.load_w("pool", Wm[:, 1], OFF_MERGE + D + fc * 128, 128, ("Wm", 1, wb))
                A = self.psb(2); Bm = self.psb(3); G1 = self.psb(0); G2 = self.psb(1)
                for h in range(8):
                    self.mm(A[:, :], Wbn[:, h, :], yn[:, h, :], h == 0, h == 7, [("Wbn", wb), "yn"], [("ps", 2)])
                for p in range(4):
                    self.mm(Bm[:, :], Wbd[:, p, :], yd[:, p, :], p == 0, p == 3, [("Wbd", wb), "yd"], [("ps", 3)])
                for k in range(KC):
                    self.mm(G1[:, :], Wm[:, 0, k, :], self.hT[:, k, qsl], k == 0, k == KC - 1, [("Wm", 0, wb), ("hT", qc)], [("ps", 0)])
                for k in range(KC):
                    self.mm(G2[:, :], Wm[:, 1, k, :], self.hT[:, k, qsl], k == 0, k == KC - 1, [("Wm", 1, wb), ("hT", qc)], [("ps", 1)])
                self.ACT(s1[:], G1[:, :], AF.Sigmoid, [("ps", 0)], ["s1"])
                self.ACT(s2[:], G2[:, :], AF.Sigmoid, [("ps", 1)], ["s2"])
                self.DVE("tensor_tensor", [("ps", 2), "s1"], ["s1"], out=s1[:], in0=s1[:], in1=A[:, :], op=ALU.mult)
                self.DVE("tensor_tensor", [("ps", 3), "s2"], ["s2"], out=s2[:], in0=s2[:], in1=Bm[:, :], op=ALU.mult)
                self.DVE("tensor_tensor", ["s1", "s2"], [("mg", fc)], out=mg[:, fc, :], in0=s1[:], in1=s2[:], op=ALU.add)
            if "merged" in self.dbg:
                if qc == 0:
                    self.dbg_merged = self.dout("dbg_merged", [128, KC, NOWN], BF16)
                S.dma("sp", self.dbg_merged.ap()[:, :, qsl], mg[:], r=[("mg", fc) for fc in range(KC)])
            for fo in range(4):
                fos = slice(fo * 512, (fo + 1) * 512)
                S.dma("pool", Wo[:], wout_in.ap().rearrange("(k p) f -> p k f", p=128)[:, :, fos], w=["Wo"])
                for ti in range(4):
                    po = self.psb(4 + ti % 2)
                    for fc in range(KC):
                        self.mm(po[:, :], mg[:, fc, ti * 128:(ti + 1) * 128], Wo[:, fc, :], fc == 0, fc == KC - 1,
                                [("mg", fc), "Wo"], [("ps", 4 + ti % 2)])
                    self.DVE("tensor_tensor", [("ps", 4 + ti % 2), "rowb"], ["t1"], out=t1[:], in0=g1b[:, fos], in1=po[:, :], op=ALU.mult)
                    self.DVE("tensor_tensor", ["t1", ("x4", ti)], [("x4", ti)], out=xt[ti][:, fos], in0=xt[ti][:, fos], in1=t1[:], op=ALU.add)
            for ti in range(4):
                tt = qc * 4 + ti
                S.dma("sp", self.y.ap()[tt * 128:(tt + 1) * 128, :], xt[ti][:], r=[("x4", ti)], w=[("y", tt)])
                self.ACT(xn[:], xt[ti][:], AF.Square, [("x4", ti)], ["xn4", "st4"], accum_out=st[:, 0:1])
                self.ACT(st[:, 1:2], st[:, 0:1], AF.Sqrt, ["st4"], ["st4"], scale=1.0 / D, bias=EPS)
                self.DVE("reciprocal", ["st4"], ["st4"], out=st[:, 2:3], in_=st[:, 1:2])
                self.DVE("tensor_scalar", [("x4", ti), "st4"], ["xn4"], out=xn[:], in0=xt[ti][:], scalar1=st[:, 2:3], scalar2=None, op0=ALU.mult)
                for half in range(2):
                    pst = self.psb(6 + half, BF16)
                    for kk in range(8):
                        k = half * 8 + kk
                        self.tr(pst[:, kk * 128:(kk + 1) * 128], xn[:, k * 128:(k + 1) * 128], self.ident_b[:],
                                ["xn4", "ident_b"], [("ps", 6 + half)])
                    for kk in range(8):
                        k = half * 8 + kk
                        dst = h2g[:, k, ti * 128:(ti + 1) * 128]
                        src = pst[:, kk * 128:(kk + 1) * 128]
                        if half == 0:
                            self.DVE("tensor_scalar", [("ps", 6 + half), "A2", "modT"], [("h2g", ti)], out=dst, in0=src,
                                     scalar1=self.A2[:, k:k + 1], scalar2=self.modT[:, 48 + k:49 + k], op0=ALU.mult, op1=ALU.add)
                        else:
                            self.ACT(dst, src, AF.Identity, [("ps", 6 + half), "A2", "modT"], [("h2g", ti)],
                                     scale=self.A2[:, k:k + 1], bias=self.modT[:, 48 + k:49 + k])
            S.dma("sp", self.h2T_d.ap().rearrange("k p t -> p k t")[:, :, qsl], h2g[:], r=[("h2g", ti) for ti in range(4)],
                  w=[("h2Td", qc)])
        if "h2T" in self.dbg:
            o = self.dout("dbg_h2T", [KC, 128, NOWN], BF16)
            S.dma("sp", o.ap(), self.h2T_d.ap(), r=[("h2Td", q) for q in range(4)])
        self.release(m)

    def peer_precast(self):
        S = self.S
        self.uT_in = self.din("peer_uT", [D, 16384])
        self.v_in = self.din("peer_v", [16384, D])
        self.uT_b = self.dscratch("uT_b", [64, 128, KC, 256], BF16)
        self.v_b = self.dscratch("v_b", [16384, D], BF16)
        self.pcq = []
        for i in range(32):
            k_, p0 = i // 2, (i % 2) * 64
            self.pcq.append((self.uT_b.ap()[:, p0:p0 + 64, k_, :].rearrange("c p e -> p c e"),
                             self.uT_in.ap()[i * 64:(i + 1) * 64, :].rearrange("p (c e) -> p c e", e=256), ("uTb", i // 2)))
        for i in range(32):
            self.pcq.append((self.v_b.ap()[i * 512:(i + 1) * 512, :], self.v_in.ap()[i * 512:(i + 1) * 512, :], ("vb", i // 2)))
        self.pcq.reverse()

    def pc(self, n=1):
        for _ in range(n):
            if getattr(self, "pcq", None):
                o, i_, key = self.pcq.pop()
                self.S.dma("pool", o, i_, w=[(key, len(self.pcq))])

    def phase5(self):
        S = self.S
        wq_in = self.din("peer_w_q", [D, D])
        gain_in = self.din("peer_q_gain_row", [1, D])
        skT_in = self.din("peer_skT", [128, 16, 128])
        self.sc_d = self.dscratch("sc_d", [NOWN, D], F32)
        self.pc(64)
        self.release(self.m_hT)
        m0 = self.mark()
        g2b = self.sb("g2b", [128, D], F32)
        self.row_bcast(g2b, 80)
        EC = 256
        NB = 4
        NEC = 16384 // EC
        coef = [self.sb("coef0", [128, 16384], BF16)]
        Ef = [self.sb("Ef%d" % i, [128, 8, 128], F32) for i in range(2)]
        C = [self.sb("C%d" % i, [128, 1024], BF16) for i in range(2)]
        sct = self.sb("sctc", [128, D], F32)
        e12 = [self.sb("e12_%d" % i, [128, 8, 2, 128], F32) for i in range(2)]
        t16 = self.sb("t16", [128, 2, 16], F32)
        wk = self.sb("pwk", [128, 256], F32)
        cand = self.sb("cand", [128, 16, 16], F32)
        c16 = self.sb("c16", [128, 16], F32)
        e16 = self.sb("e16", [128, 16], F32)
        scal = [self.sb("scal%d" % i, [128, 8, 8], F32) for i in range(2)]
        cnt = {"s3": 0, "pendD": None}

        def load_scores(tt):
            S.dma("sp", sct[:], self.sc_d.ap()[tt * 128:(tt + 1) * 128, :], r=[("scd", tt)], w=["sctc"])

        def topk(sbi, h):
            sl = scal[sbi]
            sk = ("scal", sbi, h)
            for pi in range(2):
                sv = sct[:, (2 * h + pi) * 128:(2 * h + pi + 1) * 128]
                self.DVE("max", ["sctc"], ["t16"], out=t16[:, pi, 0:8], in_=sv)
                self.DVE("match_replace", ["sctc", "t16"], ["pwk"], out=wk[:, 0:128], in_to_replace=t16[:, pi, 0:8], in_values=sv, imm_value=-1e30)
                self.DVE("max", ["pwk"], ["t16"], out=t16[:, pi, 8:16], in_=wk[:, 0:128])
            self.DVE("tensor_tensor", ["t16"], ["cand"], out=cand[:], in0=t16[:, 0, :].unsqueeze(2).to_broadcast([128, 16, 16]),
                     in1=t16[:, 1, :].unsqueeze(1).to_broadcast([128, 16, 16]), op=ALU.add)
            cf = cand[:].rearrange("p a b -> p (a b)")
            self.DVE("max", ["cand"], ["c16"], out=c16[:, 0:8], in_=cf)
            self.DVE("match_replace", ["cand", "c16"], ["pwk"], out=wk[:], in_to_replace=c16[:, 0:8], in_values=cf, imm_value=-1e30)
            self.DVE("max", ["pwk"], ["c16"], out=c16[:, 8:16], in_=wk[:])
            self.DVE("tensor_scalar", ["c16"], [sk], out=sl[:, h, 0:1], in0=c16[:, 0:1], scalar1=-1.0, scalar2=None, op0=ALU.mult)
            self.ACT(e16[:], c16[:], AF.Exp, ["c16", sk], ["e16", sk], bias=sl[:, h, 0:1], accum_out=sl[:, h, 1:2])
            self.ACT(sl[:, h, 2:3], sl[:, h, 1:2], AF.Ln, [sk], [sk])
            self.DVE("tensor_tensor", [sk], [sk], out=sl[:, h, 3:4], in0=sl[:, h, 0:1], in1=sl[:, h, 2:3], op=ALU.subtract)
            self.DVE("scalar_tensor_tensor", [sk, "t16"], [sk], out=sl[:, h, 5:6], in0=t16[:, 0, 0:1], scalar=-1.0, in1=sl[:, h, 2:3],
                     op0=ALU.mult, op1=ALU.subtract)
            self.DVE("tensor_scalar", ["t16"], [sk], out=sl[:, h, 6:7], in0=t16[:, 1, 0:1], scalar1=-1.0, scalar2=None, op0=ALU.mult)
            self.ACT(sl[:, h, 7:8], c16[:, 15:16], AF.Exp, ["c16", sk], [sk], bias=sl[:, h, 3:4])
            self.DVE("tensor_scalar", [sk], [sk], out=sl[:, h, 4:5], in0=sl[:, h, 7:8], scalar1=0.9995, scalar2=None, op0=ALU.mult)
            self.ACT(e12[sbi][:, h, 0, :], sct[:, (2 * h) * 128:(2 * h + 1) * 128], AF.Exp, ["sctc", sk], [("e12", sbi, h)], bias=sl[:, h, 5:6])
            self.ACT(e12[sbi][:, h, 1, :], sct[:, (2 * h + 1) * 128:(2 * h + 2) * 128], AF.Exp, ["sctc", sk], [("e12", sbi, h)], bias=sl[:, h, 6:7])

        def coef_step(sbi, a8, h):
            sl = scal[sbi]
            bi = cnt["s3"] % 2
            eng = "pool"
            cnt["s3"] += 1
            self.V(eng, "tensor_tensor", [("e12", sbi, h)], [("Ef", bi)], out=Ef[bi][:],
                   in0=e12[sbi][:, h, 0, a8 * 8:(a8 + 1) * 8].unsqueeze(2).to_broadcast([128, 8, 128]),
                   in1=e12[sbi][:, h, 1, :].unsqueeze(1).to_broadcast([128, 8, 128]), op=ALU.mult)
            eff = Ef[bi][:].rearrange("p a b -> p (a b)")
            self.DVE("scalar_tensor_tensor", [("Ef", bi), ("scal", sbi, h)], [("C", bi)], out=C[bi][:], in0=eff,
                     scalar=sl[:, h, 4:5], in1=eff, op0=ALU.is_ge, op1=ALU.mult)
            for j in range(2):
                self.mm(self.psb(1 + 2 * j)[:, :], self.ident_b[:], C[bi][:, j * 512:(j + 1) * 512], h == 0, h == 7,
                        ["ident_b", ("C", bi)], [("ps", 1 + 2 * j)])
            if h == 7:
                for j in range(2):
                    self.ACT(coef[sbi][:, a8 * 1024 + j * 512:a8 * 1024 + (j + 1) * 512], self.psb(1 + 2 * j)[:, :], AF.Copy,
                             [("ps", 1 + 2 * j)], [("coef", sbi, a8)])

        def flushD():
            pass

        m = self.mark()
        Wq = self.sb("pWq", [128, KC, D], BF16)
        for k in range(KC):
            S.dma("pool", Wq[:, k, :], wq_in.ap()[k * 128:(k + 1) * 128, :], w=[("pWq", k)])
        gqb = self.sb("gqb", [128, D], F32)
        S.dma("sp", gqb[:], gain_in.ap().partition_broadcast(128), w=["gqb"])
        skT = self.sb("skT", [128, 16, 128], BF16)
        S.dma("pool", skT[:], skT_in.ap(), w=["skT"])
        h2t = [self.sb("h2t%d" % i, [128, KC, 128], BF16) for i in range(2)]
        sq = self.sb("psq", [128, 512], F32)
        ss = self.sb("pss", [128, 16], F32)
        qn = self.sb("pqn", [128, D], BF16)
        tq = self.sb("ptq", [128, 512], F32)
        qnT = self.sb("pqnT", [128, 16, 128], BF16)
        scb = [self.sb("psct%d" % i, [128, D], F32) for i in range(2)]
        for tt in range(16):
            b = tt % 2
            S.dma("sp", h2t[b][:], self.h2T_d.ap().rearrange("k p t -> p k t")[:, :, tt * 128:(tt + 1) * 128], w=[("h2t", b)])
            for fo in range(4):
                pq = self.psb(2 * fo)
                for k in range(KC):
                    self.mm(pq[:, :], h2t[b][:, k, :], Wq[:, k, fo * 512:(fo + 1) * 512], k == 0, k == KC - 1,
                            [("h2t", b), ("pWq", k)], [("ps", 2 * fo)])
                self.ACT(sq[:], pq[:, :], AF.Square, [("ps", 2 * fo)], ["psq"])
                self.DVE("reduce_sum", ["psq"], [("pss", fo)], out=ss[:, fo * 4:(fo + 1) * 4],
                         in_=sq[:].rearrange("p (a b) -> p a b", b=128), axis=AX.X)
            self.ACT(ss[:], ss[:], AF.Sqrt, [("pss", f) for f in range(4)], [("pss", f) for f in range(4)], scale=1.0 / 128, bias=EPS)
            self.DVE("reciprocal", [("pss", f) for f in range(4)], [("pss", f) for f in range(4)], out=ss[:], in_=ss[:])
            for fo in range(4):
                pq = self.psb(2 * fo)
                self.DVE("tensor_tensor", [("ps", fo), ("pss", 0)], ["ptq"], out=tq[:].rearrange("p (a b) -> p a b", b=128),
                         in0=pq[:, :].rearrange("p (a b) -> p a b", b=128),
                         in1=ss[:, fo * 4:(fo + 1) * 4].unsqueeze(2).to_broadcast([128, 4, 128]), op=ALU.mult)
                self.DVE("tensor_tensor", ["ptq", "gqb"], [("pqn", fo)], out=qn[:, fo * 512:(fo + 1) * 512], in0=tq[:],
                         in1=gqb[:, fo * 512:(fo + 1) * 512], op=ALU.mult)
            for half in range(2):
                pst = self.psb(5 + 2 * half, BF16)
                for kk in range(8):
                    hp = half * 8 + kk
                    self.tr(pst[:, kk * 128:(kk + 1) * 128], qn[:, hp * 128:(hp + 1) * 128], self.ident_b[:],
                            [("pqn", hp // 4), "ident_b"], [("ps", 5 + 2 * half)])
                self.ACT(qnT[:, half * 8:(half + 1) * 8, :].rearrange("p a b -> p (a b)"), pst[:, :], AF.Copy, [("ps", 5 + 2 * half)], [("pqnT", half)])
            for fo in range(4):
                pq = self.psb(2 * fo)
                for i in range(4):
                    hp = fo * 4 + i
                    self.mm(pq[:, i * 128:(i + 1) * 128], qnT[:, hp, :], skT[:, hp, :], True, True,
                            [("pqnT", hp // 8), "skT"], [("ps", 2 * fo)])
                self.ACT(scb[b][:, fo * 512:(fo + 1) * 512], pq[:, :], AF.Copy, [("ps", 2 * fo)], [("psct", b)])
            S.dma("sp", self.sc_d.ap()[tt * 128:(tt + 1) * 128, :], scb[b][:], r=[("psct", b)], w=[("scd", tt)])
            if tt == 0:
                load_scores(0)
            elif tt == 1:
                for h in range(8):
                    topk(0, h)
            else:
                lo = (tt - 2) * 10
                for idx in range(lo, min(128, lo + 10)):
                    coef_step(0, idx // 8, idx % 8)
        self.release(m)

        coef.append(self.sb("coef1", [128, 16384], BF16))
        ub = [self.sb("ub%d" % i, [128, KC, EC], BF16) for i in range(NB)]
        vb = [self.sb("vb%d" % i, [128, EC // 128, D], BF16) for i in range(NB)]
        h2t = [self.sb("h2tc%d" % i, [128, KC, 128], BF16) for i in range(2)]
        x1c = self.sb("x1c", [128, 512], F32)
        Gb = [self.sb("Gb%d" % i, [128, EC], BF16) for i in range(2)]
        Wb = [self.sb("Wb%d" % i, [128, EC], BF16) for i in range(2)]
        WT = [self.sb("WT%d" % i, [128, EC // 128, 128], BF16) for i in range(2)]
        tf = self.sb("ptf", [128, 512], F32)

        def load_tile(tt):
            sbi = tt % 2
            S.dma("sp", h2t[sbi][:], self.h2T_d.ap().rearrange("k p t -> p k t")[:, :, tt * 128:(tt + 1) * 128], w=[("h2tc", sbi)])

        def load_chunk(ec):
            b = ec % NB
            S.dma("sp", ub[b][:], self.uT_b.ap()[ec], r=[("uTb", i) for i in range(16)], w=[("ub", b)])
            S.dma("sp", vb[b][:], self.v_b.ap()[ec * EC:(ec + 1) * EC, :].rearrange("(s p) d -> p s d", p=128),
                  r=[("vb", (ec * EC) // 1024)], w=[("vbuf", b)])

        def act_mm(sbi, ec):
            b = ec % NB
            pa = self.psb(0)[:, (ec % 2) * EC:(ec % 2 + 1) * EC]
            for k in range(KC):
                self.mm(pa, h2t[sbi][:, k, :], ub[b][:, k, :], k == 0, k == KC - 1, [("h2tc", sbi), ("ub", b)], [("ps", 0)])

        seq = [(tt, ec) for tt in range(16) for ec in range(NEC)]
        PF = NB - 1
        load_tile(0)
        load_scores(1)
        for i in range(PF):
            load_chunk(seq[i][1])
        act_mm(0, 0)
        for si, (tt, ec) in enumerate(seq):
            sbi = tt % 2
            nxt = (tt + 1) % 2
            if ec == 0 and tt + 1 < 16:
                load_tile(tt + 1)
            if si + PF < len(seq):
                load_chunk(seq[si + PF][1])
            b = ec % NB
            p2 = ec % 2
            pa = self.psb(0)[:, p2 * EC:(p2 + 1) * EC]
            self.ACT(Gb[p2][:], pa, AF.Gelu_apprx_tanh, [("ps", 0)], [("Gb", p2)])
            if si + 1 < len(seq):
                act_mm(seq[si + 1][0] % 2, seq[si + 1][1])
            self.DVE("tensor_tensor", [("Gb", p2), ("coef", sbi, (ec * EC) // 1024)], [("Wb", p2)], out=Wb[p2][:], in0=Gb[p2][:],
                     in1=coef[sbi][:, ec * EC:(ec + 1) * EC], op=ALU.mult)
            pt = self.psb(2, BF16)[:, p2 * EC:(p2 + 1) * EC]
            nsub = EC // 128
            for i in range(nsub):
                self.tr(pt[:, i * 128:(i + 1) * 128], Wb[p2][:, i * 128:(i + 1) * 128], self.ident_b[:], [("Wb", p2), "ident_b"], [("ps", 2)])
            self.ACT(WT[p2][:].rearrange("p a b -> p (a b)"), pt, AF.Copy, [("ps", 2)], [("WT", p2)])
            for i in range(nsub):
                for dc in range(4):
                    self.mm(self.psb(4 + dc)[:, :], WT[p2][:, i, :], vb[b][:, i, dc * 512:(dc + 1) * 512],
                            ec == 0 and i == 0, ec == NEC - 1 and i == nsub - 1, [("WT", p2), ("vbuf", b)], [("ps", 4 + dc)])
            if tt + 1 < 16:
                if ec == 0:
                    for h_ in range(8):
                        topk(nxt, h_)
                for idx in (2 * ec, 2 * ec + 1):
                    coef_step(nxt, idx // 8, idx % 8)
            if ec == NEC - 1:
                flushD()
                if tt + 2 < 16:
                    load_scores(tt + 2)
                for dc in range(4):
                    dsl = slice(dc * 512, (dc + 1) * 512)
                    S.dma("sp", x1c[:], self.y.ap()[tt * 128:(tt + 1) * 128, dsl], r=[("y", tt)], w=["x1c"])
                    self.DVE("tensor_tensor", [("ps", 4 + dc), "rowb"], ["ptf"], out=tf[:], in0=g2b[:, dsl], in1=self.psb(4 + dc)[:, :], op=ALU.mult)
                    self.DVE("tensor_tensor", ["ptf", "x1c"], ["x1c"], out=x1c[:], in0=x1c[:], in1=tf[:], op=ALU.add)
                    S.dma("sp", self.y.ap()[tt * 128:(tt + 1) * 128, dsl], x1c[:], r=["x1c"], w=[("y2", tt, dc)])
        self.release(m0)

    def finish(self):
        S = self.S
        S.add("sp", lambda e: e.nop(), extra_deps=[o.idx for o in S.ops if o.isdma])
        with ExitStack() as st:
            S.emit(st)
        return self.nc


def build(stop_after=None, dbg=()):
    B = Builder(stop_after, dbg)
    B.phase0()
    if stop_after != 1 and "nopeer" not in dbg:
        B.peer_precast()
    B.phase1()
    if stop_after != 1:
        if "skip2" not in dbg:
            B.phase2()
        else:
            B.w_in = B.din("w_in", [D, IN_COLS])
            B.Pbuf = [B.sb("P%d" % i, [128, 512], BF16) for i in range(3)]
            B.P_i = 0
            B.nb_kf = B.sb("nb_kf", [128, 512], F32)
            B.nb_sq = B.sb("nb_sq", [128, 512], BF16)
            B.nb_rs = B.sb("nb_rs", [128, 512], F32)
        B.phase3()
        B.phase4()
        if "nopeer" not in dbg:
            B.phase5()
    B.finish()
    return B


def _t5_bucket(n):
    n = np.maximum(n, 0)
    nf = np.maximum(n, 16).astype(np.float32)
    large = 16 + (np.log(nf / np.float32(16)) / np.float32(math.log(128.0)) * np.float32(16)).astype(np.int32)
    large = np.minimum(large, 31)
    return np.where(n < 16, n, large).astype(np.int64)


def _gather_strip(tbl_row, dist, valid):
    tb = np.concatenate([np.array([NEGB], np.float32), tbl_row.astype(np.float32)])
    idx = np.where(valid, _t5_bucket(dist) + 1, 0)
    return tb[idx]


def host_shared(inputs):
    m = {}
    rb = inputs["rel_bias"]
    p = np.arange(128)[:, None]
    strips = np.zeros((8, 128, 8064), np.float32)
    j = np.arange(2560)[None, :]
    r_sel = j - p - 384
    j2 = np.arange(1408)[None, :]
    r_win = j2 - p - 384
    j3 = np.arange(4096)[None, :]
    r_cmp = j3 - 16 * p - 31
    for h in range(8):
        strips[h, :, 0:2560] = _gather_strip(rb[h], r_sel, r_sel >= 0)
        strips[h, :, 2560:3968] = _gather_strip(rb[h], r_win, (r_win >= 0) & (r_win <= 511))
        strips[h, :, 3968:8064] = _gather_strip(rb[h], r_cmp, r_cmp >= 0)
    m["nsa_strips"] = strips
    dstrips = np.zeros((12, 128, 1024), np.float32)
    j4 = np.arange(1024)[None, :]
    r_d = j4 - p - 384
    for gi, (win, dil) in enumerate(DIL_CFG):
        for ps_ in range(4):
            hd = gi * 4 + ps_
            dstrips[hd] = _gather_strip(rb[8 + hd], r_d * dil, (r_d >= 0) & (r_d <= win // dil))
    m["dil_strips"] = dstrips
    c = (np.arange(2)[None, :, None] * 128 + np.arange(128)[:, None, None])
    jj = np.arange(64)[None, None, :]
    m["ovl"] = (((16 * c) < (64 * jj + 64)) & ((16 * c + 32) > (64 * jj))).astype(np.float32)
    jrow = np.arange(64)[:, None, None]
    kt = np.arange(32)[None, :, None]
    pp = np.arange(128)[None, None, :]
    m["e_all"] = (jrow == (2 * kt + pp // 64)).astype(np.float32)
    m["selg"] = np.ascontiguousarray(np.broadcast_to(np.eye(24, dtype=np.float32)[:, :, None], (24, 24, 128)))
    m["w_ada"] = inputs["w_ada"][0]
    m["b_ada_fm"] = np.ascontiguousarray(inputs["b_ada"][0].reshape(96, 128).T)
    m["norm1_g_fm"] = np.ascontiguousarray(inputs["norm1_g"][0].reshape(16, 128).T)
    m["norm2_g_fm"] = np.ascontiguousarray(inputs["norm2_g"][0].reshape(16, 128).T)
    m["w_in"] = inputs["w_in"][0]
    m["nsa_q_gain_fm"] = np.ascontiguousarray(inputs["nsa_q_gain"][0].reshape(128, 1))
    m["nsa_k_gain_fm"] = np.ascontiguousarray(inputs["nsa_k_gain"][0].T)
    m["cmp_posT"] = np.ascontiguousarray(inputs["cmp_pos"][0].transpose(2, 0, 1))
    m["cmp_w1"] = inputs["cmp_w1"][0]
    m["cmp_w2"] = inputs["cmp_w2"][0]
    m["dil_q_gain_fm"] = np.ascontiguousarray(inputs["dil_q_gain"][0].reshape(128, 1))
    m["dil_k_gain_fm"] = np.ascontiguousarray(inputs["dil_k_gain"][0].reshape(128, 1))
    m["w_br_nsa"] = inputs["w_br_nsa"][0]
    m["w_br_dil"] = inputs["w_br_dil"][0]
    m["w_out"] = inputs["w_out"][0]
    m["peer_w_q"] = inputs["peer_w_q"][0]
    m["peer_q_gain_row"] = np.ascontiguousarray(inputs["peer_q_gain"][0].reshape(1, 2048))
    m["peer_skT"] = np.ascontiguousarray(inputs["peer_sub_keys"][0].reshape(16, 128, 128).transpose(2, 0, 1))
    m["peer_uT"] = np.ascontiguousarray(inputs["peer_u"][0].T)
    m["peer_v"] = inputs["peer_v"][0]
    for hf in range(2):
        qi = np.arange(NOWN)
        cur = (qi + NOWN * hf) // 64
        jl = np.arange(64)[None, :]
        jg = jl - 32 + 32 * hf
        curc = cur[:, None]
        forced = (jg == 0) | (jg == curc) | (jg == curc - 1)
        bad = (jg > curc) | (jg < 0)
        add = np.where(forced & ~(jg < 0), 1e4, np.where(bad, -1e4, 0.0)).astype(np.float32)
        mult = np.where((forced & ~(jg < 0)) | bad, 0.0, 1.0).astype(np.float32)
        m["sel_add%d" % hf] = np.ascontiguousarray(add.reshape(16, 128, 64).transpose(1, 0, 2))
        m["sel_mult%d" % hf] = np.ascontiguousarray(mult.reshape(16, 128, 64).transpose(1, 0, 2))
    return m


def host_inputs(inputs, core, shared=None):
    if shared is None:
        shared = host_shared(inputs)
    b, hf = core // 2, core % 2
    x = inputs["x"]
    m = dict(shared)
    xl = np.zeros((NLOC, D), np.float32)
    if hf == 1:
        xl[:] = x[b]
    else:
        xl[NOWN:] = x[b, :NOWN]
    m["x_loc"] = xl
    m["pfx_bias"] = np.full((128, 1), 0.0 if hf == 1 else NEGB, np.float32)
    m["c_fm"] = np.ascontiguousarray(inputs["c"][b].reshape(16, 128).T)
    m["sel_add"] = shared["sel_add%d" % hf]
    m["sel_mult"] = shared["sel_mult%d" % hf]
    return m


def kernel(**inputs):
    inputs = {k: np.asarray(v) for k, v in inputs.items()}
    B = build()
    shared = host_shared(inputs)
    in_maps = []
    for c in range(8):
        hm = host_inputs(inputs, c, shared)
        in_maps.append({k: hm[k] for k in B.inputs})
    res = run_bass_kernel_spmd(B.nc, in_maps, core_ids=list(range(8)))
    out = np.zeros((4, 4096, D), np.float32)
    for c in range(8):
        out[c // 2, (c % 2) * NOWN:(c % 2 + 1) * NOWN] = res.results[c]["y"]
    return out
```

```python
import math
from contextlib import ExitStack
import numpy as np
import concourse.bass as bass
import concourse.mybir as mybir
from concourse.bass_utils import run_bass_kernel_spmd

F32 = mybir.dt.float32
BF16 = mybir.dt.bfloat16
I32 = mybir.dt.int32
U32 = mybir.dt.uint32
AF = mybir.ActivationFunctionType
ALU = mybir.AluOpType
AX = mybir.AxisListType

D = 2048
KC = 16
NLOC = 4096
NOWN = 2048
DH = 128
OFF_KV = 1024
OFF_GATE = OFF_KV + 1536
OFF_DIL = OFF_GATE + 24
OFF_MERGE = OFF_DIL + 4608
IN_COLS = OFF_MERGE + 4096
DIL_CFG = ((128, 1), (512, 4), (2048, 16))
EPS = 1e-6
NEGB = -30000.0
SCALE = DH ** -0.5
SB_BASE = 16640
SB_END = 229344


class Op:
    __slots__ = ("eng", "fn", "deps", "sig", "need", "isdma", "idx")


class Sched:
    COMPUTE = ("pe", "act", "dve", "pool")
    RING = 8

    def __init__(self, nc, same_engine_sync=True):
        self.nc = nc
        self.ops = []
        self.last_w = {}
        self.readers = {}
        self.ses = same_engine_sync
        self.last_on = {}
        self.dma_pend = []

    def add(self, eng, fn, r=(), w=(), dma=False, extra_deps=()):
        op = Op()
        op.eng = eng; op.fn = fn; op.isdma = dma; op.need = dma; op.sig = None
        op.idx = len(self.ops)
        deps = set(extra_deps)
        for k in r:
            lw = self.last_w.get(k)
            if lw is not None:
                deps.add(lw)
        for k in w:
            lw = self.last_w.get(k)
            if lw is not None:
                deps.add(lw)
            for rd in self.readers.get(k, ()):
                deps.add(rd)
        deps.discard(op.idx)
        fd = []
        for d in deps:
            o = self.ops[d]
            if (not o.isdma) and o.eng == eng and (eng == "pe" or not self.ses):
                continue
            o.need = True
            fd.append(d)
        op.deps = sorted(fd)
        self.ops.append(op)
        for k in w:
            self.last_w[k] = op.idx
            self.readers[k] = []
        for k in r:
            lst = self.readers.setdefault(k, [])
            if not dma:
                lst[:] = [x for x in lst if self.ops[x].isdma or self.ops[x].eng != eng]
            lst.append(op.idx)
        if not dma:
            self.last_on[eng] = op.idx
        else:
            self.dma_pend.append(op.idx)
        return op.idx

    def dma(self, eng, out, in_, r=(), w=(), **kw):
        return self.add(eng, lambda e: e.dma_start(out=out, in_=in_, **kw), r=r, w=w, dma=True)

    def barrier(self):
        lasts = dict(self.last_on)
        engs = ["pe", "act", "dve", "pool", "sp"]
        deps = list(lasts.values()) + list(self.dma_pend)
        for e in engs:
            self.add(e, lambda en: en.nop(), extra_deps=[d for d in deps])
        self.dma_pend = []
        self.last_w = {}
        self.readers = {}

    def emit(self, stack):
        nc = self.nc
        engs = sorted({o.eng for o in self.ops})
        csem = {}
        for e in engs:
            csem[e] = stack.enter_context(nc.semaphore("c_" + e))
        dring = {}
        for e in engs:
            if any(o.isdma and o.eng == e for o in self.ops):
                dring[e] = [stack.enter_context(nc.semaphore("d_%s%d" % (e, i))) for i in range(self.RING)]
        ccount = {e: 0 for e in engs}
        dcount = {e: 0 for e in engs}
        ringtot = {e: [0] * self.RING for e in engs}
        pre = {}
        for o in self.ops:
            if o.isdma:
                n = dcount[o.eng]; dcount[o.eng] += 1
                slot = n % self.RING
                prev = ringtot[o.eng][slot]
                ringtot[o.eng][slot] += 16
                pre[o.idx] = (dring[o.eng][slot], prev)
                o.sig = (dring[o.eng][slot], prev + 16, 16)
            elif o.need:
                ccount[o.eng] += 1
                o.sig = (csem[o.eng], ccount[o.eng], 1)
        per = {e: [o for o in self.ops if o.eng == e] for e in engs}
        self.stats = {e: (len(per[e]), ccount[e], dcount[e]) for e in engs}
        ops = self.ops
        blk = stack.enter_context(nc.Block())

        def runner(e):
            def body(engine):
                seen = {}
                for o in per[e]:
                    waits = []
                    if o.isdma:
                        s, v = pre[o.idx]
                        if v > 0:
                            waits.append((s, v))
                    for d in o.deps:
                        s, v, _ = ops[d].sig
                        waits.append((s, v))
                    for s, v in waits:
                        key = id(s)
                        if seen.get(key, 0) < v:
                            engine.wait_ge(s, v)
                            seen[key] = v
                    ins = o.fn(engine)
                    if o.sig is not None:
                        ins.then_inc(o.sig[0], o.sig[2])
            return body

        for e in engs:
            reg = {"pe": blk.tensor, "act": blk.scalar, "dve": blk.vector,
                   "pool": blk.gpsimd, "sp": blk.sync}[e]
            reg(runner(e))


def _dtsize(dt):
    return 2 if dt == BF16 else 4


class Builder:
    def __init__(self, stop_after=None, dbg=()):
        self.nc = bass.Bass("TRN2", target_bir_lowering=False)
        self.S = Sched(self.nc)
        self.off = SB_BASE
        self.cnt = 0
        self.stop_after = stop_after
        self.dbg = dbg
        self.inputs = {}
        self.outs = {}
        nc = self.nc
        self.psf = [nc.alloc_psum_tensor("psb%d" % i, [128, 512], F32) for i in range(8)]

    def sb(self, name, shape, dt):
        nbytes = int(np.prod(shape[1:])) * _dtsize(dt)
        self.cnt += 1
        t = self.nc.alloc_sbuf_tensor_at("%s_%d" % (name, self.cnt), list(shape), dt, offset=self.off)
        self.off += (nbytes + 63) // 64 * 64
        self.maxoff = max(getattr(self, "maxoff", 0), self.off)
        assert self.off <= SB_END, ("SBUF overflow", name, self.off)
        return t

    def mark(self):
        return self.off

    def release(self, m):
        self.S.barrier()
        self.off = m

    def din(self, name, shape, dt=F32):
        t = self.nc.dram_tensor(name, list(shape), dt, kind="ExternalInput")
        self.inputs[name] = t
        return t

    def dout(self, name, shape, dt=F32):
        t = self.nc.dram_tensor(name, list(shape), dt, kind="ExternalOutput")
        self.outs[name] = t
        return t

    def dscratch(self, name, shape, dt):
        return self.nc.dram_tensor(name, list(shape), dt)

    def psb(self, i, dt=F32):
        a = self.psf[i][:]
        if dt == BF16:
            a = a.bitcast(BF16)
        return a

    def PE(self, fn, r, w):
        return self.S.add("pe", fn, r=r, w=w)

    def mm(self, out, lhsT, rhs, start, stop, r, w):
        return self.S.add("pe", lambda e: e.matmul(out, lhsT=lhsT, rhs=rhs, start=start, stop=stop), r=r, w=w)

    def tr(self, out, in_, ident, r, w):
        return self.S.add("pe", lambda e: e.transpose(out, in_, ident), r=r, w=w)

    def ACT(self, out, in_, func, r, w, **kw):
        return self.S.add("act", lambda e: e.activation(out=out, in_=in_, func=func, **kw), r=r, w=w)

    def V(self, eng, name, r, w, *a, **kw):
        return self.S.add(eng, lambda e: getattr(e, name)(*a, **kw), r=r, w=w)

    def DVE(self, name, r, w, *a, **kw):
        return self.V("dve", name, r, w, *a, **kw)

    def POOL(self, name, r, w, *a, **kw):
        return self.V("pool", name, r, w, *a, **kw)

    def dump(self, name, src_tensor, shape, dt, rkeys):
        o = self.dout("dbg_" + name, shape, dt)
        self.S.dma("sp", o.ap(), src_tensor[:], r=rkeys)

    def phase0(self):
        S = self.S
        self.ident_f = self.sb("ident_f", [128, 128], F32)
        self.ident_b = self.sb("ident_b", [128, 128], BF16)
        self.ones_b = self.sb("ones_b", [128, 128], BF16)
        self.ones_f = self.sb("ones_f", [128, 128], F32)
        self.zero1 = self.sb("zero1", [128, 1], F32)
        self.pfxb = self.sb("pfxb", [128, 1], F32)
        idf = self.ident_f
        self.POOL("memset", [], ["ident_f"], idf[:], 0.0)
        S.add("pool", lambda e: e.affine_select(out=idf[:], in_=idf[:], pattern=[[-1, 128]],
                                                compare_op=ALU.not_equal, fill=1.0, base=0,
                                                channel_multiplier=1), r=["ident_f"], w=["ident_f"])
        self.DVE("tensor_copy", ["ident_f"], ["ident_b"], out=self.ident_b[:], in_=idf[:])
        self.POOL("memset", [], ["ones_b"], self.ones_b[:], 1.0)
        self.POOL("memset", [], ["ones_f"], self.ones_f[:], 1.0)
        self.POOL("memset", [], ["zero1"], self.zero1[:], 0.0)
        pf = self.din("pfx_bias", [128, 1])
        S.dma("sp", self.pfxb[:], pf.ap(), w=["pfxb"])

        c_in = self.din("c_fm", [128, 16])
        w_ada = self.din("w_ada", [D, 6 * D])
        b_ada = self.din("b_ada_fm", [128, 96])
        n1g = self.din("norm1_g_fm", [128, 16])
        n2g = self.din("norm2_g_fm", [128, 16])
        self.modT = self.sb("modT", [128, 96], F32)
        self.A1 = self.sb("A1", [128, 16], F32)
        self.A2 = self.sb("A2", [128, 16], F32)
        m = self.mark()
        cT = self.sb("cT", [128, 16], F32)
        sg = self.sb("sg", [128, 16], F32)
        condT = self.sb("condT", [128, 16], F32)
        bT = self.sb("bT", [128, 96], F32)
        g1T = self.sb("g1T", [128, 16], F32)
        g2T = self.sb("g2T", [128, 16], F32)
        wa = [self.sb("wa%d" % i, [128, 6 * D], BF16) for i in range(3)]
        condB = self.sb("condB", [128, 16], BF16)
        S.dma("sp", cT[:], c_in.ap(), w=["cT"])
        S.dma("sp", bT[:], b_ada.ap(), w=["bT"])
        S.dma("sp", g1T[:], n1g.ap(), w=["g1T"])
        S.dma("sp", g2T[:], n2g.ap(), w=["g2T"])
        self.ACT(condT[:], cT[:], AF.Silu, ["cT"], ["condT"])
        self.DVE("tensor_copy", ["condT"], ["condB"], out=condB[:], in_=condT[:])
        ps = self.psb(0)
        for k in range(KC):
            wk = wa[k % 3]
            key = "wa%d" % (k % 3)
            for hh in range(4):
                cs = slice(hh * 3072, (hh + 1) * 3072)
                S.dma("pool", wk[:, cs], w_ada.ap()[k * 128:(k + 1) * 128, cs], w=[key + "_%d" % hh])
            for j in range(96):
                self.mm(ps[:, j:j + 1], wk[:, j * 128:(j + 1) * 128], condB[:, k:k + 1], True, True,
                        [key + "_%d" % (j // 24), "condB"], [("ps", 0)])
            if k == 0:
                self.DVE("tensor_copy", [("ps", 0)], ["modT"], out=self.modT[:], in_=ps[:, 0:96])
            else:
                self.DVE("tensor_tensor", [("ps", 0), "modT"], ["modT"], out=self.modT[:], in0=self.modT[:],
                         in1=ps[:, 0:96], op=ALU.add)
        self.DVE("tensor_tensor", ["modT", "bT"], ["modT"], out=self.modT[:], in0=self.modT[:], in1=bT[:], op=ALU.add)
        self.DVE("scalar_tensor_tensor", ["modT", "g1T"], ["A1"], out=self.A1[:], in0=self.modT[:, 16:32],
                 scalar=1.0, in1=g1T[:], op0=ALU.add, op1=ALU.mult)
        self.DVE("scalar_tensor_tensor", ["modT", "g2T"], ["A2"], out=self.A2[:], in0=self.modT[:, 64:80],
                 scalar=1.0, in1=g2T[:], op0=ALU.add, op1=ALU.mult)
        if "mod" in self.dbg:
            self.dump("mod", self.modT, [128, 96], F32, ["modT"])
        self.release(m)

    def phase1(self):
        S = self.S
        x_in = self.din("x_loc", [NLOC, D])
        self.x_in = x_in
        self.hT_d = self.dscratch("hT_pfx", [KC, 128, NOWN], BF16)
        self.m_hT = self.mark()
        self.hT = self.sb("hT", [128, KC, NOWN], BF16)
        m = self.mark()
        xt = [self.sb("xt%d" % i, [128, D], F32) for i in range(2)]
        xn = [self.sb("xn%d" % i, [128, D], BF16) for i in range(2)]
        junk = self.sb("junk", [128, D], BF16)
        st = self.sb("st", [128, 3 * 32], F32)
        hg = [self.sb("hg%d" % i, [128, KC, 512], BF16) for i in range(2)]
        def stage1(tt):
            b = tt % 2
            S.dma("sp", xt[b][:], x_in.ap()[tt * 128:(tt + 1) * 128, :], w=["xt%d" % b])
            self.ACT(junk[:], xt[b][:], AF.Square, ["xt%d" % b], ["junk", ("st", tt)], accum_out=st[:, tt:tt + 1])
            self.ACT(st[:, 32 + tt:33 + tt], st[:, tt:tt + 1], AF.Sqrt, [("st", tt)], [("st", tt)],
                     scale=1.0 / D, bias=EPS)
            self.DVE("reciprocal", [("st", tt)], [("st", tt)], out=st[:, 64 + tt:65 + tt], in_=st[:, 32 + tt:33 + tt])
            self.DVE("tensor_scalar", ["xt%d" % b, ("st", tt)], ["xn%d" % b], out=xn[b][:], in0=xt[b][:],
                     scalar1=st[:, 64 + tt:65 + tt], scalar2=None, op0=ALU.mult)

        def stage2(tt):
            g, i = tt // 4, tt % 4
            b = tt % 2
            for half in range(2):
                pst = self.psb(half, BF16)
                for kk in range(8):
                    k = half * 8 + kk
                    self.tr(pst[:, kk * 128:(kk + 1) * 128], xn[b][:, k * 128:(k + 1) * 128], self.ident_b[:],
                            ["xn%d" % b, "ident_b"], [("ps", half)])
                for kk in range(8):
                    k = half * 8 + kk
                    if g < 4:
                        dst = hg[g % 2][:, k, i * 128:(i + 1) * 128]
                        wkey = [("hg", g % 2, i)]
                    else:
                        dst = self.hT[:, k, (g - 4) * 512 + i * 128:(g - 4) * 512 + (i + 1) * 128]
                        wkey = [("hT", g - 4)]
                    src = pst[:, kk * 128:(kk + 1) * 128]
                    if half == 0:
                        self.DVE("tensor_scalar", [("ps", half), "A1", "modT"], wkey, out=dst, in0=src,
                                 scalar1=self.A1[:, k:k + 1], scalar2=self.modT[:, k:k + 1], op0=ALU.mult, op1=ALU.add)
                    else:
                        self.ACT(dst, src, AF.Identity, [("ps", half), "A1", "modT"], wkey,
                                 scale=self.A1[:, k:k + 1], bias=self.modT[:, k:k + 1])
            if g < 4 and i == 3:
                S.dma("sp", self.hT_d.ap().rearrange("k p t -> p k t")[:, :, g * 512:(g + 1) * 512], hg[g % 2][:],
                      r=[("hg", g % 2, ii) for ii in range(4)], w=[("hTd", g)])

        stage1(0)
        for tt in range(32):
            if tt + 1 < 32:
                stage1(tt + 1)
            stage2(tt)
        if "hT" in self.dbg:
            self.dump("hT", self.hT, [128, KC, NOWN], BF16, [("hT", q) for q in range(4)])
        self.release(m)

    def load_w(self, eng, dst, col0, ncols, wkey):
        src = self.w_in.ap().rearrange("(k p) c -> p k c", p=128)[:, :, col0:col0 + ncols]
        self.S.dma("pool", dst, src, w=[wkey])

    def hchunk(self, tc):
        if tc >= 4:
            base = (tc - 4) * 512
            return (lambda k, lo=0, hi=512, st=1: self.hT[:, k, base + lo:base + hi:st]), [("hT", tc - 4)]
        b = self.pf_i % 2
        self.pf_i += 1
        buf = self.pfbuf[b]
        key = ("pfbuf", b)
        self.S.dma("sp", buf[:], self.hT_d.ap().rearrange("k p t -> p k t")[:, :, tc * 512:(tc + 1) * 512],
                   r=[("hTd", tc)], w=[key])
        return (lambda k, lo=0, hi=512, st=1: buf[:, k, lo:hi:st]), [key]

    def norm_fm(self, ps, pskey, dst, dkeys, gain_ap, gkey, n):
        kf, sq, rs = self.nb_kf, self.nb_sq, self.nb_rs
        self.ACT(kf[:, :n], ps, AF.Copy, [pskey], ["nb_kf"])
        self.ACT(sq[:, :n], ps, AF.Square, [pskey], ["nb_sq"])
        p7 = self.psb(7)
        self.mm(p7[:, :n], self.ones_b[:], sq[:, :n], True, True, ["ones_b", "nb_sq"], [("ps", 7)])
        self.ACT(rs[:, :n], p7[:, :n], AF.Sqrt, [("ps", 7)], ["nb_rs"], scale=1.0 / DH, bias=EPS)
        self.DVE("reciprocal", ["nb_rs"], ["nb_rs"], out=rs[:, :n], in_=rs[:, :n])
        self.DVE("scalar_tensor_tensor", ["nb_kf", "nb_rs", gkey], dkeys, out=dst, in0=kf[:, :n], scalar=gain_ap,
                 in1=rs[:, :n], op0=ALU.mult, op1=ALU.mult)

    def attn(self, tiles, q_rhs, qkeys, nq, ob, zb, imp=None):
        O = self.psb(ob)[:, :nq]
        Z = self.psb(zb)[:, :nq]
        n = len(tiles)

        def emit_S(i):
            t = tiles[i]
            sb_ = i % 2
            Sps = self.psb(sb_)[:, :nq]
            adds = t.get("adds", [])
            self.mm(Sps, t["kT"], q_rhs, True, len(adds) == 0, t["kkeys"] + qkeys, [("ps", sb_)])
            for ai, (l, r_, ks) in enumerate(adds):
                self.mm(Sps, l, r_, False, ai == len(adds) - 1, ks, [("ps", sb_)])

        emit_S(0)
        for i in range(n):
            if i + 1 < n:
                emit_S(i + 1)
            t = tiles[i]
            pb = self.P_i % 3
            self.P_i += 1
            P = self.Pbuf[pb][:, :nq]
            self.ACT(P, self.psb(i % 2)[:, :nq], AF.Exp, [("ps", i % 2), t["bkey"]], [("P", pb)], bias=t["bias"])
            self.mm(O, t["v"], P, i == 0, i == n - 1, t["vkeys"] + [("P", pb)], [("ps", ob)])
            self.mm(Z, self.ones_b[:], P, i == 0, i == n - 1, ["ones_b", ("P", pb)], [("ps", zb)])
            if imp is not None:
                lfn, ib = imp
                self.mm(self.psb(ib)[0:64, :nq], lfn(i), P, i == 0, i == n - 1, ["ovl", ("P", pb)], [("ps", ib)])

    def phase2(self):
        S = self.S
        self.w_in = self.din("w_in", [D, IN_COLS])
        gq = self.din("nsa_q_gain_fm", [128, 1])
        gk = self.din("nsa_k_gain_fm", [128, 3])
        posT_in = self.din("cmp_posT", [128, 2, 32])
        w1_in = self.din("cmp_w1", [2, 4096, 128])
        w2_in = self.din("cmp_w2", [2, 128, 128])
        strips_in = self.din("nsa_strips", [8, 128, 8064])
        ovl_in = self.din("ovl", [128, 2, 64])
        eall_in = self.din("e_all", [64, 32, 128])
        selg_in = self.din("selg", [24, 24, 128])
        mmask_in = self.din("sel_mult", [128, 16, 64])
        amask_in = self.din("sel_add", [128, 16, 64])
        self.ynsa_d = self.dscratch("ynsa_d", [8, 128, NOWN], BF16)

        self.Pbuf = [self.sb("P%d" % i, [128, 512], BF16) for i in range(3)]
        self.P_i = 0
        self.nb_kf = self.sb("nb_kf", [128, 512], F32)
        self.nb_sq = self.sb("nb_sq", [128, 512], BF16)
        self.nb_rs = self.sb("nb_rs", [128, 512], F32)
        m = self.mark()
        gqs = self.sb("gqs", [128, 1], F32)
        gkt = self.sb("gkt", [128, 3], F32)
        S.dma("sp", gqs[:], gq.ap(), w=["gqs"])
        S.dma("sp", gkt[:], gk.ap(), w=["gkt"])
        self.DVE("tensor_scalar", ["gqs"], ["gqs"], out=gqs[:], in0=gqs[:], scalar1=SCALE, scalar2=None, op0=ALU.mult)
        posterm = self.sb("posterm", [128, 2], F32)
        gateT = self.sb("gateT", [24, NOWN], BF16)
        Wg = self.sb("Wg", [128, KC, 24], BF16)
        self.load_w("pool", Wg[:], OFF_GATE, 24, "Wg")
        for qc in range(4):
            p5 = self.psb(5)
            for k in range(KC):
                self.mm(p5[0:24, :], Wg[:, k, :], self.hT[:, k, qc * 512:(qc + 1) * 512], k == 0, k == KC - 1,
                        ["Wg", ("hT", qc)], [("ps", 5)])
            self.ACT(gateT[:, qc * 512:(qc + 1) * 512], p5[0:24, :], AF.Sigmoid, [("ps", 5)], [("gateT", qc)])

        for g in range(2):
            mg = self.mark()
            kselT = self.sb("kselT", [128, NLOC], BF16)
            vsel = self.sb("vsel", [128, 32, 128], BF16)
            kwinT = self.sb("kwinT", [128, 2560], BF16)
            vwin = self.sb("vwin", [128, 20, 128], BF16)
            qT = self.sb("qT", [128, 4, NOWN], BF16)
            kcT = self.sb("kcT", [128, 256], BF16)
            vc = self.sb("vc", [128, 2, 128], BF16)
            mA = self.mark()
            kcmpT = self.sb("kcmpT", [128, 2, NLOC], BF16)
            mB = self.mark()
            Wkv = self.sb("Wkv", [128, 6, KC, 128], BF16)
            self.pfbuf = [self.sb("pfbuf%d" % i, [128, KC, 512], BF16) for i in range(2)]
            self.pf_i = 0
            for i in range(6):
                self.load_w("pool", Wkv[:, i], OFF_KV + (i * 2 + g) * 128, 128, ("Wkv", i))
            for tc in range(8):
                self.pc(1)
                hf_, hkeys = self.hchunk(tc)
                for i in (0, 1, 2, 4):
                    if i == 4 and tc < 3:
                        continue
                    bk = 5 + (i // 2) % 2
                    p5 = self.psb(bk)
                    pk = ("ps", bk)
                    for k in range(KC):
                        self.mm(p5[:, :], Wkv[:, i, k, :], hf_(k), k == 0, k == KC - 1, [("Wkv", i)] + hkeys, [pk])
                    if i < 2:
                        self.ACT(kcmpT[:, i, tc * 512:(tc + 1) * 512], p5[:, :], AF.Copy, [pk], [("kcmpT", i)])
                    elif i == 2:
                        self.norm_fm(p5[:, :], pk, kselT[:, tc * 512:(tc + 1) * 512], [("kselT", tc)], gkt[:, 1:2], "gkt", 512)
                    else:
                        self.norm_fm(p5[:, :], pk, kwinT[:, (tc - 3) * 512:(tc - 2) * 512], [("kwinT", tc - 3)], gkt[:, 2:3], "gkt", 512)
                for ti in range(4):
                    tt = tc * 4 + ti
                    p6 = self.psb(6)
                    units = (3, 5) if tc >= 3 else (3,)
                    for ui, i in enumerate(units):
                        for k in range(KC):
                            self.mm(p6[:, ui * 128:(ui + 1) * 128], hf_(k, ti * 128, (ti + 1) * 128), Wkv[:, i, k, :],
                                    k == 0, k == KC - 1, [("Wkv", i)] + hkeys, [("ps", 6)])
                    self.DVE("tensor_copy", [("ps", 6)], [("vsel", tt)], out=vsel[:, tt, :], in_=p6[:, 0:128])
                    if tc >= 3:
                        self.DVE("tensor_copy", [("ps", 6)], [("vwin", tt - 12)], out=vwin[:, tt - 12, :], in_=p6[:, 128:256])
            self.release(mB)
            Wq = self.sb("Wq", [128, 4, KC, 128], BF16)
            for r in range(4):
                self.load_w("pool", Wq[:, r], (4 * g + r) * 128, 128, ("Wq", r))
            for tc in range(4, 8):
                hf_, hkeys = self.hchunk(tc)
                for r in range(4):
                    p5 = self.psb(5)
                    for k in range(KC):
                        self.mm(p5[:, :], Wq[:, r, k, :], hf_(k), k == 0, k == KC - 1, [("Wq", r)] + hkeys, [("ps", 5)])
                    self.norm_fm(p5[:, :], ("ps", 5), qT[:, r, (tc - 4) * 512:(tc - 3) * 512], [("qT", r, tc - 4)],
                                 gqs[:, 0:1], "gqs", 512)
            self.release(mB)
            posT = self.sb("posT", [128, 2, 32], BF16)
            W1 = self.sb("W1", [128, 2, 32, 128], BF16)
            W2 = self.sb("W2", [128, 2, 128], BF16)
            hid = self.sb("hid", [128, 2, 256], BF16)
            S.dma("pool", posT[:], posT_in.ap(), w=["posT"])
            for j in range(2):
                S.dma("pool", W1[:, j], w1_in.ap()[j].rearrange("(l d) o -> d l o", d=128), w=[("W1", j)])
                S.dma("pool", W2[:, j, :], w2_in.ap()[j], w=[("W2", j)])
            self.POOL("memset", [], [("hid", 0), ("hid", 1)], hid[:], 0.0)
            if g == 0:
                for j in range(2):
                    p5 = self.psb(5)
                    for l in range(32):
                        self.mm(p5[:, 0:1], W1[:, j, l, :], posT[:, j, l:l + 1], l == 0, l == 31, [("W1", j), "posT"], [("ps", 5)])
                    self.DVE("tensor_copy", [("ps", 5)], [("posterm", j)], out=posterm[:, j:j + 1], in_=p5[:, 0:1])
            for j in range(2):
                p5 = self.psb(5)
                for l in range(32):
                    self.mm(p5[:, 0:255], W1[:, j, l, :], kcmpT[:, j, l:l + 16 * 254 + 1:16], l == 0, l == 31,
                            [("W1", j), ("kcmpT", j)], [("ps", 5)])
                self.ACT(hid[:, j, 0:255], p5[:, 0:255], AF.Gelu_apprx_tanh, [("ps", 5), ("posterm", j)], [("hid", j)],
                         bias=posterm[:, j:j + 1])
            p5 = self.psb(5)
            self.mm(p5[:, 0:256], W2[:, 0, :], hid[:, 0, :], True, True, [("W2", 0), ("hid", 0)], [("ps", 5)])
            self.norm_fm(p5[:, 0:256], ("ps", 5), kcT[:, :], ["kcT"], gkt[:, 0:1], "gkt", 256)
            for ct in range(2):
                p6 = self.psb(6)
                self.mm(p6[:, 0:128], hid[:, 1, ct * 128:(ct + 1) * 128], W2[:, 1, :], True, True,
                        [("W2", 1), ("hid", 1)], [("ps", 6)])
                self.DVE("tensor_copy", [("ps", 6)], [("vc", ct)], out=vc[:, ct, :], in_=p6[:, 0:128])
            self.release(mA)

            ovl = self.sb("ovl", [128, 2, 64], BF16)
            eall = self.sb("eall", [64, 32, 128], BF16)
            selg = self.sb("selg", [24, 24, 128], BF16)
            mmask = self.sb("mmask", [128, 16, 64], F32)
            amask = self.sb("amask", [128, 16, 64], F32)
            S.dma("pool", ovl[:], ovl_in.ap(), w=["ovl"])
            S.dma("pool", eall[:], eall_in.ap(), w=["eall"])
            S.dma("pool", selg[:], selg_in.ap(), w=["selg"])
            S.dma("sp", mmask[:], mmask_in.ap(), w=["mmask"])
            S.dma("sp", amask[:], amask_in.ap(), w=["amask"])
            strips2 = [self.sb("strip%d" % i, [128, 8064], BF16) for i in range(2)]
            sidx = {"c": 0, "sw": 0}
            selbT = self.sb("selbT", [64, NOWN], BF16)
            ycmp = self.sb("ycmp", [128, 4, 512], F32)
            impacc = self.sb("impacc", [64, 512], F32)
            rz = self.sb("rz", [128, 512], F32)
            wgt = self.sb("wgt", [128, 512], F32)
            tmpf = self.sb("tmpf", [128, 512], F32)
            yacc = self.sb("yacc", [128, 512], F32)
            ybuf = [self.sb("ybuf%d" % i, [128, 512], BF16) for i in range(2)]
            sc = self.sb("sc", [128, 4, 64], F32)
            wk = self.sb("wk", [128, 64], F32)
            m8 = self.sb("m8", [128, 16], F32)
            selb = self.sb("selb", [128, 4, 64], BF16)
            yb_i = 0
            for qc in range(4):
                qb = NOWN + 512 * qc
                qsl = slice(qc * 512, (qc + 1) * 512)
                for r in range(4):
                    h = 4 * g + r
                    sc_i = sidx["c"] % 2
                    sidx["c"] += 1
                    strip = strips2[sc_i]
                    skey_c = ("strip_c", sc_i)
                    S.dma("pool", strip[:, 3968:8064], strips_in.ap()[h][:, 3968:8064], w=[skey_c])
                    tiles = []
                    for ct in range(2):
                        c0 = qb - 2048 * ct
                        tiles.append(dict(kT=kcT[:, ct * 128:(ct + 1) * 128], kkeys=["kcT"], v=vc[:, ct, :], vkeys=[("vc", ct)],
                                          adds=[(self.ident_b[:], strip[:, 3968 + c0:3968 + c0 + 512], ["ident_b", skey_c])],
                                          bias=(self.pfxb[:, 0:1] if ct == 0 else self.zero1[:, 0:1]),
                                          bkey=("pfxb" if ct == 0 else "zero1")))
                    self.attn(tiles, qT[:, r, qsl], [("qT", r, qc)], 512, 2, 3, imp=(lambda i: ovl[:, i, :], 4))
                    O = self.psb(2); Z = self.psb(3); I = self.psb(4)
                    self.DVE("tensor_scalar", [("ps", 3)], ["rz"], out=rz[:], in0=Z[:, :], scalar1=1e-30, scalar2=None, op0=ALU.max)
                    self.DVE("reciprocal", ["rz"], ["rz"], out=rz[:], in_=rz[:])
                    self.DVE("tensor_tensor", [("ps", 2), "rz"], [("ycmp", r)], out=ycmp[:, r, :], in0=O[:, :], in1=rz[:], op=ALU.mult)
                    if r == 0:
                        self.DVE("tensor_tensor", [("ps", 4), "rz"], ["impacc"], out=impacc[:], in0=I[0:64, :], in1=rz[0:64, :], op=ALU.mult)
                    else:
                        self.DVE("tensor_tensor", [("ps", 4), "rz"], ["tmpf"], out=tmpf[0:64, :], in0=I[0:64, :], in1=rz[0:64, :], op=ALU.mult)
                        self.DVE("tensor_tensor", ["tmpf", "impacc"], ["impacc"], out=impacc[:], in0=impacc[:], in1=tmpf[0:64, :], op=ALU.add)
                p5 = self.psb(5)
                for i in range(4):
                    self.mm(p5[:, i * 64:(i + 1) * 64], impacc[:, i * 128:(i + 1) * 128], self.ident_f[0:64, 0:64], True, True,
                            ["impacc", "ident_f"], [("ps", 5)])
                scv = sc[:].rearrange("p a b -> p (a b)")
                self.DVE("tensor_tensor", [("ps", 5), "mmask"], ["sc"], out=scv, in0=p5[:, 0:256],
                         in1=mmask[:, qc * 4:(qc + 1) * 4, :].rearrange("p a b -> p (a b)"), op=ALU.mult)
                self.DVE("tensor_tensor", ["sc", "amask"], ["sc"], out=scv, in0=scv,
                         in1=amask[:, qc * 4:(qc + 1) * 4, :].rearrange("p a b -> p (a b)"), op=ALU.add)
                for i in range(4):
                    self.DVE("max", ["sc"], ["m8"], out=m8[:, 0:8], in_=sc[:, i, :])
                    self.DVE("match_replace", ["sc", "m8"], ["wk"], out=wk[:], in_to_replace=m8[:, 0:8], in_values=sc[:, i, :], imm_value=-1e30)
                    self.DVE("max", ["wk"], ["m8"], out=m8[:, 8:16], in_=wk[:])
                    self.DVE("tensor_scalar", ["sc", "m8"], [("selb", i)], out=selb[:, i, :], in0=sc[:, i, :], scalar1=m8[:, 15:16],
                             scalar2=NEGB, op0=ALU.is_lt, op1=ALU.mult)
                    p6 = self.psb(6, BF16)
                    self.tr(p6[0:64, 0:128], selb[:, i, :], self.ident_b[:], [("selb", i), "ident_b"], [("ps", 6)])
                    self.ACT(selbT[:, qc * 512 + i * 128:qc * 512 + (i + 1) * 128], p6[0:64, 0:128], AF.Copy, [("ps", 6)], [("selbT", qc)])
                for r in range(4):
                    h = 4 * g + r
                    self.pc(1)
                    sw_i = sidx["sw"] % 2
                    sidx["sw"] += 1
                    strip = strips2[sw_i]
                    skey_sw = ("strip_sw", sw_i)
                    S.dma("pool", strip[:, 0:3968], strips_in.ap()[h][:, 0:3968], w=[skey_sw])
                    G = self.psb(4)
                    self.mm(G[:, :], selg[:, 3 * h + 0, :], gateT[:, qsl], True, True, ["selg", ("gateT", qc)], [("ps", 4)])
                    self.DVE("tensor_tensor", [("ps", 4), ("ycmp", r)], ["yacc"], out=yacc[:], in0=ycmp[:, r, :], in1=G[:, :], op=ALU.mult)
                    for br in (1, 2):
                        tiles = []
                        if br == 1:
                            kts = range(0, 16 + 4 * qc + 4)
                        else:
                            kts = range(16 + 4 * qc - 4, 16 + 4 * qc + 4)
                        for kt in kts:
                            dlt = qb - 128 * kt
                            bias = self.pfxb[:, 0:1] if kt < 16 else self.zero1[:, 0:1]
                            bkey = "pfxb" if kt < 16 else "zero1"
                            if br == 1:
                                c0 = min(dlt + 384, 2048)
                                adds = [(eall[:, kt, :], selbT[:, qsl], ["eall", ("selbT", qc)]),
                                        (self.ident_b[:], strip[:, c0:c0 + 512], ["ident_b", skey_sw])]
                                tiles.append(dict(kT=kselT[:, kt * 128:(kt + 1) * 128], kkeys=[("kselT", kt // 4)],
                                                  v=vsel[:, kt, :], vkeys=[("vsel", kt)], adds=adds, bias=bias, bkey=bkey))
                            else:
                                c0 = 2560 + dlt + 384
                                adds = [(self.ident_b[:], strip[:, c0:c0 + 512], ["ident_b", skey_sw])]
                                tiles.append(dict(kT=kwinT[:, (kt - 12) * 128:(kt - 11) * 128], kkeys=[("kwinT", (kt - 12) // 4)],
                                                  v=vwin[:, kt - 12, :], vkeys=[("vwin", kt - 12)], adds=adds, bias=bias, bkey=bkey))
                        self.attn(tiles, qT[:, r, qsl], [("qT", r, qc)], 512, 2, 3)
                        O = self.psb(2); Z = self.psb(3)
                        self.mm(G[:, :], selg[:, 3 * h + br, :], gateT[:, qsl], True, True, ["selg", ("gateT", qc)], [("ps", 4)])
                        self.DVE("reciprocal", [("ps", 3)], ["rz"], out=rz[:], in_=Z[:, :])
                        self.DVE("tensor_tensor", [("ps", 4), "rz"], ["wgt"], out=wgt[:], in0=rz[:], in1=G[:, :], op=ALU.mult)
                        self.DVE("tensor_tensor", [("ps", 2), "wgt"], ["tmpf"], out=tmpf[:], in0=wgt[:], in1=O[:, :], op=ALU.mult)
                        if br == 1:
                            self.DVE("tensor_tensor", ["tmpf", "yacc"], ["yacc"], out=yacc[:], in0=yacc[:], in1=tmpf[:], op=ALU.add)
                        else:
                            yb = yb_i % 2
                            yb_i += 1
                            self.DVE("tensor_tensor", ["tmpf", "yacc"], [("ybuf", yb)], out=ybuf[yb][:], in0=yacc[:], in1=tmpf[:], op=ALU.add)
                            S.dma("sp", self.ynsa_d.ap()[h][:, qsl], ybuf[yb][:], r=[("ybuf", yb)], w=[("ynsa_d", h, qc)])
            self.release(mg)
        if "ynsa" in self.dbg:
            o = self.dout("dbg_ynsa", [8, 128, NOWN], BF16)
            S.dma("sp", o.ap(), self.ynsa_d.ap())
        self.release(m)

    def phase3(self):
        S = self.S
        gq = self.din("dil_q_gain_fm", [128, 1])
        gk = self.din("dil_k_gain_fm", [128, 1])
        dstr_in = self.din("dil_strips", [12, 128, 1024])
        self.ydil_d = self.dscratch("ydil_d", [4, 128, NOWN], BF16)
        m = self.mark()
        hTp = self.sb("hTp", [128, KC, NOWN], BF16)
        for c in range(4):
            S.dma("sp", hTp[:, :, c * 512:(c + 1) * 512], self.hT_d.ap().rearrange("k p t -> p k t")[:, :, c * 512:(c + 1) * 512],
                  w=[("hTp", c)])
        gqs = self.sb("dgqs", [128, 1], F32)
        gks = self.sb("dgks", [128, 1], F32)
        S.dma("sp", gqs[:], gq.ap(), w=["dgqs"])
        S.dma("sp", gks[:], gk.ap(), w=["dgks"])
        self.DVE("tensor_scalar", ["dgqs"], ["dgqs"], out=gqs[:], in0=gqs[:], scalar1=SCALE, scalar2=None, op0=ALU.mult)
        dacc = self.sb("dacc", [128, NOWN], F32)
        zacc = self.sb("zacc", [128, NOWN], F32)
        ybuf = self.sb("dybuf", [128, NOWN], BF16)
        W3 = self.sb("W3", [128, 3, KC, 128], BF16)
        qT = self.sb("dqT", [128, NOWN], BF16)
        kT = self.sb("dkT", [128, NLOC], BF16)
        vt = self.sb("dvt", [128, 32, 128], BF16)
        strip = self.sb("dstrip", [128, 1024], BF16)
        for p in range(4):
            for gi, (win, dil) in enumerate(DIL_CFG):
                self.pc(1)
                hd = gi * 4 + p
                for i in range(3):
                    self.load_w("pool", W3[:, i], OFF_DIL + (i * 12 + hd) * 128, 128, ("W3", i))
                S.dma("pool", strip[:], dstr_in.ap()[hd], w=["dstrip"])
                for tc in range(4):
                    p5 = self.psb(5)
                    for k in range(KC):
                        self.mm(p5[:, :], W3[:, 0, k, :], self.hT[:, k, tc * 512:(tc + 1) * 512], k == 0, k == KC - 1,
                                [("W3", 0), ("hT", tc)], [("ps", 5)])
                    self.norm_fm(p5[:, :], ("ps", 5), qT[:, tc * 512:(tc + 1) * 512], [("dqT", tc)], gqs[:, 0:1], "dgqs", 512)
                    p6 = self.psb(6)
                    for k in range(KC):
                        self.mm(p6[:, :], W3[:, 1, k, :], self.hT[:, k, tc * 512:(tc + 1) * 512], k == 0, k == KC - 1,
                                [("W3", 1), ("hT", tc)], [("ps", 6)])
                    self.norm_fm(p6[:, :], ("ps", 6), kT[:, NOWN + tc * 512:NOWN + (tc + 1) * 512], ["dkT"], gks[:, 0:1], "dgks", 512)
                pl = 128 * dil
                pieces = []
                t0 = NOWN - pl
                while t0 < NOWN:
                    n = min(512, NOWN - t0)
                    pieces.append((t0, n))
                    t0 += n
                for (t0, n) in pieces:
                    p6 = self.psb(6)
                    for k in range(KC):
                        self.mm(p6[:, :n], W3[:, 1, k, :], hTp[:, k, t0:t0 + n], k == 0, k == KC - 1,
                                [("W3", 1), ("hTp", t0 // 512)], [("ps", 6)])
                    self.norm_fm(p6[:, :n], ("ps", 6), kT[:, t0:t0 + n], ["dkT"], gks[:, 0:1], "dgks", n)
                ntile = 16 // dil + 1
                for r in range(dil):
                    for mt in range(ntile):
                        p6 = self.psb(6)
                        if mt == 0:
                            st = NOWN - pl + r
                            src = lambda k: hTp[:, k, st:st + dil * 127 + 1:dil]
                            keys = [("hTp", c) for c in range((NOWN - pl) // 512, 4)]
                        else:
                            st = r + dil * 128 * (mt - 1)
                            src = lambda k: self.hT[:, k, st:st + dil * 127 + 1:dil]
                            keys = [("hT", c) for c in range(st // 512, (st + dil * 127) // 512 + 1)]
                        for k in range(KC):
                            self.mm(p6[:, 0:128], src(k), W3[:, 2, k, :], k == 0, k == KC - 1, [("W3", 2)] + keys, [("ps", 6)])
                        self.DVE("tensor_copy", [("ps", 6)], [("dvt", r * ntile + mt)], out=vt[:, r * ntile + mt, :], in_=p6[:, 0:128])
                nq = min(512, NOWN // dil)
                nch = (NOWN // dil) // nq
                for r in range(dil):
                    for ci in range(nch):
                        qs = r + dil * ci * nq
                        q_rhs = qT[:, qs:qs + dil * (nq - 1) + 1:dil]
                        tiles = []
                        for mt in range(ci * nq // 128, ci * nq // 128 + nq // 128 + 1):
                            delta = (128 + ci * nq) - 128 * mt
                            c0 = delta + 384
                            ks = NOWN - pl + r + dil * 128 * mt
                            tiles.append(dict(kT=kT[:, ks:ks + dil * 127 + 1:dil], kkeys=["dkT"], v=vt[:, r * ntile + mt, :],
                                              vkeys=[("dvt", r * ntile + mt)],
                                              adds=[(self.ident_b[:], strip[:, c0:c0 + nq], ["ident_b", "dstrip"])],
                                              bias=(self.pfxb[:, 0:1] if mt == 0 else self.zero1[:, 0:1]),
                                              bkey=("pfxb" if mt == 0 else "zero1")))
                        self.attn(tiles, q_rhs, [("dqT", c) for c in range(4)], nq, 2, 3)
                        O = self.psb(2)[:, :nq]; Z = self.psb(3)[:, :nq]
                        dsl = slice(qs, qs + dil * (nq - 1) + 1, dil)
                        if gi == 0:
                            self.DVE("tensor_copy", [("ps", 2)], ["dacc"], out=dacc[:, dsl], in_=O)
                            self.DVE("tensor_copy", [("ps", 3)], ["zacc"], out=zacc[:, dsl], in_=Z)
                        else:
                            self.DVE("tensor_tensor", [("ps", 2), "dacc"], ["dacc"], out=dacc[:, dsl], in0=dacc[:, dsl], in1=O, op=ALU.add)
                            self.DVE("tensor_tensor", [("ps", 3), "zacc"], ["zacc"], out=zacc[:, dsl], in0=zacc[:, dsl], in1=Z, op=ALU.add)
            self.DVE("reciprocal", ["zacc"], ["zacc"], out=zacc[:], in_=zacc[:])
            self.DVE("tensor_tensor", ["zacc", "dacc"], ["dybuf"], out=ybuf[:], in0=dacc[:], in1=zacc[:], op=ALU.mult)
            S.dma("sp", self.ydil_d.ap()[p], ybuf[:], r=["dybuf"], w=[("ydil_d", p)])
        if "ydil" in self.dbg:
            o = self.dout("dbg_ydil", [4, 128, NOWN], BF16)
            S.dma("sp", o.ap(), self.ydil_d.ap(), r=[("ydil_d", p) for p in range(4)])
        self.release(m)

    def row_bcast(self, dst, col0):
        diag = self.sb("diag", [128, 128], F32)
        for k in range(KC):
            self.DVE("tensor_scalar", ["ident_f", "modT"], ["diag"], out=diag[:], in0=self.ident_f[:],
                     scalar1=self.modT[:, col0 + k:col0 + k + 1], scalar2=None, op0=ALU.mult)
            p5 = self.psb(5)
            self.mm(p5[:, 0:128], self.ones_f[:], diag[:], True, True, ["ones_f", "diag"], [("ps", 5)])
            self.ACT(dst[:, k * 128:(k + 1) * 128], p5[:, 0:128], AF.Copy, [("ps", 5)], ["rowb"])

    def phase4(self):
        S = self.S
        wbn_in = self.din("w_br_nsa", [1024, D])
        wbd_in = self.din("w_br_dil", [512, D])
        wout_in = self.din("w_out", [D, D])
        self.y = self.dout("y", [NOWN, D])
        self.h2T_d = self.dscratch("h2T_d", [KC, 128, NOWN], BF16)
        m = self.mark()
        g1b = self.sb("g1b", [128, D], F32)
        self.row_bcast(g1b, 32)
        yn = self.sb("yn", [128, 8, 512], BF16)
        yd = self.sb("yd", [128, 4, 512], BF16)
        Wbn2 = [self.sb("Wbn%d" % i, [128, 8, 128], BF16) for i in range(2)]
        Wbd2 = [self.sb("Wbd%d" % i, [128, 4, 128], BF16) for i in range(2)]
        Wm2 = [self.sb("Wm%d" % i, [128, 2, KC, 128], BF16) for i in range(2)]
        mg = self.sb("mg", [128, KC, 512], BF16)
        Wo = self.sb("Wo", [128, KC, 512], BF16)
        xt = [self.sb("x4_%d" % i, [128, D], F32) for i in range(4)]
        xn = self.sb("xn4", [128, D], BF16)
        h2g = self.sb("h2g", [128, KC, 512], BF16)
        s1 = self.sb("s1", [128, 512], F32)
        s2 = self.sb("s2", [128, 512], F32)
        t1 = self.sb("t1", [128, 512], F32)
        st = self.sb("st4", [128, 3], F32)
        for qc in range(4):
            qsl = slice(qc * 512, (qc + 1) * 512)
            S.dma("sp", yn[:], self.ynsa_d.ap().rearrange("h p t -> p h t")[:, :, qsl], w=["yn"])
            S.dma("sp", yd[:], self.ydil_d.ap().rearrange("h p t -> p h t")[:, :, qsl], w=["yd"])
            for ti in range(4):
                tt = qc * 4 + ti
                S.dma("sp", xt[ti][:], self.x_in.ap()[NOWN + tt * 128:NOWN + (tt + 1) * 128, :], w=[("x4", ti)])
            for fc in range(KC):
                fsl = slice(fc * 128, (fc + 1) * 128)
                wb = fc % 2
                Wbn, Wbd, Wm = Wbn2[wb], Wbd2[wb], Wm2[wb]
                S.dma("pool", Wbn[:], wbn_in.ap().rearrange("(h d) f -> d h f", d=128)[:, :, fsl], w=[("Wbn", wb)])
                S.dma("pool", Wbd[:], wbd_in.ap().rearrange("(h d) f -> d h f", d=128)[:, :, fsl], w=[("Wbd", wb)])
                self.load_w("pool", Wm[:, 0], OFF_MERGE + fc * 128, 128, ("Wm", 0, wb))
                self.load_w("pool", Wm[:, 1], OFF_MERGE + D + fc * 128, 128, ("Wm", 1, wb))
                A = self.psb(2); Bm = self.psb(3); G1 = self.psb(0); G2 = self.psb(1)
                for h in range(8):
                    self.mm(A[:, :], Wbn[:, h, :], yn[:, h, :], h == 0, h == 7, [("Wbn", wb), "yn"], [("ps", 2)])
                for p in range(4):
                    self.mm(Bm[:, :], Wbd[:, p, :], yd[:, p, :], p == 0, p == 3, [("Wbd", wb), "yd"], [("ps", 3)])
                for k in range(KC):
                    self.mm(G1[:, :], Wm[:, 0, k, :], self.hT[:, k, qsl], k == 0, k == KC - 1, [("Wm", 0, wb), ("hT", qc)], [("ps", 0)])
                for k in range(KC):
                    self.mm(G2[:, :], Wm[:, 1, k, :], self.hT[:, k, qsl], k == 0, k == KC - 1, [("Wm", 1, wb), ("hT", qc)], [("ps", 1)])
                self.ACT(s1[:], G1[:, :], AF.Sigmoid, [("ps", 0)], ["s1"])
                self.ACT(s2[:], G2[:, :], AF.Sigmoid, [("ps", 1)], ["s2"])
                self.DVE("tensor_tensor", [("ps", 2), "s1"], ["s1"], out=s1[:], in0=s1[:], in1=A[:, :], op=ALU.mult)
                self.DVE("tensor_tensor", [("ps", 3), "s2"], ["s2"], out=s2[:], in0=s2[:], in1=Bm[:, :], op=ALU.mult)
                self.DVE("tensor_tensor", ["s1", "s2"], [("mg", fc)], out=mg[:, fc, :], in0=s1[:], in1=s2[:], op=ALU.add)
            if "merged" in self.dbg:
                if qc == 0:
                    self.dbg_merged = self.dout("dbg_merged", [128, KC, NOWN], BF16)
                S.dma("sp", self.dbg_merged.ap()[:, :, qsl], mg[:], r=[("mg", fc) for fc in range(KC)])
            for fo in range(4):
                fos = slice(fo * 512, (fo + 1) * 512)
                S.dma("pool", Wo[:], wout_in.ap().rearrange("(k p) f -> p k f", p=128)[:, :, fos], w=["Wo"])
                for ti in range(4):
                    po = self.psb(4 + ti % 2)
                    for fc in range(KC):
                        self.mm(po[:, :], mg[:, fc, ti * 128:(ti + 1) * 128], Wo[:, fc, :], fc == 0, fc == KC - 1,
                                [("mg", fc), "Wo"], [("ps", 4 + ti % 2)])
                    self.DVE("tensor_tensor", [("ps", 4 + ti % 2), "rowb"], ["t1"], out=t1[:], in0=g1b[:, fos], in1=po[:, :], op=ALU.mult)
                    self.DVE("tensor_tensor", ["t1", ("x4", ti)], [("x4", ti)], out=xt[ti][:, fos], in0=xt[ti][:, fos], in1=t1[:], op=ALU.add)
            for ti in range(4):
                tt = qc * 4 + ti
                S.dma("sp", self.y.ap()[tt * 128:(tt + 1) * 128, :], xt[ti][:], r=[("x4", ti)], w=[("y", tt)])
                self.ACT(xn[:], xt[ti][:], AF.Square, [("x4", ti)], ["xn4", "st4"], accum_out=st[:, 0:1])
                self.ACT(st[:, 1:2], st[:, 0:1], AF.Sqrt, ["st4"], ["st4"], scale=1.0 / D, bias=EPS)
                self.DVE("reciprocal", ["st4"], ["st4"], out=st[:, 2:3], in_=st[:, 1:2])
                self.DVE("tensor_scalar", [("x4", ti), "st4"], ["xn4"], out=xn[:], in0=xt[ti][:], scalar1=st[:, 2:3], scalar2=None, op0=ALU.mult)
                for half in range(2):
                    pst = self.psb(6 + half, BF16)
                    for kk in range(8):
                        k = half * 8 + kk
                        self.tr(pst[:, kk * 128:(kk + 1) * 128], xn[:, k * 128:(k + 1) * 128], self.ident_b[:],
                                ["xn4", "ident_b"], [("ps", 6 + half)])
                    for kk in range(8):
                        k = half * 8 + kk
                        dst = h2g[:, k, ti * 128:(ti + 1) * 128]
                        src = pst[:, kk * 128:(kk + 1) * 128]
                        if half == 0:
                            self.DVE("tensor_scalar", [("ps", 6 + half), "A2", "modT"], [("h2g", ti)], out=dst, in0=src,
                                     scalar1=self.A2[:, k:k + 1], scalar2=self.modT[:, 48 + k:49 + k], op0=ALU.mult, op1=ALU.add)
                        else:
                            self.ACT(dst, src, AF.Identity, [("ps", 6 + half), "A2", "modT"], [("h2g", ti)],
                                     scale=self.A2[:, k:k + 1], bias=self.modT[:, 48 + k:49 + k])
            S.dma("sp", self.h2T_d.ap().rearrange("k p t -> p k t")[:, :, qsl], h2g[:], r=[("h2g", ti) for ti in range(4)],
                  w=[("h2Td", qc)])
        if "h2T" in self.dbg:
            o = self.dout("dbg_h2T", [KC, 128, NOWN], BF16)
            S.dma("sp", o.ap(), self.h2T_d.ap(), r=[("h2Td", q) for q in range(4)])
        self.release(m)

    def peer_precast(self):
        S = self.S
        self.uT_in = self.din("peer_uT", [D, 16384])
        self.v_in = self.din("peer_v", [16384, D])
        self.uT_b = self.dscratch("uT_b", [64, 128, KC, 256], BF16)
        self.v_b = self.dscratch("v_b", [16384, D], BF16)
        self.pcq = []
        for i in range(32):
            k_, p0 = i // 2, (i % 2) * 64
            self.pcq.append((self.uT_b.ap()[:, p0:p0 + 64, k_, :].rearrange("c p e -> p c e"),
                             self.uT_in.ap()[i * 64:(i + 1) * 64, :].rearrange("p (c e) -> p c e", e=256), ("uTb", i // 2)))
        for i in range(32):
            self.pcq.append((self.v_b.ap()[i * 512:(i + 1) * 512, :], self.v_in.ap()[i * 512:(i + 1) * 512, :], ("vb", i // 2)))
        self.pcq.reverse()

    def pc(self, n=1):
        for _ in range(n):
            if getattr(self, "pcq", None):
                o, i_, key = self.pcq.pop()
                self.S.dma("pool", o, i_, w=[(key, len(self.pcq))])

    def phase5(self):
        S = self.S
        wq_in = self.din("peer_w_q", [D, D])
        gain_in = self.din("peer_q_gain_row", [1, D])
        skT_in = self.din("peer_skT", [128, 16, 128])
        self.sc_d = self.dscratch("sc_d", [NOWN, D], F32)
        self.pc(64)
        self.release(self.m_hT)
        m0 = self.mark()
        g2b = self.sb("g2b", [128, D], F32)
        self.row_bcast(g2b, 80)
        EC = 256
        NB = 4
        NEC = 16384 // EC
        coef = [self.sb("coef0", [128, 16384], BF16)]
        Ef = [self.sb("Ef%d" % i, [128, 8, 128], F32) for i in range(2)]
        C = [self.sb("C%d" % i, [128, 1024], BF16) for i in range(2)]
        sct = self.sb("sctc", [128, D], F32)
        e12 = [self.sb("e12_%d" % i, [128, 8, 2, 128], F32) for i in range(2)]
        t16 = self.sb("t16", [128, 2, 16], F32)
        wk = self.sb("pwk", [128, 256], F32)
        cand = self.sb("cand", [128, 16, 16], F32)
        c16 = self.sb("c16", [128, 16], F32)
        e16 = self.sb("e16", [128, 16], F32)
        scal = [self.sb("scal%d" % i, [128, 8, 8], F32) for i in range(2)]
        cnt = {"s3": 0, "pendD": None}

        def load_scores(tt):
            S.dma("sp", sct[:], self.sc_d.ap()[tt * 128:(tt + 1) * 128, :], r=[("scd", tt)], w=["sctc"])

        def topk(sbi, h):
            sl = scal[sbi]
            sk = ("scal", sbi, h)
            for pi in range(2):
                sv = sct[:, (2 * h + pi) * 128:(2 * h + pi + 1) * 128]
                self.DVE("max", ["sctc"], ["t16"], out=t16[:, pi, 0:8], in_=sv)
                self.DVE("match_replace", ["sctc", "t16"], ["pwk"], out=wk[:, 0:128], in_to_replace=t16[:, pi, 0:8], in_values=sv, imm_value=-1e30)
                self.DVE("max", ["pwk"], ["t16"], out=t16[:, pi, 8:16], in_=wk[:, 0:128])
            self.DVE("tensor_tensor", ["t16"], ["cand"], out=cand[:], in0=t16[:, 0, :].unsqueeze(2).to_broadcast([128, 16, 16]),
                     in1=t16[:, 1, :].unsqueeze(1).to_broadcast([128, 16, 16]), op=ALU.add)
            cf = cand[:].rearrange("p a b -> p (a b)")
            self.DVE("max", ["cand"], ["c16"], out=c16[:, 0:8], in_=cf)
            self.DVE("match_replace", ["cand", "c16"], ["pwk"], out=wk[:], in_to_replace=c16[:, 0:8], in_values=cf, imm_value=-1e30)
            self.DVE("max", ["pwk"], ["c16"], out=c16[:, 8:16], in_=wk[:])
            self.DVE("tensor_scalar", ["c16"], [sk], out=sl[:, h, 0:1], in0=c16[:, 0:1], scalar1=-1.0, scalar2=None, op0=ALU.mult)
            self.ACT(e16[:], c16[:], AF.Exp, ["c16", sk], ["e16", sk], bias=sl[:, h, 0:1], accum_out=sl[:, h, 1:2])
            self.ACT(sl[:, h, 2:3], sl[:, h, 1:2], AF.Ln, [sk], [sk])
            self.DVE("tensor_tensor", [sk], [sk], out=sl[:, h, 3:4], in0=sl[:, h, 0:1], in1=sl[:, h, 2:3], op=ALU.subtract)
            self.DVE("scalar_tensor_tensor", [sk, "t16"], [sk], out=sl[:, h, 5:6], in0=t16[:, 0, 0:1], scalar=-1.0, in1=sl[:, h, 2:3],
                     op0=ALU.mult, op1=ALU.subtract)
            self.DVE("tensor_scalar", ["t16"], [sk], out=sl[:, h, 6:7], in0=t16[:, 1, 0:1], scalar1=-1.0, scalar2=None, op0=ALU.mult)
            self.ACT(sl[:, h, 7:8], c16[:, 15:16], AF.Exp, ["c16", sk], [sk], bias=sl[:, h, 3:4])
            self.DVE("tensor_scalar", [sk], [sk], out=sl[:, h, 4:5], in0=sl[:, h, 7:8], scalar1=0.9995, scalar2=None, op0=ALU.mult)
            self.ACT(e12[sbi][:, h, 0, :], sct[:, (2 * h) * 128:(2 * h + 1) * 128], AF.Exp, ["sctc", sk], [("e12", sbi, h)], bias=sl[:, h, 5:6])
            self.ACT(e12[sbi][:, h, 1, :], sct[:, (2 * h + 1) * 128:(2 * h + 2) * 128], AF.Exp, ["sctc", sk], [("e12", sbi, h)], bias=sl[:, h, 6:7])

        def coef_step(sbi, a8, h):
            sl = scal[sbi]
            bi = cnt["s3"] % 2
            eng = "pool"
            cnt["s3"] += 1
            self.V(eng, "tensor_tensor", [("e12", sbi, h)], [("Ef", bi)], out=Ef[bi][:],
                   in0=e12[sbi][:, h, 0, a8 * 8:(a8 + 1) * 8].unsqueeze(2).to_broadcast([128, 8, 128]),
                   in1=e12[sbi][:, h, 1, :].unsqueeze(1).to_broadcast([128, 8, 128]), op=ALU.mult)
            eff = Ef[bi][:].rearrange("p a b -> p (a b)")
            self.DVE("scalar_tensor_tensor", [("Ef", bi), ("scal", sbi, h)], [("C", bi)], out=C[bi][:], in0=eff,
                     scalar=sl[:, h, 4:5], in1=eff, op0=ALU.is_ge, op1=ALU.mult)
            for j in range(2):
                self.mm(self.psb(1 + 2 * j)[:, :], self.ident_b[:], C[bi][:, j * 512:(j + 1) * 512], h == 0, h == 7,
                        ["ident_b", ("C", bi)], [("ps", 1 + 2 * j)])
            if h == 7:
                for j in range(2):
                    self.ACT(coef[sbi][:, a8 * 1024 + j * 512:a8 * 1024 + (j + 1) * 512], self.psb(1 + 2 * j)[:, :], AF.Copy,
                             [("ps", 1 + 2 * j)], [("coef", sbi, a8)])

        def flushD():
            pass

        m = self.mark()
        Wq = self.sb("pWq", [128, KC, D], BF16)
        for k in range(KC):
            S.dma("pool", Wq[:, k, :], wq_in.ap()[k * 128:(k + 1) * 128, :], w=[("pWq", k)])
        gqb = self.sb("gqb", [128, D], F32)
        S.dma("sp", gqb[:], gain_in.ap().partition_broadcast(128), w=["gqb"])
        skT = self.sb("skT", [128, 16, 128], BF16)
        S.dma("pool", skT[:], skT_in.ap(), w=["skT"])
        h2t = [self.sb("h2t%d" % i, [128, KC, 128], BF16) for i in range(2)]
        sq = self.sb("psq", [128, 512], F32)
        ss = self.sb("pss", [128, 16], F32)
        qn = self.sb("pqn", [128, D], BF16)
        tq = self.sb("ptq", [128, 512], F32)
        qnT = self.sb("pqnT", [128, 16, 128], BF16)
        scb = [self.sb("psct%d" % i, [128, D], F32) for i in range(2)]
        for tt in range(16):
            b = tt % 2
            S.dma("sp", h2t[b][:], self.h2T_d.ap().rearrange("k p t -> p k t")[:, :, tt * 128:(tt + 1) * 128], w=[("h2t", b)])
            for fo in range(4):
                pq = self.psb(2 * fo)
                for k in range(KC):
                    self.mm(pq[:, :], h2t[b][:, k, :], Wq[:, k, fo * 512:(fo + 1) * 512], k == 0, k == KC - 1,
                            [("h2t", b), ("pWq", k)], [("ps", 2 * fo)])
                self.ACT(sq[:], pq[:, :], AF.Square, [("ps", 2 * fo)], ["psq"])
                self.DVE("reduce_sum", ["psq"], [("pss", fo)], out=ss[:, fo * 4:(fo + 1) * 4],
                         in_=sq[:].rearrange("p (a b) -> p a b", b=128), axis=AX.X)
            self.ACT(ss[:], ss[:], AF.Sqrt, [("pss", f) for f in range(4)], [("pss", f) for f in range(4)], scale=1.0 / 128, bias=EPS)
            self.DVE("reciprocal", [("pss", f) for f in range(4)], [("pss", f) for f in range(4)], out=ss[:], in_=ss[:])
            for fo in range(4):
                pq = self.psb(2 * fo)
                self.DVE("tensor_tensor", [("ps", fo), ("pss", 0)], ["ptq"], out=tq[:].rearrange("p (a b) -> p a b", b=128),
                         in0=pq[:, :].rearrange("p (a b) -> p a b", b=128),
                         in1=ss[:, fo * 4:(fo + 1) * 4].unsqueeze(2).to_broadcast([128, 4, 128]), op=ALU.mult)
                self.DVE("tensor_tensor", ["ptq", "gqb"], [("pqn", fo)], out=qn[:, fo * 512:(fo + 1) * 512], in0=tq[:],
                         in1=gqb[:, fo * 512:(fo + 1) * 512], op=ALU.mult)
            for half in range(2):
                pst = self.psb(5 + 2 * half, BF16)
                for kk in range(8):
                    hp = half * 8 + kk
                    self.tr(pst[:, kk * 128:(kk + 1) * 128], qn[:, hp * 128:(hp + 1) * 128], self.ident_b[:],
                            [("pqn", hp // 4), "ident_b"], [("ps", 5 + 2 * half)])
                self.ACT(qnT[:, half * 8:(half + 1) * 8, :].rearrange("p a b -> p (a b)"), pst[:, :], AF.Copy, [("ps", 5 + 2 * half)], [("pqnT", half)])
            for fo in range(4):
                pq = self.psb(2 * fo)
                for i in range(4):
                    hp = fo * 4 + i
                    self.mm(pq[:, i * 128:(i + 1) * 128], qnT[:, hp, :], skT[:, hp, :], True, True,
                            [("pqnT", hp // 8), "skT"], [("ps", 2 * fo)])
                self.ACT(scb[b][:, fo * 512:(fo + 1) * 512], pq[:, :], AF.Copy, [("ps", 2 * fo)], [("psct", b)])
            S.dma("sp", self.sc_d.ap()[tt * 128:(tt + 1) * 128, :], scb[b][:], r=[("psct", b)], w=[("scd", tt)])
            if tt == 0:
                load_scores(0)
            elif tt == 1:
                for h in range(8):
                    topk(0, h)
            else:
                lo = (tt - 2) * 10
                for idx in range(lo, min(128, lo + 10)):
                    coef_step(0, idx // 8, idx % 8)
        self.release(m)

        coef.append(self.sb("coef1", [128, 16384], BF16))
        ub = [self.sb("ub%d" % i, [128, KC, EC], BF16) for i in range(NB)]
        vb = [self.sb("vb%d" % i, [128, EC // 128, D], BF16) for i in range(NB)]
        h2t = [self.sb("h2tc%d" % i, [128, KC, 128], BF16) for i in range(2)]
        x1c = self.sb("x1c", [128, 512], F32)
        Gb = [self.sb("Gb%d" % i, [128, EC], BF16) for i in range(2)]
        Wb = [self.sb("Wb%d" % i, [128, EC], BF16) for i in range(2)]
        WT = [self.sb("WT%d" % i, [128, EC // 128, 128], BF16) for i in range(2)]
        tf = self.sb("ptf", [128, 512], F32)

        def load_tile(tt):
            sbi = tt % 2
            S.dma("sp", h2t[sbi][:], self.h2T_d.ap().rearrange("k p t -> p k t")[:, :, tt * 128:(tt + 1) * 128], w=[("h2tc", sbi)])

        def load_chunk(ec):
            b = ec % NB
            S.dma("sp", ub[b][:], self.uT_b.ap()[ec], r=[("uTb", i) for i in range(16)], w=[("ub", b)])
            S.dma("sp", vb[b][:], self.v_b.ap()[ec * EC:(ec + 1) * EC, :].rearrange("(s p) d -> p s d", p=128),
                  r=[("vb", (ec * EC) // 1024)], w=[("vbuf", b)])

        def act_mm(sbi, ec):
            b = ec % NB
            pa = self.psb(0)[:, (ec % 2) * EC:(ec % 2 + 1) * EC]
            for k in range(KC):
                self.mm(pa, h2t[sbi][:, k, :], ub[b][:, k, :], k == 0, k == KC - 1, [("h2tc", sbi), ("ub", b)], [("ps", 0)])

        seq = [(tt, ec) for tt in range(16) for ec in range(NEC)]
        PF = NB - 1
        load_tile(0)
        load_scores(1)
        for i in range(PF):
            load_chunk(seq[i][1])
        act_mm(0, 0)
        for si, (tt, ec) in enumerate(seq):
            sbi = tt % 2
            nxt = (tt + 1) % 2
            if ec == 0 and tt + 1 < 16:
                load_tile(tt + 1)
            if si + PF < len(seq):
                load_chunk(seq[si + PF][1])
            b = ec % NB
            p2 = ec % 2
            pa = self.psb(0)[:, p2 * EC:(p2 + 1) * EC]
            self.ACT(Gb[p2][:], pa, AF.Gelu_apprx_tanh, [("ps", 0)], [("Gb", p2)])
            if si + 1 < len(seq):
                act_mm(seq[si + 1][0] % 2, seq[si + 1][1])
            self.DVE("tensor_tensor", [("Gb", p2), ("coef", sbi, (ec * EC) // 1024)], [("Wb", p2)], out=Wb[p2][:], in0=Gb[p2][:],
                     in1=coef[sbi][:, ec * EC:(ec + 1) * EC], op=ALU.mult)
            pt = self.psb(2, BF16)[:, p2 * EC:(p2 + 1) * EC]
            nsub = EC // 128
            for i in range(nsub):
                self.tr(pt[:, i * 128:(i + 1) * 128], Wb[p2][:, i * 128:(i + 1) * 128], self.ident_b[:], [("Wb", p2), "ident_b"], [("ps", 2)])
            self.ACT(WT[p2][:].rearrange("p a b -> p (a b)"), pt, AF.Copy, [("ps", 2)], [("WT", p2)])
            for i in range(nsub):
                for dc in range(4):
                    self.mm(self.psb(4 + dc)[:, :], WT[p2][:, i, :], vb[b][:, i, dc * 512:(dc + 1) * 512],
                            ec == 0 and i == 0, ec == NEC - 1 and i == nsub - 1, [("WT", p2), ("vbuf", b)], [("ps", 4 + dc)])
            if tt + 1 < 16:
                if ec == 0:
                    for h_ in range(8):
                        topk(nxt, h_)
                for idx in (2 * ec, 2 * ec + 1):
                    coef_step(nxt, idx // 8, idx % 8)
            if ec == NEC - 1:
                flushD()
                if tt + 2 < 16:
                    load_scores(tt + 2)
                for dc in range(4):
                    dsl = slice(dc * 512, (dc + 1) * 512)
                    S.dma("sp", x1c[:], self.y.ap()[tt * 128:(tt + 1) * 128, dsl], r=[("y", tt)], w=["x1c"])
                    self.DVE("tensor_tensor", [("ps", 4 + dc), "rowb"], ["ptf"], out=tf[:], in0=g2b[:, dsl], in1=self.psb(4 + dc)[:, :], op=ALU.mult)
                    self.DVE("tensor_tensor", ["ptf", "x1c"], ["x1c"], out=x1c[:], in0=x1c[:], in1=tf[:], op=ALU.add)
                    S.dma("sp", self.y.ap()[tt * 128:(tt + 1) * 128, dsl], x1c[:], r=["x1c"], w=[("y2", tt, dc)])
        self.release(m0)

    def finish(self):
        S = self.S
        S.add("sp", lambda e: e.nop(), extra_deps=[o.idx for o in S.ops if o.isdma])
        with ExitStack() as st:
            S.emit(st)
        return self.nc


def build(stop_after=None, dbg=()):
    B = Builder(stop_after, dbg)
    B.phase0()
    if stop_after != 1 and "nopeer" not in dbg:
        B.peer_precast()
    B.phase1()
    if stop_after != 1:
        if "skip2" not in dbg:
            B.phase2()
        else:
            B.w_in = B.din("w_in", [D, IN_COLS])
            B.Pbuf = [B.sb("P%d" % i, [128, 512], BF16) for i in range(3)]
            B.P_i = 0
            B.nb_kf = B.sb("nb_kf", [128, 512], F32)
            B.nb_sq = B.sb("nb_sq", [128, 512], BF16)
            B.nb_rs = B.sb("nb_rs", [128, 512], F32)
        B.phase3()
        B.phase4()
        if "nopeer" not in dbg:
            B.phase5()
    B.finish()
    return B


def _t5_bucket(n):
    n = np.maximum(n, 0)
    nf = np.maximum(n, 16).astype(np.float32)
    large = 16 + (np.log(nf / np.float32(16)) / np.float32(math.log(128.0)) * np.float32(16)).astype(np.int32)
    large = np.minimum(large, 31)
    return np.where(n < 16, n, large).astype(np.int64)


def _gather_strip(tbl_row, dist, valid):
    tb = np.concatenate([np.array([NEGB], np.float32), tbl_row.astype(np.float32)])
    idx = np.where(valid, _t5_bucket(dist) + 1, 0)
    return tb[idx]


def host_shared(inputs):
    m = {}
    rb = inputs["rel_bias"]
    p = np.arange(128)[:, None]
    strips = np.zeros((8, 128, 8064), np.float32)
    j = np.arange(2560)[None, :]
    r_sel = j - p - 384
    j2 = np.arange(1408)[None, :]
    r_win = j2 - p - 384
    j3 = np.arange(4096)[None, :]
    r_cmp = j3 - 16 * p - 31
    for h in range(8):
        strips[h, :, 0:2560] = _gather_strip(rb[h], r_sel, r_sel >= 0)
        strips[h, :, 2560:3968] = _gather_strip(rb[h], r_win, (r_win >= 0) & (r_win <= 511))
        strips[h, :, 3968:8064] = _gather_strip(rb[h], r_cmp, r_cmp >= 0)
    m["nsa_strips"] = strips
    dstrips = np.zeros((12, 128, 1024), np.float32)
    j4 = np.arange(1024)[None, :]
    r_d = j4 - p - 384
    for gi, (win, dil) in enumerate(DIL_CFG):
        for ps_ in range(4):
            hd = gi * 4 + ps_
            dstrips[hd] = _gather_strip(rb[8 + hd], r_d * dil, (r_d >= 0) & (r_d <= win // dil))
    m["dil_strips"] = dstrips
    c = (np.arange(2)[None, :, None] * 128 + np.arange(128)[:, None, None])
    jj = np.arange(64)[None, None, :]
    m["ovl"] = (((16 * c) < (64 * jj + 64)) & ((16 * c + 32) > (64 * jj))).astype(np.float32)
    jrow = np.arange(64)[:, None, None]
    kt = np.arange(32)[None, :, None]
    pp = np.arange(128)[None, None, :]
    m["e_all"] = (jrow == (2 * kt + pp // 64)).astype(np.float32)
    m["selg"] = np.ascontiguousarray(np.broadcast_to(np.eye(24, dtype=np.float32)[:, :, None], (24, 24, 128)))
    m["w_ada"] = inputs["w_ada"][0]
    m["b_ada_fm"] = np.ascontiguousarray(inputs["b_ada"][0].reshape(96, 128).T)
    m["norm1_g_fm"] = np.ascontiguousarray(inputs["norm1_g"][0].reshape(16, 128).T)
    m["norm2_g_fm"] = np.ascontiguousarray(inputs["norm2_g"][0].reshape(16, 128).T)
    m["w_in"] = inputs["w_in"][0]
    m["nsa_q_gain_fm"] = np.ascontiguousarray(inputs["nsa_q_gain"][0].reshape(128, 1))
    m["nsa_k_gain_fm"] = np.ascontiguousarray(inputs["nsa_k_gain"][0].T)
    m["cmp_posT"] = np.ascontiguousarray(inputs["cmp_pos"][0].transpose(2, 0, 1))
    m["cmp_w1"] = inputs["cmp_w1"][0]
    m["cmp_w2"] = inputs["cmp_w2"][0]
    m["dil_q_gain_fm"] = np.ascontiguousarray(inputs["dil_q_gain"][0].reshape(128, 1))
    m["dil_k_gain_fm"] = np.ascontiguousarray(inputs["dil_k_gain"][0].reshape(128, 1))
    m["w_br_nsa"] = inputs["w_br_nsa"][0]
    m["w_br_dil"] = inputs["w_br_dil"][0]
    m["w_out"] = inputs["w_out"][0]
    m["peer_w_q"] = inputs["peer_w_q"][0]
    m["peer_q_gain_row"] = np.ascontiguousarray(inputs["peer_q_gain"][0].reshape(1, 2048))
    m["peer_skT"] = np.ascontiguousarray(inputs["peer_sub_keys"][0].reshape(16, 128, 128).transpose(2, 0, 1))
    m["peer_uT"] = np.ascontiguousarray(inputs["peer_u"][0].T)
    m["peer_v"] = inputs["peer_v"][0]
    for hf in range(2):
        qi = np.arange(NOWN)
        cur = (qi + NOWN * hf) // 64
        jl = np.arange(64)[None, :]
        jg = jl - 32 + 32 * hf
        curc = cur[:, None]
        forced = (jg == 0) | (jg == curc) | (jg == curc - 1)
        bad = (jg > curc) | (jg < 0)
        add = np.where(forced & ~(jg < 0), 1e4, np.where(bad, -1e4, 0.0)).astype(np.float32)
        mult = np.where((forced & ~(jg < 0)) | bad, 0.0, 1.0).astype(np.float32)
        m["sel_add%d" % hf] = np.ascontiguousarray(add.reshape(16, 128, 64).transpose(1, 0, 2))
        m["sel_mult%d" % hf] = np.ascontiguousarray(mult.reshape(16, 128, 64).transpose(1, 0, 2))
    return m


def host_inputs(inputs, core, shared=None):
    if shared is None:
        shared = host_shared(inputs)
    b, hf = core // 2, core % 2
    x = inputs["x"]
    m = dict(shared)
    xl = np.zeros((NLOC, D), np.float32)
    if hf == 1:
        xl[:] = x[b]
    else:
        xl[NOWN:] = x[b, :NOWN]
    m["x_loc"] = xl
    m["pfx_bias"] = np.full((128, 1), 0.0 if hf == 1 else NEGB, np.float32)
    m["c_fm"] = np.ascontiguousarray(inputs["c"][b].reshape(16, 128).T)
    m["sel_add"] = shared["sel_add%d" % hf]
    m["sel_mult"] = shared["sel_mult%d" % hf]
    return m


def kernel(**inputs):
    inputs = {k: np.asarray(v) for k, v in inputs.items()}
    B = build()
    shared = host_shared(inputs)
    in_maps = []
    for c in range(8):
        hm = host_inputs(inputs, c, shared)
        in_maps.append({k: hm[k] for k in B.inputs})
    res = run_bass_kernel_spmd(B.nc, in_maps, core_ids=list(range(8)))
    out = np.zeros((4, 4096, D), np.float32)
    for c in range(8):
        out[c // 2, (c % 2) * NOWN:(c % 2 + 1) * NOWN] = res.results[c]["y"]
    return out
```
